# Optimizing a Trainium2 kernel written in Bass

```python
import jax, jax.numpy as jnp
from jax import lax
import numpy as np

D_MODEL = 1024
BATCH = 16
SEQ = 4096
DEPTH = 1

GRID_W = 64
CTX_LEN = 256
HEAD_DIM = 64
KV_HEADS = 2
A_HEADS = 8
B_HEADS = 8
GROUP = A_HEADS // KV_HEADS
Q_BLOCK = 128
WINDOW = 128
BAND = Q_BLOCK + 2 * WINDOW
ROPE_HALF = HEAD_DIM // 2
ROPE_THETA = 10000.0
N_EXPERTS = 32
TOP_K = 4
D_EXPERT = D_MODEL
SWIGLU_ALPHA = 1.702
SWIGLU_LIMIT = 7.0
RMS_EPS = 1e-6
ATTN_SCALE = HEAD_DIM ** -0.5
NEG_INF = -1e30
KV_W = KV_HEADS * HEAD_DIM
Q_A = A_HEADS * HEAD_DIM
Q_B = B_HEADS * HEAD_DIM
KV_COLS = 4 * KV_W
IN_COLS = KV_COLS + Q_A + Q_B + 2 * D_MODEL

kernel_name = 'hybrid_gated_gqa_window_moe_dit_block'


def rms_norm(x, g):
    xf = x.astype(jnp.float32)
    y = xf * lax.rsqrt(jnp.mean(xf * xf, axis=-1, keepdims=True) + RMS_EPS)
    return (y * g.astype(jnp.float32)).astype(x.dtype)


def adaln(cond, w, b):
    return jnp.split(jax.nn.silu(cond) @ w + b, 6, axis=-1)


def modulate(h, shift, scale):
    return h * (1 + scale) + shift


def to_heads(t):
    return t.reshape(t.shape[:-1] + (t.shape[-1] // HEAD_DIM, HEAD_DIM))


def split_kv(p):
    k_a, v_a, k_b, v_b = jnp.split(p, [KV_W, 2 * KV_W, 3 * KV_W], axis=-1)
    return to_heads(k_a), to_heads(v_a), to_heads(k_b), to_heads(v_b)


def split_qg(p):
    q_a, q_b, g_a, g_b = jnp.split(p, [Q_A, Q_A + Q_B, Q_A + Q_B + D_MODEL], axis=-1)
    return to_heads(q_a), to_heads(q_b), g_a, g_b


def axis_table(pos):
    inv = ROPE_THETA ** (-(jnp.arange(ROPE_HALF // 2, dtype=jnp.float32) * 2.0 / ROPE_HALF))
    ang = pos.astype(jnp.float32)[:, None] * inv[None, :]
    return jnp.cos(ang), jnp.sin(ang)


def rope_axis(x, cos, sin):
    x1, x2 = x[..., :ROPE_HALF // 2], x[..., ROPE_HALF // 2:]
    c, s = cos[:, None, :], sin[:, None, :]
    return jnp.concatenate([x1 * c - x2 * s, x2 * c + x1 * s], axis=-1)


def rope_2d(x, tabs):
    (cr, sr), (cc, sc) = tabs
    y = jnp.concatenate([rope_axis(x[..., :ROPE_HALF], cr, sr),
                         rope_axis(x[..., ROPE_HALF:], cc, sc)], axis=-1)
    return y.astype(x.dtype)


def group_q(q):
    return q.reshape(q.shape[:-2] + (KV_HEADS, q.shape[-2] // KV_HEADS, HEAD_DIM))


def attend(q, k, v, mask=None, sink=None):
    s = jnp.einsum('bqkgd,bskd->bkgqs', q, k).astype(jnp.float32) * ATTN_SCALE
    if mask is not None:
        s = jnp.where(mask, s, NEG_INF)
    if sink is not None:
        sk = jnp.broadcast_to(sink.astype(jnp.float32).reshape(KV_HEADS, -1, 1, 1), s.shape[:-1] + (1,))
        p = jax.nn.softmax(jnp.concatenate([s, sk], axis=-1), axis=-1)[..., :-1]
    else:
        p = jax.nn.softmax(s, axis=-1)
    return jnp.einsum('bkgqs,bskd->bqkgd', p.astype(v.dtype), v)


def global_attn(q, k, v):
    b, s = q.shape[:2]
    nb = s // Q_BLOCK
    qb = group_q(q).reshape(b, nb, Q_BLOCK, KV_HEADS, GROUP, HEAD_DIM).swapaxes(0, 1)
    out = lax.map(lambda qblk: attend(qblk, k, v), qb)
    return out.swapaxes(0, 1).reshape(b, s, A_HEADS * HEAD_DIM)


def band_blocks(t, nb):
    b = t.shape[0]
    tp = jnp.pad(t, ((0, 0), (WINDOW, WINDOW), (0, 0), (0, 0)))
    tb = tp.reshape(b, nb + 2, Q_BLOCK, KV_HEADS, HEAD_DIM)
    band = jnp.concatenate([tb[:, :-2], tb[:, 1:-1], tb[:, 2:]], axis=2)
    return band.swapaxes(0, 1)


def window_attn(q, k, v, k_ctx, v_ctx, sink):
    b, s = q.shape[:2]
    nb = s // Q_BLOCK
    n_ctx = k_ctx.shape[1]
    qb = group_q(q).reshape(b, nb, Q_BLOCK, KV_HEADS, GROUP, HEAD_DIM).swapaxes(0, 1)
    offs = jnp.arange(BAND) - WINDOW
    rel_ok = jnp.abs(offs[None, :] - jnp.arange(Q_BLOCK)[:, None]) <= WINDOW
    ctx_ok = jnp.ones((Q_BLOCK, n_ctx), dtype=bool)

    def one(args):
        qblk, kb, vb, n = args
        kpos = n * Q_BLOCK + offs
        band_ok = rel_ok & ((kpos >= 0) & (kpos < s))[None, :]
        mask = jnp.concatenate([band_ok, ctx_ok], axis=-1)
        return attend(qblk, jnp.concatenate([kb, k_ctx], axis=1),
                      jnp.concatenate([vb, v_ctx], axis=1), mask, sink)

    out = lax.map(one, (qb, band_blocks(k, nb), band_blocks(v, nb), jnp.arange(nb)))
    return out.swapaxes(0, 1).reshape(b, s, B_HEADS * HEAD_DIM)


def dense_attn(q, k, v, sink=None):
    return attend(group_q(q), k, v, None, sink).reshape(q.shape[:2] + (-1,))


def merge_branches(y_a, y_b, g_a, g_b, w_br_a, w_br_b, w_o):
    return (jax.nn.sigmoid(g_a) * (y_a @ w_br_a) + jax.nn.sigmoid(g_b) * (y_b @ w_br_b)) @ w_o


def moe(h, w_router, b_router, w_e1, b_e1, w_e2, b_e2):
    shape = h.shape
    t = h.reshape(-1, shape[-1])
    logits = (t @ w_router + b_router).astype(jnp.float32)
    top_logit, top_idx = lax.top_k(logits, TOP_K)
    top_w = jax.nn.softmax(top_logit, axis=-1)
    combine = jnp.einsum('tk,tke->te', top_w,
                         jax.nn.one_hot(top_idx, N_EXPERTS, dtype=jnp.float32)).astype(t.dtype)
    out = jnp.zeros_like(t)
    for e in range(N_EXPERTS):
        u = t @ w_e1[e] + b_e1[e]
        u_glu = jnp.minimum(u[:, 0::2], SWIGLU_LIMIT)
        u_lin = jnp.clip(u[:, 1::2], -SWIGLU_LIMIT, SWIGLU_LIMIT)
        act = u_glu * jax.nn.sigmoid(SWIGLU_ALPHA * u_glu) * (u_lin + 1)
        out = out + combine[:, e:e + 1] * (act @ w_e2[e] + b_e2[e])
    return out.reshape(shape)


def setup_inputs(seed: int = 0) -> dict:
    key = jax.random.key(seed)
    ks = jax.random.split(key, 22)
    L, D = DEPTH, D_MODEL

    def nrm(k, shape, scale):
        return jax.random.normal(k, shape, jnp.float32) * scale

    return {
        'x': nrm(ks[0], (BATCH, SEQ, D), 1.0),
        'c': nrm(ks[1], (BATCH, D), 1.0),
        'ctx': nrm(ks[2], (BATCH, CTX_LEN, D), 1.0),
        'c_ctx': nrm(ks[3], (D,), 1.0),
        'w_mod': nrm(ks[4], (L, D, 6 * D), 0.5 * D ** -0.5),
        'b_mod': nrm(ks[5], (L, 6 * D), 0.01),
        'norm1_g': 1.0 + nrm(ks[6], (L, D), 0.02),
        'norm2_g': 1.0 + nrm(ks[7], (L, D), 0.02),
        'w_in': nrm(ks[8], (L, D, IN_COLS), D ** -0.5),
        'q_norm_g': 1.0 + nrm(ks[9], (L, HEAD_DIM), 0.02),
        'k_norm_g': 1.0 + nrm(ks[10], (L, HEAD_DIM), 0.02),
        'sink': nrm(ks[11], (L, B_HEADS), 0.5),
        'w_br_a': nrm(ks[12], (L, Q_A, D), Q_A ** -0.5),
        'w_br_b': nrm(ks[13], (L, Q_B, D), Q_B ** -0.5),
        'w_o': nrm(ks[14], (L, D, D), D ** -0.5),
        'w_router': nrm(ks[15], (L, D, N_EXPERTS), D ** -0.5),
        'b_router': nrm(ks[16], (L, N_EXPERTS), 0.01),
        'w_e1': nrm(ks[17], (L, N_EXPERTS, D, 2 * D_EXPERT), D ** -0.5),
        'b_e1': nrm(ks[18], (L, N_EXPERTS, 2 * D_EXPERT), 0.01),
        'w_e2': nrm(ks[19], (L, N_EXPERTS, D_EXPERT, D), D_EXPERT ** -0.5),
        'b_e2': nrm(ks[20], (L, N_EXPERTS, D), 0.01),
        'final_g': 1.0 + nrm(ks[21], (D,), 0.02),
    }


def reference(x, c, ctx, c_ctx, w_mod, b_mod, norm1_g, norm2_g, w_in, q_norm_g, k_norm_g,
              sink, w_br_a, w_br_b, w_o, w_router, b_router, w_e1, b_e1, w_e2, b_e2, final_g):
    s = x.shape[1]
    rows = s // GRID_W
    row = jnp.repeat(jnp.arange(rows), GRID_W)
    col = jnp.tile(jnp.arange(GRID_W), rows)
    tabs = (axis_table(row), axis_table(col))
    for l in range(DEPTH):
        sh1, sc1, g1, sh2, sc2, g2 = [m[:, None, :] for m in adaln(c, w_mod[l], b_mod[l])]
        csh1, csc1, cg1, csh2, csc2, cg2 = adaln(c_ctx, w_mod[l], b_mod[l])

        h = modulate(rms_norm(x, norm1_g[l]), sh1, sc1)
        hc = modulate(rms_norm(ctx, norm1_g[l]), csh1, csc1)
        p = h @ w_in[l]
        k_a, v_a, k_b, v_b = split_kv(p[..., :KV_COLS])
        q_a, q_b, g_a, g_b = split_qg(p[..., KV_COLS:])
        k_a_c, v_a_c, k_b_c, v_b_c = split_kv(hc @ w_in[l, :, :KV_COLS])
        k_a_c = rms_norm(k_a_c, k_norm_g[l])

        q_a = rope_2d(rms_norm(q_a, q_norm_g[l]), tabs)
        k_a = rope_2d(rms_norm(k_a, k_norm_g[l]), tabs)
        q_b = rope_2d(q_b, tabs)
        k_b = rope_2d(k_b, tabs)

        y_a = global_attn(q_a, jnp.concatenate([k_a, k_a_c], axis=1),
                          jnp.concatenate([v_a, v_a_c], axis=1))
        y_b = window_attn(q_b, k_b, v_b, k_b_c, v_b_c, sink[l])
        x = x + g1 * merge_branches(y_a, y_b, g_a, g_b, w_br_a[l], w_br_b[l], w_o[l])

        h2 = modulate(rms_norm(x, norm2_g[l]), sh2, sc2)
        x = x + g2 * moe(h2, w_router[l], b_router[l], w_e1[l], b_e1[l], w_e2[l], b_e2[l])

        if l < DEPTH - 1:
            q_a_c, q_b_c, g_a_c, g_b_c = split_qg(hc @ w_in[l, :, KV_COLS:])
            y_a_c = dense_attn(rms_norm(q_a_c, q_norm_g[l]), k_a_c, v_a_c)
            y_b_c = dense_attn(q_b_c, k_b_c, v_b_c, sink[l])
            ctx = ctx + cg1 * merge_branches(y_a_c, y_b_c, g_a_c, g_b_c, w_br_a[l], w_br_b[l], w_o[l])
            hc2 = modulate(rms_norm(ctx, norm2_g[l]), csh2, csc2)
            ctx = ctx + cg2 * moe(hc2, w_router[l], b_router[l], w_e1[l], b_e1[l], w_e2[l], b_e2[l])
    return rms_norm(x, final_g)
```

```python
import contextlib
import numpy as np
import ml_dtypes
import concourse.bass as bass
import concourse.mybir as mybir
from concourse.bass_utils import run_bass_kernel_spmd

F32 = mybir.dt.float32
BF16 = mybir.dt.bfloat16
I32 = mybir.dt.int32
U32 = mybir.dt.uint32
AF = mybir.ActivationFunctionType
ALU = mybir.AluOpType
AX = mybir.AxisListType

D = 1024
CTX = 256
GRID_W = 64
TOPK = 4
EPS = 1e-6
MASKV = -30000.0

CFG = dict(NCORE=8, NB=2, T=4096, E=32)

ENGS = ["pe", "act", "dve", "pool", "sp"]


class Buf:
    __slots__ = ("w", "r", "rd")

    def __init__(self):
        self.w = None
        self.r = {}
        self.rd = []


class Prog:
    def __init__(self):
        self.ops = []
        self.byeng = {e: [] for e in ENGS}
        self.phase = 0
        self.pending_dma = []

    def barrier(self):
        deps = set(self.pending_dma)
        for en in ENGS:
            for i in reversed(self.byeng[en]):
                if self.ops[i]["fn"] is not None:
                    deps.add(i)
                    break
        for en in ENGS:
            idx = len(self.ops)
            self.ops.append(dict(eng=en, fn=None, deps=set(deps), dma=False, sig=False, phase=self.phase, ndma=1, out=False))
            self.byeng[en].append(idx)
        self.pending_dma = []

    def op(self, eng, fn, reads=(), writes=(), dma=False, ndma=1, out=False):
        idx = len(self.ops)
        if dma:
            self.pending_dma.append(idx)
        deps = set()
        for b in reads:
            if b.w is not None:
                deps.add(b.w)
        for b in writes:
            if b.w is not None:
                deps.add(b.w)
            deps.update(b.r.values())
            deps.update(b.rd)
        for b in reads:
            if dma:
                b.rd.append(idx)
            else:
                b.r[eng] = idx
        for b in writes:
            b.w = idx
            b.r = {}
            b.rd = []
        deps.discard(idx)
        self.ops.append(dict(eng=eng, fn=fn, deps=deps, dma=dma, sig=False, phase=self.phase, ndma=ndma, out=out))
        self.byeng[eng].append(idx)
        return idx


def build(cfg):
    NB, T, E = cfg["NB"], cfg["T"], cfg["E"]
    NG = T // 512
    NQB = T // 128
    NKT = NQB + 2
    NTOK = NB * T
    NTT = NTOK // 128
    NT = (NTOK * TOPK) // 512 + E
    NSLOT = NT * 512

    nc = bass.Bass("TRN2", target_bir_lowering=False)
    P = Prog()

    def din(name, shape, dt=F32):
        return nc.dram_tensor(name, list(shape), dt, kind="ExternalInput").ap()

    def dscr(name, shape, dt):
        return nc.dram_tensor(name, list(shape), dt).ap()

    x_d = din("x", [NTOK, D])
    ctx_d = din("ctx", [NB * CTX, D])
    cT_d = din("cT", [128, 8, NB + 1])
    wmod_d = din("w_mod", [D, 6 * D])
    bmod_d = din("b_mod", [6 * D])
    n1g_d = din("norm1_g", [D])
    n2g_d = din("norm2_g", [D])
    fg_d = din("final_g", [D])
    win_d = din("w_in", [D, 3584])
    gq_d = din("gq2", [128, 1])
    gk_d = din("gk2", [128, 1])
    sink_d = din("sinkT", [128, 4])
    wbra_d = din("w_br_a", [4, 128, D])
    wbrb_d = din("w_br_b", [4, 128, D])
    wo_d = din("w_o", [D, D])
    wr_d = din("w_router", [D, E])
    br_d = din("b_router", [E])
    we1_d = [din("w_e1_%d" % q, [E * 128, 2048]) for q in range(8)]
    be1_d = din("b_e1T", [E, 128, 16])
    we2_d = [din("w_e2_%d" % q, [E * 128, 2048]) for q in range(4)]
    be2_d = din("b_e2", [E, D])
    ropeC_d = din("ropeC", [128, T])
    ropeS_d = din("ropeS", [128, T])
    cm_d = din("cmats", [128, 4, 128])
    mb_d = din("maskb", [128, 2, 512])
    iota_d = din("iotas", [128, 128])
    pcol_d = din("pcol", [128, 8])
    out_d = nc.dram_tensor("out", [NTOK, D], F32, kind="ExternalOutput").ap()

    modrows_d = dscr("modrows", [NB + 1, 6 * D], F32)
    qt_d = dscr("qt_scr", [2, 128, 4, NTOK], BF16)
    g_d = dscr("g_scr", [16, 128, NTOK], BF16)
    x1_d = dscr("x1_scr", [NTOK, D], F32)
    h2_d = dscr("h2_scr", [NTOK, D], BF16)
    hs_d = dscr("hs_scr", [NSLOT, D], BF16)
    ys_d = dscr("ys_scr", [NSLOT, D], F32)

    B_modrows = Buf()
    B_qt = [[[Buf() for _ in range(8)] for _ in range(NG)] for _ in range(NB)]
    B_g = [[[Buf() for _ in range(16)] for _ in range(NG)] for _ in range(NB)]
    B_x1 = [Buf() for _ in range(NTT)]
    B_h2 = [Buf() for _ in range(NTT)]
    B_hs = [Buf() for _ in range(NTT * TOPK)]
    B_ys = [Buf() for _ in range(NT)]

    SB_LO, SB_HI = 16896, 229376
    cnt = [0]

    class Arena:
        def __init__(self, lo, hi):
            self.lo, self.hi, self.cur = lo, hi, lo

        def alloc(self, shape, dt, nbuf=None):
            cnt[0] += 1
            esz = 4 if dt in (F32, I32, U32) else 2
            n = esz
            for s in shape[1:]:
                n *= s
            n = (n + 63) // 64 * 64
            assert self.cur + n <= self.hi, ("SBUF overflow", shape, self.cur, self.hi)
            t = nc.alloc_sbuf_tensor_at("sb%d" % cnt[0], list(shape), dt, offset=self.cur)
            self.cur += n
            return t

        def sub(self):
            return Arena(self.cur, self.hi)

    root = Arena(SB_LO, SB_HI)

    cm_f = root.alloc([128, 4, 128], F32)
    cm_b = root.alloc([128, 4, 128], BF16)
    mb_b = root.alloc([128, 2, 512], BF16)
    iota_f = root.alloc([128, 128], F32)
    B_const = Buf()
    ident_f, ident_b = cm_f[:, 0, :], cm_b[:, 0, :]
    swap_b, bones_b, ltri_b = cm_b[:, 1, :], cm_b[:, 2, :], cm_b[:, 3, :]
    ones_b = None

    ones_t = root.alloc([128, 128], BF16)
    NTK = NTT * TOPK
    r_idx = root.alloc([128, NTK], F32)
    r_rank = root.alloc([128, NTK], F32)
    r_w = root.alloc([128, NTK], F32)
    r_pos = root.alloc([128, NTK], I32)
    r_run = root.alloc([128, E], F32)
    te_i = root.alloc([1, NT], I32)
    idx_b1 = root.alloc([128, NT], I32)
    idx_b2 = root.alloc([128, NT], I32)
    pcol = root.alloc([128, 8], F32)
    epsc = root.alloc([128, 2], F32)
    B_ridx, B_rrank, B_rw, B_rpos, B_run, B_te = Buf(), Buf(), Buf(), Buf(), Buf(), Buf()

    class _V:
        def __init__(self, ap):
            self.ap = ap

        def __getitem__(self, k):
            return self.ap[k]

    PQ = [nc.alloc_psum_tensor("pq%d" % i, [128, 1024], F32) for i in range(4)]
    pb = [_V(PQ[i // 2][:, (i % 2) * 512:(i % 2 + 1) * 512]) for i in range(8)]
    pbt = _V(PQ[3][:, 512:1024].bitcast(BF16))
    B_pb = [Buf() for _ in range(8)]
    B_pbt = B_pb[7]

    def dma(q, out, in_, reads=(), writes=(), **kw):
        return P.op(q, lambda e: e.dma_start(out=out, in_=in_, **kw), reads=reads, writes=writes, dma=True)

    def bc_load(q, dst, src_row, writes, reads=()):
        return dma(q, dst, src_row.partition_broadcast(128), reads=reads, writes=writes)

    class _Stop(Exception):
        pass

    def stop_at(n):
        if cfg.get("STOP", 99) == n:
            raise _Stop()

    try:
        dma("sp", cm_f[:], cm_d[:, :, :], writes=[B_const])
        dma("sp", iota_f[:], iota_d[:, :], writes=[B_const])
        dma("sp", pcol[:], pcol_d[:, :], writes=[B_const])
        P.op("dve", lambda e: e.tensor_copy(out=cm_b[:], in_=cm_f[:]), reads=[B_const], writes=[B_const])
        P.op("dve", lambda e: e.memset(ones_t[:], 1.0), writes=[B_const])
        P.op("dve", lambda e: e.memset(epsc[:, 0:1], EPS), writes=[B_const])
        P.op("dve", lambda e: e.memset(epsc[:, 1:2], 64.0 * EPS), writes=[B_const])
        P.op("dve", lambda e: e.memset(r_run[:], 0.0), writes=[B_run])

        ATT = root.sub()
        KT = [ATT.alloc([128, NKT * 128], BF16) for _ in range(2)]
        VA = ATT.alloc([128, NKT, 2, 2, 128], BF16)
        B_KT = [[Buf() for _ in range(NG + 1)] for _ in range(2)]
        B_VA = [Buf() for _ in range(NG + 1)]
        gq = ATT.alloc([128, 1], F32)
        gk = ATT.alloc([128, 1], F32)
        sinkT = ATT.alloc([128, 4], F32)
        sinkE = ATT.alloc([128, 4], F32)
        B_small = Buf()
        dma("sp", gq[:], gq_d[:, :], writes=[B_small])
        dma("sp", gk[:], gk_d[:, :], writes=[B_small])
        dma("sp", sinkT[:], sink_d[:, :], writes=[B_small])
        P.op("act", lambda e: e.activation(out=sinkE[:], in_=sinkT[:], func=AF.Exp), reads=[B_small], writes=[B_small])
        P.op("pool", lambda e: e.memset(VA[:, :, :, 0, 64:128], 1.0), writes=B_VA)
        P.op("pool", lambda e: e.memset(VA[:, :, :, 1, 0:64], 1.0), writes=B_VA)

        PH = ATT.sub()
        A = PH.sub()
        mb_f = A.alloc([128, 2, 512], F32)
        B_mbf = Buf()
        dma("sp", mb_f[:], mb_d[:, :, :], writes=[B_mbf])
        P.op("dve", lambda e: e.tensor_copy(out=mb_b[:], in_=mb_f[:]), reads=[B_mbf], writes=[B_const])
        cT = A.alloc([128, 8, NB + 1], F32)
        scb = A.alloc([128, NB + 1, 8, 128], BF16)
        sc1 = A.alloc([128, 8, NB + 1], BF16)
        bmod_bc = A.alloc([128, 6 * D], F32)
        wm = [A.alloc([128, 8, 512], BF16) for _ in range(2)]
        mrow = [A.alloc([128, 512], F32) for _ in range(2)]
        B_cT, B_scb, B_bmod = Buf(), Buf(), Buf()
        B_wm = [Buf(), Buf()]
        B_mrow = [Buf(), Buf()]
        dma("sp", cT[:], cT_d[:, :, :], writes=[B_cT])
        bc_load("sp", bmod_bc[:], bmod_d, writes=[B_bmod])
        P.op("act", lambda e: e.activation(out=sc1[:], in_=cT[:], func=AF.Silu), reads=[B_cT], writes=[B_cT])
        for v in range(NB + 1):
            for kc in range(8):
                P.op("dve", lambda e, v=v, kc=kc: e.tensor_copy(
                    out=scb[:, v, kc, :], in_=sc1[:, kc, v:v + 1].to_broadcast([128, 128])),
                    reads=[B_cT], writes=[B_scb])
        wmod_v = wmod_d.rearrange("(kc p) n -> p kc n", p=128)
        nmr = 0
        for cc in range(12):
            s = cc % 2
            dma("pool", wm[s][:], wmod_v[:, :, cc * 512:(cc + 1) * 512], writes=[B_wm[s]])
            for v in range(NB + 1):
                bk = (cc * (NB + 1) + v) % 4
                def mm(e, v=v, s=s, bk=bk):
                    r = None
                    for kc in range(8):
                        r = e.matmul(pb[bk][:], lhsT=scb[:, v, kc, :], rhs=wm[s][:, kc, :], start=(kc == 0), stop=(kc == 7))
                    return r
                P.op("pe", mm, reads=[B_scb, B_wm[s]], writes=[B_pb[bk]])
                ms = nmr % 2
                nmr += 1
                P.op("dve", lambda e, bk=bk, ms=ms, cc=cc: e.tensor_tensor(
                    out=mrow[ms][:], in0=pb[bk][:], in1=bmod_bc[:, cc * 512:(cc + 1) * 512], op=ALU.add),
                    reads=[B_pb[bk], B_bmod], writes=[B_mrow[ms]])
                dma("sp", modrows_d[v:v + 1, cc * 512:(cc + 1) * 512], mrow[ms][0:1, :], reads=[B_mrow[ms]], writes=[B_modrows])

        P.barrier()

        def rstd_from_ss(ss, out, n, dep_bufs, wbuf):
            P.op("act", lambda e: e.activation(out=out, in_=ss, func=AF.Sqrt, bias=epsc[:, 0:1], scale=1.0 / n),
                 reads=list(dep_bufs) + [B_const], writes=[wbuf])
            P.op("dve", lambda e: e.reciprocal(out=out, in_=out), reads=[wbuf], writes=[wbuf])

        stop_at(1)
        for b in range(NB):
            if b > 0:
                P.barrier()
            P.phase += 1
            Bm = PH.sub()
            win = Bm.alloc([128, 8, 3584], BF16)
            B_win = Buf()
            win_v = win_d.rearrange("(kc p) n -> p kc n", p=128)
            for (c0, c1) in ((0, 1536), (1536, 3584)):
                dma("pool", win[:, :, c0:c1], win_v[:, :, c0:c1], writes=[B_win])
            A1 = Bm.alloc([128, D], F32)
            B1 = Bm.alloc([128, D], F32)
            A1c = Bm.alloc([128, D], F32)
            B1c = Bm.alloc([128, D], F32)
            B_ab = Buf()
            tmpg = Bm.alloc([128, D], F32)
            bc_load("sp", tmpg[:], n1g_d, writes=[B_ab])
            for (v, Ad, Bd) in ((b, A1, B1), (NB, A1c, B1c)):
                bc_load("sp", Ad[:], modrows_d[v, D:2 * D], reads=[B_modrows], writes=[B_ab])
                bc_load("sp", Bd[:], modrows_d[v, 0:D], reads=[B_modrows], writes=[B_ab])
                P.op("dve", lambda e, Ad=Ad: e.scalar_tensor_tensor(out=Ad[:], in0=Ad[:], scalar=1.0, in1=tmpg[:], op0=ALU.add, op1=ALU.mult),
                     reads=[B_ab], writes=[B_ab])
            xt = [Bm.alloc([128, D], F32) for _ in range(2)]
            B_xt = [Buf(), Buf()]
            junk = Bm.alloc([128, D], F32)
            B_junk = Buf()
            ssv = [Bm.alloc([128, 1], F32) for _ in range(2)]
            B_ss = [Buf(), Buf()]
            hf = Bm.alloc([128, D], F32)
            B_hf = Buf()
            hb = [Bm.alloc([128, D], BF16) for _ in range(2)]
            B_hb = [Buf(), Buf()]
            hT = [Bm.alloc([128, 8, 512], BF16) for _ in range(2)]
            B_hT = [Buf(), Buf()]
            rC = [Bm.alloc([128, 512], F32) for _ in range(2)]
            rS = [Bm.alloc([128, 512], F32) for _ in range(2)]
            B_rope = [Buf(), Buf()]
            xg = [Bm.alloc([128, 512], BF16) for _ in range(2)]
            sq = [Bm.alloc([128, 512], BF16) for _ in range(2)]
            B_xg = [Buf(), Buf()]
            B_sq = [Buf(), Buf()]
            t1 = [Bm.alloc([128, 512], F32) for _ in range(2)]
            t2 = [Bm.alloc([128, 512], F32) for _ in range(2)]
            rs = [Bm.alloc([128, 512], F32) for _ in range(2)]
            B_t1 = [Buf(), Buf()]
            B_t2 = [Buf(), Buf()]
            B_rs = [Buf(), Buf()]
            qo = [Bm.alloc([128, 512], BF16) for _ in range(2)]
            B_qo = [Buf() for _ in range(2)]
            go = [Bm.alloc([128, 512], BF16) for _ in range(2)]
            B_go = [Buf() for _ in range(2)]
            ctr = dict(tile=0, blk=0, qo=0, go=0, mm=0)

            def prep_tile(src_rows, Ad, Bd, slot, col0):
                i = ctr["tile"] % 2
                ctr["tile"] += 1
                dma("sp", xt[i][:], src_rows, writes=[B_xt[i]])
                P.op("act", lambda e: e.activation(out=junk[:], in_=xt[i][:], func=AF.Square, accum_out=ssv[i][:]),
                     reads=[B_xt[i]], writes=[B_junk, B_ss[i]])
                rstd_from_ss(ssv[i][:], ssv[i][:], float(D), [B_ss[i]], B_ss[i])
                P.op("dve", lambda e: e.scalar_tensor_tensor(out=hf[:], in0=xt[i][:], scalar=ssv[i][:, 0:1], in1=Ad[:], op0=ALU.mult, op1=ALU.mult),
                     reads=[B_xt[i], B_ss[i], B_ab], writes=[B_hf])
                P.op("pool", lambda e: e.tensor_tensor(out=hb[i][:], in0=hf[:], in1=Bd[:], op=ALU.add),
                     reads=[B_hf, B_ab], writes=[B_hb[i]])
                def tr(e):
                    r = None
                    for kc in range(8):
                        r = e.transpose(pbt[:, kc * 128:(kc + 1) * 128], hb[i][:, kc * 128:(kc + 1) * 128], ident_b)
                    return r
                P.op("pe", tr, reads=[B_hb[i], B_const], writes=[B_pbt])
                P.op("act", lambda e: e.activation(out=hT[slot][:, :, col0:col0 + 128], in_=pbt[:].rearrange("p (k t) -> p k t", k=8), func=AF.Copy),
                     reads=[B_pbt], writes=[B_hT[slot]])

            def proj_mm(slot, c0, ntok):
                bk = ctr["mm"] % 4
                ctr["mm"] += 1
                def mm(e):
                    r = None
                    for kc in range(8):
                        r = e.matmul(pb[bk][:, 0:ntok], lhsT=win[:, kc, c0:c0 + 128], rhs=hT[slot][:, kc, 0:ntok], start=(kc == 0), stop=(kc == 7))
                    return r
                P.op("pe", mm, reads=[B_win, B_hT[slot]], writes=[B_pb[bk]])
                return bk

            def qk_block(slot, c0, ntok, norm, rope, gvec, dest, dest_bufs, ri):
                bk = proj_mm(slot, c0, ntok)
                i = ctr["blk"] % 2
                ctr["blk"] += 1
                if not norm and not rope:
                    P.op("act", lambda e: e.activation(out=dest, in_=pb[bk][:, 0:ntok], func=AF.Copy), reads=[B_pb[bk]], writes=dest_bufs)
                    return
                if norm:
                    P.op("act", lambda e: e.activation(out=xg[i][:, 0:ntok], in_=pb[bk][:, 0:ntok], func=AF.Copy, scale=gvec[:, 0:1]),
                         reads=[B_pb[bk], B_small], writes=[B_xg[i]])
                    P.op("act", lambda e: e.activation(out=sq[i][:, 0:ntok], in_=pb[bk][:, 0:ntok], func=AF.Square),
                         reads=[B_pb[bk]], writes=[B_sq[i]])
                    P.op("pe", lambda e: e.matmul(pb[5][:, 0:ntok], lhsT=bones_b, rhs=sq[i][:, 0:ntok], start=True, stop=True),
                         reads=[B_sq[i], B_const], writes=[B_pb[5]])
                    P.op("act", lambda e: e.activation(out=rs[i][:, 0:ntok], in_=pb[5][:, 0:ntok], func=AF.Sqrt, bias=epsc[:, 1:2], scale=1.0),
                         reads=[B_pb[5], B_const], writes=[B_rs[i]])
                    P.op("dve", lambda e: e.reciprocal(out=rs[i][:, 0:ntok], in_=rs[i][:, 0:ntok]), reads=[B_rs[i]], writes=[B_rs[i]])
                else:
                    P.op("act", lambda e: e.activation(out=xg[i][:, 0:ntok], in_=pb[bk][:, 0:ntok], func=AF.Copy),
                         reads=[B_pb[bk]], writes=[B_xg[i]])
                if rope:
                    P.op("pe", lambda e: e.matmul(pb[4][:, 0:ntok], lhsT=swap_b, rhs=xg[i][:, 0:ntok], start=True, stop=True),
                         reads=[B_xg[i], B_const], writes=[B_pb[4]])
                    P.op("dve", lambda e: e.tensor_tensor(out=t1[i][:, 0:ntok], in0=xg[i][:, 0:ntok], in1=rC[ri][:, 0:ntok], op=ALU.mult),
                         reads=[B_xg[i], B_rope[ri]], writes=[B_t1[i]])
                    P.op("dve", lambda e: e.tensor_tensor(out=t2[i][:, 0:ntok], in0=pb[4][:, 0:ntok], in1=rS[ri][:, 0:ntok], op=ALU.mult),
                         reads=[B_pb[4], B_rope[ri]], writes=[B_t2[i]])
                    if norm:
                        P.op("pool", lambda e: e.tensor_tensor(out=t1[i][:, 0:ntok], in0=t1[i][:, 0:ntok], in1=t2[i][:, 0:ntok], op=ALU.add),
                             reads=[B_t1[i], B_t2[i]], writes=[B_t1[i]])
                        P.op("dve", lambda e: e.scalar_tensor_tensor(out=dest, in0=t1[i][:, 0:ntok], scalar=8.0, in1=rs[i][:, 0:ntok], op0=ALU.mult, op1=ALU.mult),
                             reads=[B_t1[i], B_rs[i]], writes=dest_bufs)
                    else:
                        P.op("pool", lambda e: e.tensor_tensor(out=dest, in0=t1[i][:, 0:ntok], in1=t2[i][:, 0:ntok], op=ALU.add),
                             reads=[B_t1[i], B_t2[i]], writes=dest_bufs)
                else:
                    P.op("dve", lambda e: e.scalar_tensor_tensor(out=dest, in0=xg[i][:, 0:ntok], scalar=8.0, in1=rs[i][:, 0:ntok], op0=ALU.mult, op1=ALU.mult),
                         reads=[B_xg[i], B_rs[i]], writes=dest_bufs)

            def v_tiles(slot, ntiles, kt0, gi):
                for tt in range(ntiles):
                    def mm(e, tt=tt):
                        r = None
                        for kc in range(8):
                            r = e.matmul(pb[6][:, 0:256], lhsT=hT[slot][:, kc, tt * 128:(tt + 1) * 128], rhs=win[:, kc, 1280:1536], start=(kc == 0), stop=(kc == 7))
                        return r
                    P.op("pe", mm, reads=[B_win, B_hT[slot]], writes=[B_pb[6]])
                    kt = kt0 + tt
                    pv = pb[6][:, 0:256].rearrange("p (br kv d) -> p br kv d", br=2, kv=2)
                    P.op("act", lambda e, kt=kt, pv=pv: e.activation(out=VA[:, kt, :, 0, 0:64], in_=pv[:, :, 0, :], func=AF.Copy),
                         reads=[B_pb[6]], writes=[B_VA[gi]])
                    P.op("act", lambda e, kt=kt, pv=pv: e.activation(out=VA[:, kt, :, 1, 64:128], in_=pv[:, :, 1, :], func=AF.Copy),
                         reads=[B_pb[6]], writes=[B_VA[gi]])

            slot = 0
            for tt in range(2):
                prep_tile(ctx_d[b * CTX + tt * 128: b * CTX + (tt + 1) * 128, :], A1c, B1c, slot, tt * 128)
            qk_block(slot, 0, 256, True, False, gk, KT[0][:, NQB * 128:NKT * 128], [B_KT[0][NG]], 0)
            qk_block(slot, 128, 256, False, False, None, KT[1][:, NQB * 128:NKT * 128], [B_KT[1][NG]], 0)
            v_tiles(slot, 2, NQB, NG)

            def prep_group_tile(g, tt):
                slot_ = (g + 1) % 2
                ri_ = g % 2
                tok0_ = b * T + g * 512
                if tt == 0:
                    dma("sp", rC[ri_][:], ropeC_d[:, g * 512:(g + 1) * 512], writes=[B_rope[ri_]])
                    dma("sp", rS[ri_][:], ropeS_d[:, g * 512:(g + 1) * 512], writes=[B_rope[ri_]])
                prep_tile(x_d[tok0_ + tt * 128: tok0_ + (tt + 1) * 128, :], A1, B1, slot_, tt * 128)

            for tt in range(4):
                prep_group_tile(0, tt)
            for g in range(NG):
                slot = (g + 1) % 2
                ri = g % 2
                tok0 = b * T + g * 512
                thunks = []
                thunks.append(lambda: qk_block(slot, 0, 512, True, True, gk, KT[0][:, g * 512:(g + 1) * 512], [B_KT[0][g]], ri))
                thunks.append(lambda: qk_block(slot, 128, 512, False, True, None, KT[1][:, g * 512:(g + 1) * 512], [B_KT[1][g]], ri))
                thunks.append(lambda: v_tiles(slot, 4, g * 4, g))
                for br in range(2):
                    for j in range(4):
                        def qthunk(br=br, j=j):
                            qi = ctr["qo"] % 2
                            ctr["qo"] += 1
                            qk_block(slot, 256 + br * 512 + j * 128, 512, br == 0, True, gq if br == 0 else None, qo[qi][:], [B_qo[qi]], ri)
                            dma("sp", qt_d[br, :, j, tok0:tok0 + 512], qo[qi][:], reads=[B_qo[qi]], writes=[B_qt[b][g][br * 4 + j]])
                        thunks.append(qthunk)
                for gb in range(16):
                    def gthunk(gb=gb):
                        bk = proj_mm(slot, 1536 + gb * 128, 512)
                        gi = ctr["go"] % 2
                        ctr["go"] += 1
                        P.op("act", lambda e, bk=bk, gi=gi: e.activation(out=go[gi][:], in_=pb[bk][:], func=AF.Sigmoid),
                             reads=[B_pb[bk]], writes=[B_go[gi]])
                        dma("sp", g_d[gb, :, tok0:tok0 + 512], go[gi][:], reads=[B_go[gi]], writes=[B_g[b][g][gb]])
                    thunks.append(gthunk)
                nth = len(thunks)
                marks = {nth // 5: 0, 2 * nth // 5: 1, 3 * nth // 5: 2, 4 * nth // 5: 3}
                for ti_, th in enumerate(thunks):
                    th()
                    if g + 1 < NG and ti_ in marks:
                        prep_group_tile(g + 1, marks[ti_])

            stop_at(2)
            P.barrier()
            P.phase += 1
            Cm = PH.sub()
            wbr = [Cm.alloc([128, 4, D], BF16) for _ in range(2)]
            wo = Cm.alloc([128, 8, D], BF16)
            wr = Cm.alloc([128, 8, E], F32)
            brb = Cm.alloc([128, E], F32)
            B_wC = Buf()
            dma("pool", wbr[0][:], wbra_d.rearrange("j p n -> p j n"), writes=[B_wC])
            dma("pool", wbr[1][:], wbrb_d.rearrange("j p n -> p j n"), writes=[B_wC])
            dma("pool", wo[:], wo_d.rearrange("(kc p) n -> p kc n", p=128), writes=[B_wC])
            dma("sp", wr[:], wr_d.rearrange("(kc p) n -> p kc n", p=128), writes=[B_wC])
            bc_load("sp", brb[:], br_d, writes=[B_wC])
            g1bc = Cm.alloc([128, D], F32)
            A2 = Cm.alloc([128, D], F32)
            B2 = Cm.alloc([128, D], F32)
            B_c2 = Buf()
            tmpg2 = Cm.alloc([128, D], F32)
            bc_load("sp", tmpg2[:], n2g_d, writes=[B_c2])
            bc_load("sp", g1bc[:], modrows_d[b, 2 * D:3 * D], reads=[B_modrows], writes=[B_c2])
            bc_load("sp", B2[:], modrows_d[b, 3 * D:4 * D], reads=[B_modrows], writes=[B_c2])
            bc_load("sp", A2[:], modrows_d[b, 4 * D:5 * D], reads=[B_modrows], writes=[B_c2])
            P.op("dve", lambda e: e.scalar_tensor_tensor(out=A2[:], in0=A2[:], scalar=1.0, in1=tmpg2[:], op0=ALU.add, op1=ALU.mult),
                 reads=[B_c2], writes=[B_c2])
            QT = [[Cm.alloc([128, 4, 512], BF16) for _ in range(2)] for _ in range(2)]
            B_QT = [Buf(), Buf()]
            GT = Cm.alloc([128, 16, 512], BF16)
            B_GT = Buf()
            yT = [Cm.alloc([128, 4, 512], BF16) for _ in range(2)]
            B_yT = [Buf(), Buf()]
            rcp = [Cm.alloc([128, 512], F32) for _ in range(2)]
            B_rcp = [Buf(), Buf()]
            mT = Cm.alloc([128, 8, 512], BF16)
            B_mT = Buf()
            ma = Cm.alloc([128, 512], F32)
            mbb = Cm.alloc([128, 512], F32)
            B_ma, B_mbb = Buf(), Buf()
            xc = Cm.alloc([128, D], F32)
            x1 = Cm.alloc([128, D], F32)
            h2f = Cm.alloc([128, D], F32)
            h2b = Cm.alloc([128, D], BF16)
            h2T = Cm.alloc([128, 8, 128], F32)
            junk2 = Cm.alloc([128, D], F32)
            ss2 = Cm.alloc([128, 1], F32)
            B_xc, B_x1t, B_h2f, B_h2b, B_h2T, B_junk2, B_ss2 = Buf(), Buf(), Buf(), Buf(), Buf(), Buf(), Buf()
            lg = Cm.alloc([128, E], F32)
            mx8 = Cm.alloc([128, 8], F32)
            ix8 = Cm.alloc([128, 8], U32)
            e4 = Cm.alloc([128, 4], F32)
            s4 = Cm.alloc([128, 1], F32)
            nmx = Cm.alloc([128, 1], F32)
            Mf = Cm.alloc([128, E], F32)
            Mb = Cm.alloc([128, E], BF16)
            rkf = Cm.alloc([128, E], F32)
            oh = Cm.alloc([128, E], F32)
            B_lg, B_mx8, B_ix8, B_e4, B_M, B_rkf, B_oh = Buf(), Buf(), Buf(), Buf(), Buf(), Buf(), Buf()
            cc = dict(s=0, o=0, p=0)

            LA = 1
            PT2 = [Cm.alloc([128, 1024], BF16) for _ in range(3)]
            B_PT2 = [Buf() for _ in range(3)]

            def attn_items(qslot, br, nblk, nloc):
                if br == 0:
                    kts = [(kt, None) for kt in range(NKT)]
                else:
                    kts = []
                    if nblk > 0:
                        kts.append((nblk - 1, 0))
                    kts.append((nblk, None))
                    if nblk < NQB - 1:
                        kts.append((nblk + 1, 1))
                    kts += [(NQB, None), (NQB + 1, None)]
                oq = 2 + cc["o"] % 2
                cc["o"] += 1
                return [dict(qslot=qslot, br=br, nloc=nloc, kt=kt, mk=mk, first=(ci == 0), last=(ci == len(kts) - 1), oq=oq)
                        for ci, (kt, mk) in enumerate(kts)]

            def attn_s(it):
                br, kt, mk = it["br"], it["kt"], it["mk"]
                ss_ = cc["s"] % 2
                cc["s"] += 1
                it["ss"] = ss_
                gk_ = NG if kt >= NQB else kt // 4
                it["gk"] = gk_
                def smm(e):
                    r = None
                    for kvh in range(2):
                        r0 = kvh * 64
                        rhsq = QT[it["qslot"]][br][r0:r0 + 64, :, it["nloc"] * 128:(it["nloc"] + 1) * 128]
                        o2 = PQ[ss_][:, kvh * 512:(kvh + 1) * 512]
                        r = e.matmul(o2.rearrange("p (j q) -> p j q", j=4), lhsT=KT[br][r0:r0 + 64, kt * 128:(kt + 1) * 128], rhs=rhsq, start=True, stop=(mk is None))
                    if mk is not None:
                        for kvh in range(2):
                            r = e.matmul(PQ[ss_][:, kvh * 512:(kvh + 1) * 512], lhsT=ident_b, rhs=mb_b[:, mk, :], start=False, stop=True)
                    return r
                P.op("pe", smm, reads=[B_KT[br][gk_], B_QT[it["qslot"]], B_const], writes=[B_pb[2 * ss_], B_pb[2 * ss_ + 1]])

            def attn_pv(it):
                br, kt, oq, ss_ = it["br"], it["kt"], it["oq"], it["ss"]
                nloc = it["nloc"]
                pi = cc["p"] % 3
                cc["p"] += 1
                P.op("act", lambda e: e.activation(out=PT2[pi][:], in_=PQ[ss_][:], func=AF.Exp, scale=0.125),
                     reads=[B_pb[2 * ss_], B_pb[2 * ss_ + 1]], writes=[B_PT2[pi]])
                def pv(e):
                    r = None
                    for kvh in range(2):
                        r = e.matmul(PQ[oq][:, kvh * 512:(kvh + 1) * 512], lhsT=VA[:, kt, br, kvh, :], rhs=PT2[pi][:, kvh * 512:(kvh + 1) * 512],
                                     start=it["first"], stop=it["last"])
                    return r
                P.op("pe", pv, reads=[B_VA[it["gk"]], B_PT2[pi]], writes=[B_pb[2 * oq], B_pb[2 * oq + 1]])
                if not it["last"]:
                    return
                for kvh in range(2):
                    ob = 2 * oq + kvh
                    o0, d0 = (0, 64) if kvh == 0 else (64, 0)
                    ri_ = kvh
                    den = pb[ob][d0:d0 + 64, :]
                    if br == 1:
                        P.op("dve", lambda e, ri_=ri_, o0=o0, d0=d0, den=den: e.tensor_tensor(out=rcp[ri_][o0:o0 + 64, :].rearrange("p (j q) -> p j q", j=4),
                                                              in0=den.rearrange("p (j q) -> p j q", j=4),
                                                              in1=sinkE[d0:d0 + 64, :].unsqueeze(2).to_broadcast([64, 4, 128]), op=ALU.add),
                             reads=[B_pb[ob], B_small], writes=[B_rcp[ri_]])
                        P.op("dve", lambda e, ri_=ri_, o0=o0: e.reciprocal(out=rcp[ri_][o0:o0 + 64, :], in_=rcp[ri_][o0:o0 + 64, :]),
                             reads=[B_rcp[ri_]], writes=[B_rcp[ri_]])
                    else:
                        P.op("dve", lambda e, ri_=ri_, o0=o0, den=den: e.reciprocal(out=rcp[ri_][o0:o0 + 64, :], in_=den), reads=[B_pb[ob]], writes=[B_rcp[ri_]])
                    P.op("dve", lambda e, ri_=ri_, o0=o0, ob=ob: e.tensor_tensor(out=yT[br][o0:o0 + 64, :, nloc * 128:(nloc + 1) * 128],
                                                          in0=pb[ob][o0:o0 + 64, :].rearrange("p (j q) -> p j q", j=4),
                                                          in1=rcp[ri_][o0:o0 + 64, :].rearrange("p (j q) -> p j q", j=4), op=ALU.mult),
                         reads=[B_pb[ob], B_rcp[ri_]], writes=[B_yT[br]])

            def load_q(g):
                qslot = g % 2
                tok0 = b * T + g * 512
                for br in range(2):
                    dma("sp", QT[qslot][br][:], qt_d[br, :, :, tok0:tok0 + 512], reads=B_qt[b][g], writes=[B_QT[qslot]])

            load_q(0)
            for g in range(NG):
                qslot = g % 2
                tok0 = b * T + g * 512
                if g + 1 < NG:
                    load_q(g + 1)
                dma("sp", GT[:], g_d[:, :, tok0:tok0 + 512].rearrange("c p t -> p c t"), reads=B_g[b][g], writes=[B_GT])
                items = []
                for nloc in range(4):
                    for br in range(2):
                        items += attn_items(qslot, br, g * 4 + nloc, nloc)
                for ii in range(len(items) + LA):
                    if ii < len(items):
                        attn_s(items[ii])
                    if ii - LA >= 0:
                        attn_pv(items[ii - LA])
                for fb in range(8):
                    for br in range(2):
                        bk = 4 + br
                        def mm(e, br=br, fb=fb, bk=bk):
                            r = None
                            for j in range(4):
                                r = e.matmul(pb[bk][:], lhsT=wbr[br][:, j, fb * 128:(fb + 1) * 128], rhs=yT[br][:, j, :], start=(j == 0), stop=(j == 3))
                            return r
                        P.op("pe", mm, reads=[B_wC, B_yT[br]], writes=[B_pb[bk]])
                    P.op("dve", lambda e, fb=fb: e.tensor_tensor(out=ma[:], in0=pb[4][:], in1=GT[:, fb, :], op=ALU.mult),
                         reads=[B_pb[4], B_GT], writes=[B_ma])
                    P.op("dve", lambda e, fb=fb: e.tensor_tensor(out=mbb[:], in0=pb[5][:], in1=GT[:, 8 + fb, :], op=ALU.mult),
                         reads=[B_pb[5], B_GT], writes=[B_mbb])
                    P.op("pool", lambda e, fb=fb: e.tensor_tensor(out=mT[:, fb, :], in0=ma[:], in1=mbb[:], op=ALU.add),
                         reads=[B_ma, B_mbb], writes=[B_mT])
                for tt in range(4):
                    ti = (tok0 + tt * 128) // 128
                    rows = slice(tok0 + tt * 128, tok0 + (tt + 1) * 128)
                    dma("sp", xc[:], x_d[rows, :], writes=[B_xc])
                    for nh in range(2):
                        bk = 4 + nh
                        def mm(e, tt=tt, nh=nh, bk=bk):
                            r = None
                            for kc in range(8):
                                r = e.matmul(pb[bk][:], lhsT=mT[:, kc, tt * 128:(tt + 1) * 128], rhs=wo[:, kc, nh * 512:(nh + 1) * 512], start=(kc == 0), stop=(kc == 7))
                            return r
                        P.op("pe", mm, reads=[B_wC, B_mT], writes=[B_pb[bk]])
                        P.op("dve", lambda e, nh=nh, bk=bk: e.tensor_tensor(out=x1[:, nh * 512:(nh + 1) * 512], in0=pb[bk][:], in1=g1bc[:, nh * 512:(nh + 1) * 512], op=ALU.mult),
                             reads=[B_pb[bk], B_c2], writes=[B_x1t])
                    P.op("pool", lambda e: e.tensor_tensor(out=x1[:], in0=x1[:], in1=xc[:], op=ALU.add), reads=[B_x1t, B_xc], writes=[B_x1t])
                    dma("sp", x1_d[rows, :], x1[:], reads=[B_x1t], writes=[B_x1[ti]])
                    P.op("act", lambda e: e.activation(out=junk2[:], in_=x1[:], func=AF.Square, accum_out=ss2[:]),
                         reads=[B_x1t], writes=[B_junk2, B_ss2])
                    rstd_from_ss(ss2[:], ss2[:], float(D), [B_ss2], B_ss2)
                    P.op("dve", lambda e: e.scalar_tensor_tensor(out=h2f[:], in0=x1[:], scalar=ss2[:, 0:1], in1=A2[:], op0=ALU.mult, op1=ALU.mult),
                         reads=[B_x1t, B_ss2, B_c2], writes=[B_h2f])
                    P.op("pool", lambda e: e.tensor_tensor(out=h2f[:], in0=h2f[:], in1=B2[:], op=ALU.add), reads=[B_h2f, B_c2], writes=[B_h2f])
                    P.op("act", lambda e: e.activation(out=h2b[:], in_=h2f[:], func=AF.Copy), reads=[B_h2f], writes=[B_h2b])
                    dma("sp", h2_d[rows, :], h2b[:], reads=[B_h2b], writes=[B_h2[ti]])
                    for hh in range(2):
                        def tr(e, hh=hh):
                            r = None
                            for k4 in range(4):
                                kc = hh * 4 + k4
                                r = e.transpose(pb[6][:, k4 * 128:(k4 + 1) * 128], h2f[:, kc * 128:(kc + 1) * 128], ident_f)
                            return r
                        P.op("pe", tr, reads=[B_h2f, B_const], writes=[B_pb[6]])
                        P.op("act", lambda e, hh=hh: e.activation(out=h2T[:, hh * 4:(hh + 1) * 4, :], in_=pb[6][:].rearrange("p (k t) -> p k t", k=4), func=AF.Copy),
                             reads=[B_pb[6]], writes=[B_h2T])
                    def rmm(e):
                        r = None
                        for kc in range(8):
                            r = e.matmul(pb[6][:, 0:E], lhsT=h2T[:, kc, :], rhs=wr[:, kc, :], start=(kc == 0), stop=(kc == 7))
                        return r
                    P.op("pe", rmm, reads=[B_h2T, B_wC], writes=[B_pb[6]])
                    P.op("dve", lambda e: e.tensor_tensor(out=lg[:], in0=pb[6][:, 0:E], in1=brb[:], op=ALU.add), reads=[B_pb[6], B_wC], writes=[B_lg])
                    P.op("dve", lambda e: e.max(out=mx8[:], in_=lg[:]), reads=[B_lg], writes=[B_mx8])
                    P.op("dve", lambda e: e.max_index(out=ix8[:], in_max=mx8[:], in_values=lg[:]), reads=[B_lg, B_mx8], writes=[B_ix8])
                    c0 = ti * TOPK
                    P.op("dve", lambda e, c0=c0: e.tensor_copy(out=r_idx[:, c0:c0 + 4], in_=ix8[:, 0:4]), reads=[B_ix8], writes=[B_ridx])
                    P.op("dve", lambda e: e.tensor_scalar(out=nmx[:], in0=mx8[:, 0:1], scalar1=-1.0, scalar2=None, op0=ALU.mult), reads=[B_mx8], writes=[B_e4])
                    P.op("act", lambda e: e.activation(out=e4[:], in_=mx8[:, 0:4], func=AF.Exp, bias=nmx[:, 0:1], scale=1.0, accum_out=s4[:]),
                         reads=[B_mx8, B_e4], writes=[B_e4])
                    P.op("dve", lambda e: e.reciprocal(out=s4[:], in_=s4[:]), reads=[B_e4], writes=[B_e4])
                    P.op("dve", lambda e, c0=c0: e.tensor_scalar(out=r_w[:, c0:c0 + 4], in0=e4[:], scalar1=s4[:, 0:1], scalar2=None, op0=ALU.mult),
                         reads=[B_e4], writes=[B_rw])
                    P.op("dve", lambda e: e.tensor_scalar(out=Mf[:], in0=lg[:], scalar1=mx8[:, 3:4], scalar2=None, op0=ALU.is_ge), reads=[B_lg, B_mx8], writes=[B_M])
                    P.op("dve", lambda e: e.tensor_copy(out=Mb[:], in_=Mf[:]), reads=[B_M], writes=[B_M])
                    P.op("pe", lambda e: e.matmul(pb[6][:, 0:E], lhsT=ltri_b, rhs=Mb[:], start=True, stop=True), reads=[B_M, B_const], writes=[B_pb[6]])
                    P.op("dve", lambda e: e.tensor_tensor(out=rkf[:], in0=pb[6][:, 0:E], in1=r_run[:], op=ALU.add), reads=[B_pb[6], B_run], writes=[B_rkf])
                    P.op("pe", lambda e: e.matmul(pb[6][:, 0:E], lhsT=ones_t[:], rhs=Mb[:], start=True, stop=True), reads=[B_M, B_const], writes=[B_pb[6]])
                    P.op("dve", lambda e: e.tensor_tensor(out=r_run[:], in0=pb[6][:, 0:E], in1=r_run[:], op=ALU.add), reads=[B_pb[6], B_run], writes=[B_run])
                    for k in range(TOPK):
                        P.op("dve", lambda e, c=c0 + k: e.tensor_scalar(out=oh[:], in0=iota_f[:, 0:E], scalar1=r_idx[:, c:c + 1], scalar2=None, op0=ALU.is_equal),
                             reads=[B_ridx, B_const], writes=[B_oh])
                        P.op("dve", lambda e: e.tensor_tensor(out=oh[:], in0=oh[:], in1=rkf[:], op=ALU.mult), reads=[B_oh, B_rkf], writes=[B_oh])
                        P.op("dve", lambda e, c=c0 + k: e.reduce_sum(out=r_rank[:, c:c + 1], in_=oh[:], axis=AX.X), reads=[B_oh], writes=[B_rrank])

        stop_at(3)
        P.barrier()
        P.phase += 1
        Dm = root.sub()
        cntp = Dm.alloc([128, E], F32)
        mod_ = Dm.alloc([128, E], F32)
        incl = Dm.alloc([128, E], F32)
        base = Dm.alloc([128, E], F32)
        B_d = Buf()
        MT = NTOK // 512
        thr = Dm.alloc([128, MT], F32)
        cmpc = Dm.alloc([128, E, MT], F32)
        P.op("dve", lambda e: e.tensor_scalar(out=thr[:], in0=iota_f[:, 0:MT], scalar1=512.0, scalar2=None, op0=ALU.mult), reads=[B_const], writes=[B_d])
        P.op("dve", lambda e: e.tensor_tensor(out=cmpc[:], in0=r_run[:].unsqueeze(2).to_broadcast([128, E, MT]),
                                              in1=thr[:].unsqueeze(1).to_broadcast([128, E, MT]), op=ALU.is_gt), reads=[B_run, B_d], writes=[B_d])
        P.op("dve", lambda e: e.reduce_sum(out=cntp[:], in_=cmpc[:], axis=AX.X), reads=[B_d], writes=[B_d])
        P.op("dve", lambda e: e.tensor_scalar(out=cntp[:], in0=cntp[:], scalar1=512.0, scalar2=None, op0=ALU.mult), reads=[B_d], writes=[B_d])
        P.op("dve", lambda e: e.tensor_copy(out=incl[:, 0:1], in_=cntp[:, 0:1]), reads=[B_d], writes=[B_d])
        for ei in range(1, E):
            P.op("dve", lambda e, ei=ei: e.tensor_tensor(out=incl[:, ei:ei + 1], in0=incl[:, ei - 1:ei], in1=cntp[:, ei:ei + 1], op=ALU.add), reads=[B_d], writes=[B_d])
        P.op("dve", lambda e: e.tensor_tensor(out=base[:], in0=incl[:], in1=cntp[:], op=ALU.subtract), reads=[B_d], writes=[B_d])
        tef = Dm.alloc([128, NT], F32)
        cmp3 = Dm.alloc([128, NT, E], F32)
        jv = Dm.alloc([128, NT], F32)
        P.op("dve", lambda e: e.tensor_scalar(out=jv[:], in0=iota_f[:, 0:NT], scalar1=512.0, scalar2=None, op0=ALU.mult), reads=[B_const], writes=[B_d])
        P.op("dve", lambda e: e.tensor_tensor(out=cmp3[:], in0=incl[:].unsqueeze(1).to_broadcast([128, NT, E]),
                                              in1=jv[:].unsqueeze(2).to_broadcast([128, NT, E]), op=ALU.is_le), reads=[B_d], writes=[B_d])
        P.op("dve", lambda e: e.reduce_sum(out=tef[:], in_=cmp3[:], axis=AX.X), reads=[B_d], writes=[B_d])
        P.op("dve", lambda e: e.tensor_scalar(out=tef[:], in0=tef[:], scalar1=float(E - 1), scalar2=None, op0=ALU.min), reads=[B_d], writes=[B_d])
        P.op("dve", lambda e: e.tensor_copy(out=te_i[:], in_=tef[0:1, :]), reads=[B_d], writes=[B_te])
        tk = Dm.alloc([128, NT], F32)
        P.op("dve", lambda e: e.scalar_tensor_tensor(out=tk[:], in0=tef[:], scalar=128.0, in1=pcol[:, 0:1].to_broadcast([128, NT]), op0=ALU.mult, op1=ALU.add),
             reads=[B_d, B_const], writes=[B_d])
        P.op("dve", lambda e: e.tensor_copy(out=idx_b1[:], in_=tk[:]), reads=[B_d], writes=[B_te])
        P.op("dve", lambda e: e.tensor_copy(out=idx_b2[:], in_=tef[:]), reads=[B_d], writes=[B_te])
        CH = min(64, NTK)
        oh3 = Dm.alloc([128, CH, E], F32)
        posf = Dm.alloc([128, NTK], F32)
        for c0 in range(0, NTK, CH):
            P.op("dve", lambda e, c0=c0: e.tensor_tensor(out=oh3[:], in0=iota_f[:, 0:E].unsqueeze(1).to_broadcast([128, CH, E]),
                                                         in1=r_idx[:, c0:c0 + CH].unsqueeze(2).to_broadcast([128, CH, E]), op=ALU.is_equal),
                 reads=[B_ridx, B_const], writes=[B_oh])
            P.op("dve", lambda e: e.tensor_tensor(out=oh3[:], in0=oh3[:], in1=base[:].unsqueeze(1).to_broadcast([128, CH, E]), op=ALU.mult),
                 reads=[B_oh, B_d], writes=[B_oh])
            P.op("dve", lambda e, c0=c0: e.reduce_sum(out=posf[:, c0:c0 + CH], in_=oh3[:], axis=AX.X), reads=[B_oh], writes=[B_d])
        P.op("dve", lambda e: e.tensor_tensor(out=posf[:], in0=posf[:], in1=r_rank[:], op=ALU.add), reads=[B_d, B_rrank], writes=[B_d])
        P.op("dve", lambda e: e.tensor_copy(out=r_pos[:], in_=posf[:]), reads=[B_d], writes=[B_rpos])

        hrow = [Dm.alloc([128, D], BF16) for _ in range(3)]
        B_hrow = [Buf() for _ in range(3)]
        for ti in range(NTT):
            s = ti % 3
            dma("sp", hrow[s][:], h2_d[ti * 128:(ti + 1) * 128, :], reads=[B_h2[ti]], writes=[B_hrow[s]])
            for k in range(TOPK):
                c = ti * TOPK + k
                P.op("pool", lambda e, s=s, c=c: e.indirect_dma_start(
                    out=hs_d[:, :], out_offset=bass.IndirectOffsetOnAxis(ap=r_pos[:, c:c + 1], axis=0), in_=hrow[s][:], in_offset=None),
                    reads=[B_hrow[s], B_rpos], writes=[B_hs[c]], dma=True)

        stop_at(4)
        P.barrier()
        P.phase += 1
        Em = root.sub()
        w1 = [Em.alloc([128, 8, 2 * D], BF16) for _ in range(2)]
        w2 = Em.alloc([128, 8, D], BF16)
        NSTG = 5
        stg = [Em.alloc([128, 2048], F32) for _ in range(NSTG)]
        B_stg = [Buf() for _ in range(NSTG)]
        B_w1c = [[Buf() for _ in range(8)] for _ in range(2)]
        B_w2c = [Buf() for _ in range(4)]
        b1 = [Em.alloc([128, 16], F32) for _ in range(2)]
        b2 = [Em.alloc([128, D], F32) for _ in range(2)]
        B_b = [Buf(), Buf()]
        hs = [Em.alloc([128, 4, D], BF16) for _ in range(2)]
        B_hsb = [Buf(), Buf()]
        hsT = [Em.alloc([128, 8, 512], BF16) for _ in range(2)]
        B_hsT = [Buf(), Buf()]
        actT = Em.alloc([128, 8, 512], BF16)
        B_actT = Buf()
        glu = [Em.alloc([128, 512], F32) for _ in range(2)]
        sg = [Em.alloc([128, 512], F32) for _ in range(2)]
        lin = [Em.alloc([128, 512], F32) for _ in range(2)]
        B_glu, B_sg, B_lin = [Buf(), Buf()], [Buf(), Buf()], [Buf(), Buf()]
        yo = [Em.alloc([128, D], F32) for _ in range(2)]
        B_yo = [Buf(), Buf()]
        ec = dict(u=0, y=0, g=0)

        be1_rows = be1_d.rearrange("e p c -> (e p) c")

        sc_ = dict(n=0)

        def load_b(j):
            s = j % 2
            def ld(e_, s=s, j=j):
                r = []
                r.append(e_.indirect_dma_start(out=b1[s][:], out_offset=None, in_=be1_rows,
                                               in_offset=bass.IndirectOffsetOnAxis(ap=idx_b1[:, j:j + 1], axis=0)))
                r.append(e_.indirect_dma_start(out=b2[s][:], out_offset=None, in_=be2_d,
                                               in_offset=bass.IndirectOffsetOnAxis(ap=idx_b2[:, j:j + 1], axis=0)))
                return r
            P.op("pool", ld, reads=[B_te], writes=[B_b[s]], dma=True, ndma=2)

        def load_w1(j, qs=range(8)):
            for q in qs:
                r_ = sc_["n"] % NSTG
                sc_["n"] += 1
                P.op("pool", lambda e_, q=q, r_=r_, j=j: e_.indirect_dma_start(
                    out=stg[r_][:], out_offset=None, in_=we1_d[q], in_offset=bass.IndirectOffsetOnAxis(ap=idx_b1[:, j:j + 1], axis=0)),
                    reads=[B_te], writes=[B_stg[r_]], dma=True)
                P.op("act", lambda e, q=q, r_=r_, j=j: e.activation(out=w1[j % 2][:, q, :], in_=stg[r_][:], func=AF.Copy),
                     reads=[B_stg[r_]], writes=[B_w1c[j % 2][q]])

        def load_w2(j):
            for q in range(4):
                r_ = sc_["n"] % NSTG
                sc_["n"] += 1
                P.op("pool", lambda e_, q=q, r_=r_, j=j: e_.indirect_dma_start(
                    out=stg[r_][:], out_offset=None, in_=we2_d[q], in_offset=bass.IndirectOffsetOnAxis(ap=idx_b1[:, j:j + 1], axis=0)),
                    reads=[B_te], writes=[B_stg[r_]], dma=True)
                P.op("act", lambda e, q=q, r_=r_: e.activation(out=w2[:, 2 * q:2 * q + 2, :].rearrange("p k n -> p (k n)"), in_=stg[r_][:], func=AF.Copy),
                     reads=[B_stg[r_]], writes=[B_w2c[q]])

        def load_h(j):
            s = j % 2
            dma("sp", hs[s][:], hs_d[j * 512:(j + 1) * 512, :].rearrange("(s p) d -> p s d", p=128), reads=B_hs, writes=[B_hsb[s]])

        def moe_transposes(j):
            s = j % 2
            for st in range(4):
                def tr(e, st=st, s=s):
                    r = None
                    for kc in range(8):
                        r = e.transpose(pbt[:, kc * 128:(kc + 1) * 128], hs[s][:, st, kc * 128:(kc + 1) * 128], ident_b)
                    return r
                P.op("pe", tr, reads=[B_hsb[s], B_const], writes=[B_pbt])
                if st % 2 == 0:
                    P.op("act", lambda e, st=st, s=s: e.activation(out=hsT[s][:, :, st * 128:(st + 1) * 128], in_=pbt[:].rearrange("p (k t) -> p k t", k=8), func=AF.Copy),
                         reads=[B_pbt], writes=[B_hsT[s]])
                else:
                    P.op("dve", lambda e, st=st, s=s: e.tensor_copy(out=hsT[s][:, :, st * 128:(st + 1) * 128], in_=pbt[:].rearrange("p (k t) -> p k t", k=8)),
                         reads=[B_pbt], writes=[B_hsT[s]])

        load_b(0)
        load_w1(0)
        load_w2(0)
        load_h(0)
        moe_transposes(0)
        for j in range(NT):
            s = j % 2
            if j + 1 < NT:
                load_b(j + 1)
                load_h(j + 1)
            for c in range(8):
                bks = []
                for half in range(2):
                    bk = ec["u"] % 4
                    ec["u"] += 1
                    col = half * D + c * 128
                    def mm(e, s=s, col=col, bk=bk):
                        r = None
                        for kc in range(8):
                            r = e.matmul(pb[bk][:], lhsT=w1[s][:, kc, col:col + 128], rhs=hsT[s][:, kc, :], start=(kc == 0), stop=(kc == 7))
                        return r
                    P.op("pe", mm, reads=B_w1c[s] + [B_hsT[s]], writes=[B_pb[bk]])
                    bks.append(bk)
                gi = ec["g"] % 2
                ec["g"] += 1
                P.op("dve", lambda e, s=s, c=c, gi=gi, bk=bks[0]: e.tensor_scalar(out=glu[gi][:], in0=pb[bk][:], scalar1=b1[s][:, c:c + 1], scalar2=7.0, op0=ALU.add, op1=ALU.min),
                     reads=[B_pb[bks[0]], B_b[s]], writes=[B_glu[gi]])
                P.op("act", lambda e, gi=gi: e.activation(out=sg[gi][:], in_=glu[gi][:], func=AF.Sigmoid, scale=1.702), reads=[B_glu[gi]], writes=[B_sg[gi]])
                P.op("dve", lambda e, s=s, c=c, gi=gi, bk=bks[1]: e.tensor_scalar(out=lin[gi][:], in0=pb[bk][:], scalar1=b1[s][:, 8 + c:9 + c], scalar2=7.0, op0=ALU.add, op1=ALU.min),
                     reads=[B_pb[bks[1]], B_b[s]], writes=[B_lin[gi]])
                P.op("dve", lambda e, gi=gi: e.tensor_scalar(out=lin[gi][:], in0=lin[gi][:], scalar1=-7.0, scalar2=1.0, op0=ALU.max, op1=ALU.add),
                     reads=[B_lin[gi]], writes=[B_lin[gi]])
                P.op("dve", lambda e, gi=gi: e.tensor_tensor(out=glu[gi][:], in0=glu[gi][:], in1=sg[gi][:], op=ALU.mult),
                     reads=[B_glu[gi], B_sg[gi]], writes=[B_glu[gi]])
                P.op("dve", lambda e, gi=gi, c=c: e.tensor_tensor(out=actT[:, c, :], in0=glu[gi][:], in1=lin[gi][:], op=ALU.mult),
                     reads=[B_glu[gi], B_lin[gi]], writes=[B_actT])
                if j + 1 < NT:
                    load_w1(j + 1, [c])
            if j + 1 < NT:
                moe_transposes(j + 1)
            for st in range(4):
                yi = ec["y"] % 2
                ec["y"] += 1
                for nh in range(2):
                    bk = 4 + nh
                    def mm(e, s=s, st=st, nh=nh, bk=bk):
                        r = None
                        for c in range(8):
                            r = e.matmul(pb[bk][:], lhsT=actT[:, c, st * 128:(st + 1) * 128], rhs=w2[:, c, nh * 512:(nh + 1) * 512], start=(c == 0), stop=(c == 7))
                        return r
                    P.op("pe", mm, reads=B_w2c + [B_actT], writes=[B_pb[bk]])
                    P.op("dve", lambda e, s=s, nh=nh, bk=bk, yi=yi: e.tensor_tensor(out=yo[yi][:, nh * 512:(nh + 1) * 512], in0=pb[bk][:], in1=b2[s][:, nh * 512:(nh + 1) * 512], op=ALU.add),
                         reads=[B_pb[bk], B_b[s]], writes=[B_yo[yi]])
                r0 = j * 512 + st * 128
                dma("sp", ys_d[r0:r0 + 128, :], yo[yi][:], reads=[B_yo[yi]], writes=[B_ys[j]])
            if j + 1 < NT:
                load_w2(j + 1)

        stop_at(5)
        P.barrier()
        P.phase += 1
        Fm = root.sub()
        g2bc = [Fm.alloc([128, D], F32) for _ in range(NB)]
        fgbc = Fm.alloc([128, D], F32)
        B_f = Buf()
        for b in range(NB):
            bc_load("sp", g2bc[b][:], modrows_d[b, 5 * D:6 * D], reads=[B_modrows], writes=[B_f])
        bc_load("sp", fgbc[:], fg_d, writes=[B_f])
        yk = [[Fm.alloc([128, D], F32) for _ in range(TOPK)] for _ in range(2)]
        B_yk = [[Buf() for _ in range(TOPK)] for _ in range(2)]
        x1f = [Fm.alloc([128, D], F32) for _ in range(2)]
        B_x1f = [Buf(), Buf()]
        acc = Fm.alloc([128, D], F32)
        B_acc = Buf()
        jf = Fm.alloc([128, D], F32)
        B_jf = Buf()
        ssf = Fm.alloc([128, 1], F32)
        B_ssf = Buf()
        of = [Fm.alloc([128, D], F32) for _ in range(2)]
        B_of = [Buf(), Buf()]
        for ti in range(NTT):
            s = ti % 2
            b = (ti * 128) // T
            for k in range(TOPK):
                c = ti * TOPK + k
                P.op("pool", lambda e, s=s, k=k, c=c: e.indirect_dma_start(
                    out=yk[s][k][:], out_offset=None, in_=ys_d[:, :], in_offset=bass.IndirectOffsetOnAxis(ap=r_pos[:, c:c + 1], axis=0)),
                    reads=[B_rpos] + B_ys, writes=[B_yk[s][k]], dma=True)
            dma("sp", x1f[s][:], x1_d[ti * 128:(ti + 1) * 128, :], reads=[B_x1[ti]], writes=[B_x1f[s]])
            for k in range(TOPK):
                c = ti * TOPK + k
                if k == 0:
                    P.op("dve", lambda e, s=s, c=c: e.tensor_scalar(out=acc[:], in0=yk[s][0][:], scalar1=r_w[:, c:c + 1], scalar2=None, op0=ALU.mult),
                         reads=[B_yk[s][0], B_rw], writes=[B_acc])
                else:
                    P.op("dve", lambda e, s=s, k=k, c=c: e.scalar_tensor_tensor(out=acc[:], in0=yk[s][k][:], scalar=r_w[:, c:c + 1], in1=acc[:], op0=ALU.mult, op1=ALU.add),
                         reads=[B_yk[s][k], B_rw, B_acc], writes=[B_acc])
            P.op("dve", lambda e, b=b: e.tensor_tensor(out=acc[:], in0=acc[:], in1=g2bc[b][:], op=ALU.mult), reads=[B_acc, B_f], writes=[B_acc])
            P.op("dve", lambda e, s=s: e.tensor_tensor(out=acc[:], in0=acc[:], in1=x1f[s][:], op=ALU.add), reads=[B_acc, B_x1f[s]], writes=[B_acc])
            P.op("act", lambda e: e.activation(out=jf[:], in_=acc[:], func=AF.Square, accum_out=ssf[:]), reads=[B_acc], writes=[B_jf, B_ssf])
            rstd_from_ss(ssf[:], ssf[:], float(D), [B_ssf], B_ssf)
            P.op("dve", lambda e, s=s: e.scalar_tensor_tensor(out=of[s][:], in0=acc[:], scalar=ssf[:, 0:1], in1=fgbc[:], op0=ALU.mult, op1=ALU.mult),
                 reads=[B_acc, B_ssf, B_f], writes=[B_of[s]])
            P.op("sp", lambda e, ti=ti, s=s: e.dma_start(out=out_d[ti * 128:(ti + 1) * 128, :], in_=of[s][:]), reads=[B_of[s]], writes=[Buf()], dma=True, out=True)
    except _Stop:
        pass
    ops = P.ops
    for o in ops:
        for d in o["deps"]:
            ops[d]["sig"] = True
    for o in ops:
        if o["out"] or o["dma"]:
            o["sig"] = True
    nphase = P.phase + 1
    with contextlib.ExitStack() as es:
        engsem = {}
        for ph in range(nphase):
            for en in ENGS[:4]:
                engsem[(ph, en)] = es.enter_context(nc.semaphore("s_%s_%d" % (en, ph)))
        NDS = 8
        dmasem = {q: [es.enter_context(nc.semaphore("d_%s_%d" % (q, i))) for i in range(NDS)] for q in ("sp", "pool", "act")}
        seq = {}
        dcount = {q: [0] * NDS for q in dmasem}
        drr = {q: 0 for q in dmasem}
        for i, o in enumerate(ops):
            if not o["sig"]:
                continue
            if o["dma"]:
                q = o["eng"]
                k = drr[q] % NDS
                drr[q] += 1
                o["sem"] = dmasem[q][k]
                o["semk"] = (q, k)
            else:
                key = (o["phase"], o["eng"])
                seq[key] = seq.get(key, 0) + 1
                o["sem"] = engsem[key]
                o["val"] = seq[key]
        block = es.enter_context(nc.Block())

        def emit(en, e):
            waited = {}
            for i in P.byeng[en]:
                o = ops[i]
                need = {}
                for d in o["deps"]:
                    od = ops[d]
                    if en == "pe" and od["eng"] == "pe" and not od["dma"]:
                        continue
                    sem, val = od["sem"], od["val"]
                    key = id(sem)
                    if key not in need or need[key][1] < val:
                        need[key] = (sem, val)
                for key, (sem, val) in need.items():
                    if waited.get(key, 0) < val:
                        e.wait_ge(sem, val)
                        waited[key] = val
                if o["fn"] is None:
                    continue
                r = o["fn"](e)
                if o["sig"]:
                    if o["dma"]:
                        rl = r if isinstance(r, list) else [r]
                        q, k = o["semk"]
                        for ins in rl:
                            ins.then_inc(o["sem"], 16)
                    else:
                        r.then_inc(o["sem"], 1)
            if en == "sp":
                for k in range(NDS):
                    if dcount["sp"][k] > 0:
                        e.wait_ge(dmasem["sp"][k], dcount["sp"][k])

        for q in dmasem:
            for i in P.byeng[q]:
                o = ops[i]
                if o["dma"] and o["sig"]:
                    qq, k = o["semk"]
                    n = o["ndma"]
                    dcount[qq][k] += 16 * n
                    o["val"] = dcount[qq][k]

        @block.tensor
        def _(e):
            emit("pe", e)

        @block.scalar
        def _(e):
            emit("act", e)

        @block.vector
        def _(e):
            emit("dve", e)

        @block.gpsimd
        def _(e):
            emit("pool", e)

        @block.sync
        def _(e):
            emit("sp", e)
    return nc


def _rope_tables(T):
    half = 32
    inv = (10000.0 ** (-(np.arange(half // 2, dtype=np.float32) * 2.0 / half))).astype(np.float32)
    t = np.arange(T)
    row = (t // GRID_W).astype(np.float32)
    col = (t % GRID_W).astype(np.float32)
    C = np.zeros((128, T), np.float32)
    S = np.zeros((128, T), np.float32)
    for p in range(128):
        d = p % 64
        pos = row if d < 32 else col
        j = d % 16
        ang = (pos * inv[j]).astype(np.float32)
        C[p] = np.cos(ang)
        sgn = -1.0 if (d % 32) < 16 else 1.0
        S[p] = sgn * np.sin(ang)
    return C, S


def _consts():
    cm = np.zeros((128, 4, 128), np.float32)
    cm[:, 0, :] = np.eye(128)
    for m in range(128):
        d = m % 32
        k = m + 16 if d < 16 else m - 16
        cm[k, 1, m] = 1.0
    for k in range(128):
        for m in range(128):
            if k // 64 == m // 64:
                cm[k, 2, m] = 1.0
            if k < m:
                cm[k, 3, m] = 1.0
    mb = np.zeros((128, 2, 4, 128), np.float32)
    kj = np.arange(128)[:, None]
    qi = np.arange(128)[None, :]
    lo = np.where(kj >= qi, 0.0, MASKV).astype(np.float32)
    hi = np.where(kj <= qi, 0.0, MASKV).astype(np.float32)
    mb[:, 0, :, :] = lo[:, None, :]
    mb[:, 1, :, :] = hi[:, None, :]
    iot = np.tile(np.arange(128, dtype=np.float32)[None, :], (128, 1))
    return cm, mb.reshape(128, 2, 512), iot


def _prep_shared(cfg, inp):
    E = cfg["E"]
    f = lambda a: np.ascontiguousarray(a, dtype=np.float32)
    w_in = inp["w_in"][0]
    KVW = 128
    k_a, v_a, k_b, v_b = (w_in[:, i * KVW:(i + 1) * KVW] for i in range(4))
    q_a = w_in[:, 512:1024]
    q_b = w_in[:, 1024:1536]
    g_a = w_in[:, 1536:2560]
    g_b = w_in[:, 2560:3584]

    def pairs(q):
        cols = []
        for j in range(4):
            cols.append(q[:, j * 64:(j + 1) * 64])
            cols.append(q[:, (j + 4) * 64:(j + 5) * 64])
        return np.concatenate(cols, axis=1)

    w_in_p = np.concatenate([k_a, k_b, pairs(q_a), pairs(q_b), v_a, v_b, g_a, g_b], axis=1)

    def brp(w):
        w = w[0]
        return np.stack([np.concatenate([w[j * 64:(j + 1) * 64], w[(j + 4) * 64:(j + 5) * 64]], axis=0) for j in range(4)], axis=0)

    sink = inp["sink"][0]
    sinkT = np.zeros((128, 4), np.float32)
    sinkT[0:64, :] = sink[4:8][None, :]
    sinkT[64:128, :] = sink[0:4][None, :]
    we1 = inp["w_e1"][0]
    we1p = np.concatenate([we1[:, :, 0::2], we1[:, :, 1::2]], axis=2)
    be1 = inp["b_e1"][0]
    be1p = np.concatenate([be1[:, 0::2], be1[:, 1::2]], axis=1)
    be1T = be1p.reshape(E, 16, 128).transpose(0, 2, 1)
    cm, mb, iot = _consts()
    C, S = _rope_tables(cfg["T"])
    return {
        "w_mod": f(inp["w_mod"][0]), "b_mod": f(inp["b_mod"][0]),
        "norm1_g": f(inp["norm1_g"][0]), "norm2_g": f(inp["norm2_g"][0]), "final_g": f(inp["final_g"]),
        "w_in": f(w_in_p),
        "gq2": f(np.tile(inp["q_norm_g"][0], 2).reshape(128, 1)),
        "gk2": f(np.tile(inp["k_norm_g"][0], 2).reshape(128, 1)),
        "sinkT": f(sinkT),
        "w_br_a": f(brp(inp["w_br_a"])), "w_br_b": f(brp(inp["w_br_b"])),
        "w_o": f(inp["w_o"][0]), "w_router": f(inp["w_router"][0]), "b_router": f(inp["b_router"][0]),
        "b_e1T": f(be1T), "b_e2": f(inp["b_e2"][0]),
        **{"w_e1_%d" % q: f(we1p.reshape(E, 8, 128, 2 * D)[:, q, :, :].reshape(E * 128, 2048)) for q in range(8)},
        **{"w_e2_%d" % q: f(inp["w_e2"][0].reshape(E, 8, 128, D).transpose(0, 2, 1, 3)[:, :, 2 * q:2 * q + 2, :].reshape(E * 128, 2048)) for q in range(4)},
        "ropeC": f(C), "ropeS": f(S), "cmats": f(cm), "maskb": f(mb), "iotas": f(iot),
        "pcol": f(np.arange(128, dtype=np.float32)[:, None] + 128.0 * np.arange(8, dtype=np.float32)[None, :]),
    }


def run(cfg, inp, trace=False):
    NCORE, NB, T = cfg["NCORE"], cfg["NB"], cfg["T"]
    nc = build(cfg)
    shared = _prep_shared(cfg, inp)
    in_maps = []
    for c in range(NCORE):
        bs = slice(c * NB, (c + 1) * NB)
        m = dict(shared)
        m["x"] = np.ascontiguousarray(inp["x"][bs].reshape(NB * T, D), dtype=np.float32)
        m["ctx"] = np.ascontiguousarray(inp["ctx"][bs].reshape(NB * CTX, D), dtype=np.float32)
        cv = np.concatenate([inp["c"][bs], inp["c_ctx"][None, :]], axis=0)
        m["cT"] = np.ascontiguousarray(cv.reshape(NB + 1, 8, 128).transpose(2, 1, 0), dtype=np.float32)
        in_maps.append(m)
    res = run_bass_kernel_spmd(nc, in_maps, core_ids=list(range(NCORE)), trace=trace)
    out = np.concatenate([r["out"].reshape(NB, T, D) for r in res.results], axis=0)
    return out.astype(np.float32), res


def kernel(**inputs):
    inp = {k: np.asarray(v) for k, v in inputs.items()}
    out, _ = run(CFG, inp)
    return out
```

```python
import contextlib
import numpy as np
import ml_dtypes
import concourse.bass as bass
import concourse.mybir as mybir
from concourse.bass_utils import run_bass_kernel_spmd

F32 = mybir.dt.float32
BF16 = mybir.dt.bfloat16
I32 = mybir.dt.int32
U32 = mybir.dt.uint32
AF = mybir.ActivationFunctionType
ALU = mybir.AluOpType
AX = mybir.AxisListType

D = 1024
CTX = 256
GRID_W = 64
TOPK = 4
EPS = 1e-6
MASKV = -30000.0

CFG = dict(NCORE=8, NB=2, T=4096, E=32)

ENGS = ["pe", "act", "dve", "pool", "sp"]


class Buf:
    __slots__ = ("w", "r", "rd")

    def __init__(self):
        self.w = None
        self.r = {}
        self.rd = []


class Prog:
    def __init__(self):
        self.ops = []
        self.byeng = {e: [] for e in ENGS}
        self.phase = 0
        self.pending_dma = []

    def barrier(self):
        deps = set(self.pending_dma)
        for en in ENGS:
            for i in reversed(self.byeng[en]):
                if self.ops[i]["fn"] is not None:
                    deps.add(i)
                    break
        for en in ENGS:
            idx = len(self.ops)
            self.ops.append(dict(eng=en, fn=None, deps=set(deps), dma=False, sig=False, phase=self.phase, ndma=1, out=False))
            self.byeng[en].append(idx)
        self.pending_dma = []

    def op(self, eng, fn, reads=(), writes=(), dma=False, ndma=1, out=False):
        idx = len(self.ops)
        if dma:
            self.pending_dma.append(idx)
        deps = set()
        for b in reads:
            if b.w is not None:
                deps.add(b.w)
        for b in writes:
            if b.w is not None:
                deps.add(b.w)
            deps.update(b.r.values())
            deps.update(b.rd)
        for b in reads:
            if dma:
                b.rd.append(idx)
            else:
                b.r[eng] = idx
        for b in writes:
            b.w = idx
            b.r = {}
            b.rd = []
        deps.discard(idx)
        self.ops.append(dict(eng=eng, fn=fn, deps=deps, dma=dma, sig=False, phase=self.phase, ndma=ndma, out=out))
        self.byeng[eng].append(idx)
        return idx


def build(cfg):
    NB, T, E = cfg["NB"], cfg["T"], cfg["E"]
    NG = T // 512
    NQB = T // 128
    NKT = NQB + 2
    NTOK = NB * T
    NTT = NTOK // 128
    NT = (NTOK * TOPK) // 512 + E
    NSLOT = NT * 512

    nc = bass.Bass("TRN2", target_bir_lowering=False)
    P = Prog()

    def din(name, shape, dt=F32):
        return nc.dram_tensor(name, list(shape), dt, kind="ExternalInput").ap()

    def dscr(name, shape, dt):
        return nc.dram_tensor(name, list(shape), dt).ap()

    x_d = din("x", [NTOK, D])
    ctx_d = din("ctx", [NB * CTX, D])
    cT_d = din("cT", [128, 8, NB + 1])
    wmod_d = din("w_mod", [D, 6 * D])
    bmod_d = din("b_mod", [6 * D])
    n1g_d = din("norm1_g", [D])
    n2g_d = din("norm2_g", [D])
    fg_d = din("final_g", [D])
    win_d = din("w_in", [D, 3584])
    gq_d = din("gq2", [128, 1])
    gk_d = din("gk2", [128, 1])
    sink_d = din("sinkT", [128, 4])
    wbra_d = din("w_br_a", [4, 128, D])
    wbrb_d = din("w_br_b", [4, 128, D])
    wo_d = din("w_o", [D, D])
    wr_d = din("w_router", [D, E])
    br_d = din("b_router", [E])
    we1_d = [din("w_e1_%d" % q, [E * 128, 2048]) for q in range(8)]
    be1_d = din("b_e1T", [E, 128, 16])
    we2_d = [din("w_e2_%d" % q, [E * 128, 2048]) for q in range(4)]
    be2_d = din("b_e2", [E, D])
    ropeC_d = din("ropeC", [128, T])
    ropeS_d = din("ropeS", [128, T])
    cm_d = din("cmats", [128, 4, 128])
    mb_d = din("maskb", [128, 2, 512])
    iota_d = din("iotas", [128, 128])
    pcol_d = din("pcol", [128, 8])
    out_d = nc.dram_tensor("out", [NTOK, D], F32, kind="ExternalOutput").ap()

    modrows_d = dscr("modrows", [NB + 1, 6 * D], F32)
    qt_d = dscr("qt_scr", [2, 128, 4, NTOK], BF16)
    g_d = dscr("g_scr", [16, 128, NTOK], BF16)
    x1_d = dscr("x1_scr", [NTOK, D], F32)
    h2_d = dscr("h2_scr", [NTOK, D], BF16)
    hs_d = dscr("hs_scr", [NSLOT, D], BF16)
    ys_d = dscr("ys_scr", [NSLOT, D], F32)

    B_modrows = Buf()
    B_qt = [[Buf() for _ in range(NG)] for _ in range(NB)]
    B_g = [[Buf() for _ in range(NG)] for _ in range(NB)]
    B_x1 = [Buf() for _ in range(NTT)]
    B_h2 = [Buf() for _ in range(NTT)]
    B_hs = [Buf() for _ in range(NTT * TOPK)]
    B_ys = [Buf() for _ in range(NT)]

    SB_LO, SB_HI = 16896, 229376
    cnt = [0]

    class Arena:
        def __init__(self, lo, hi):
            self.lo, self.hi, self.cur = lo, hi, lo

        def alloc(self, shape, dt, nbuf=None):
            cnt[0] += 1
            esz = 4 if dt in (F32, I32, U32) else 2
            n = esz
            for s in shape[1:]:
                n *= s
            n = (n + 63) // 64 * 64
            assert self.cur + n <= self.hi, ("SBUF overflow", shape, self.cur, self.hi)
            t = nc.alloc_sbuf_tensor_at("sb%d" % cnt[0], list(shape), dt, offset=self.cur)
            self.cur += n
            return t

        def sub(self):
            return Arena(self.cur, self.hi)

    root = Arena(SB_LO, SB_HI)

    cm_f = root.alloc([128, 4, 128], F32)
    cm_b = root.alloc([128, 4, 128], BF16)
    mb_b = root.alloc([128, 2, 512], BF16)
    iota_f = root.alloc([128, 128], F32)
    B_const = Buf()
    ident_f, ident_b = cm_f[:, 0, :], cm_b[:, 0, :]
    swap_b, bones_b, ltri_b = cm_b[:, 1, :], cm_b[:, 2, :], cm_b[:, 3, :]
    ones_b = None

    ones_t = root.alloc([128, 128], BF16)
    NTK = NTT * TOPK
    r_idx = root.alloc([128, NTK], F32)
    r_rank = root.alloc([128, NTK], F32)
    r_w = root.alloc([128, NTK], F32)
    r_pos = root.alloc([128, NTK], I32)
    r_run = root.alloc([128, E], F32)
    te_i = root.alloc([1, NT], I32)
    idx_b1 = root.alloc([128, NT], I32)
    idx_b2 = root.alloc([128, NT], I32)
    pcol = root.alloc([128, 8], F32)
    epsc = root.alloc([128, 2], F32)
    B_ridx, B_rrank, B_rw, B_rpos, B_run, B_te = Buf(), Buf(), Buf(), Buf(), Buf(), Buf()

    class _V:
        def __init__(self, ap):
            self.ap = ap

        def __getitem__(self, k):
            return self.ap[k]

    PQ = [nc.alloc_psum_tensor("pq%d" % i, [128, 1024], F32) for i in range(4)]
    pb = [_V(PQ[i // 2][:, (i % 2) * 512:(i % 2 + 1) * 512]) for i in range(8)]
    pbt = _V(PQ[3][:, 512:1024].bitcast(BF16))
    B_pb = [Buf() for _ in range(8)]
    B_pbt = B_pb[7]

    def dma(q, out, in_, reads=(), writes=(), **kw):
        return P.op(q, lambda e: e.dma_start(out=out, in_=in_, **kw), reads=reads, writes=writes, dma=True)

    def bc_load(q, dst, src_row, writes, reads=()):
        return dma(q, dst, src_row.partition_broadcast(128), reads=reads, writes=writes)

    class _Stop(Exception):
        pass

    def stop_at(n):
        if cfg.get("STOP", 99) == n:
            raise _Stop()

    try:
        dma("sp", cm_f[:], cm_d[:, :, :], writes=[B_const])
        dma("sp", iota_f[:], iota_d[:, :], writes=[B_const])
        dma("sp", pcol[:], pcol_d[:, :], writes=[B_const])
        P.op("dve", lambda e: e.tensor_copy(out=cm_b[:], in_=cm_f[:]), reads=[B_const], writes=[B_const])
        P.op("dve", lambda e: e.memset(ones_t[:], 1.0), writes=[B_const])
        P.op("dve", lambda e: e.memset(epsc[:, 0:1], EPS), writes=[B_const])
        P.op("dve", lambda e: e.memset(epsc[:, 1:2], 64.0 * EPS), writes=[B_const])
        P.op("dve", lambda e: e.memset(r_run[:], 0.0), writes=[B_run])

        ATT = root.sub()
        KT = [ATT.alloc([128, NKT * 128], BF16) for _ in range(2)]
        VA = ATT.alloc([128, NKT, 2, 2, 128], BF16)
        B_KT = [[Buf() for _ in range(NG + 1)] for _ in range(2)]
        B_VA = [Buf() for _ in range(NG + 1)]
        gq = ATT.alloc([128, 1], F32)
        gk = ATT.alloc([128, 1], F32)
        sinkT = ATT.alloc([128, 4], F32)
        sinkE = ATT.alloc([128, 4], F32)
        B_small = Buf()
        dma("sp", gq[:], gq_d[:, :], writes=[B_small])
        dma("sp", gk[:], gk_d[:, :], writes=[B_small])
        dma("sp", sinkT[:], sink_d[:, :], writes=[B_small])
        P.op("act", lambda e: e.activation(out=sinkE[:], in_=sinkT[:], func=AF.Exp), reads=[B_small], writes=[B_small])
        P.op("pool", lambda e: e.memset(VA[:, :, :, 0, 64:128], 1.0), writes=B_VA)
        P.op("pool", lambda e: e.memset(VA[:, :, :, 1, 0:64], 1.0), writes=B_VA)

        PH = ATT.sub()
        A = PH.sub()
        mb_f = A.alloc([128, 2, 512], F32)
        B_mbf = Buf()
        dma("sp", mb_f[:], mb_d[:, :, :], writes=[B_mbf])
        P.op("dve", lambda e: e.tensor_copy(out=mb_b[:], in_=mb_f[:]), reads=[B_mbf], writes=[B_const])
        cT = A.alloc([128, 8, NB + 1], F32)
        scb = A.alloc([128, NB + 1, 8, 128], BF16)
        sc1 = A.alloc([128, 8, NB + 1], BF16)
        bmod_bc = A.alloc([128, 6 * D], F32)
        wm = [A.alloc([128, 8, 512], BF16) for _ in range(2)]
        mrow = [A.alloc([128, 512], F32) for _ in range(2)]
        B_cT, B_scb, B_bmod = Buf(), Buf(), Buf()
        B_wm = [Buf(), Buf()]
        B_mrow = [Buf(), Buf()]
        dma("sp", cT[:], cT_d[:, :, :], writes=[B_cT])
        bc_load("sp", bmod_bc[:], bmod_d, writes=[B_bmod])
        P.op("act", lambda e: e.activation(out=sc1[:], in_=cT[:], func=AF.Silu), reads=[B_cT], writes=[B_cT])
        for v in range(NB + 1):
            for kc in range(8):
                P.op("dve", lambda e, v=v, kc=kc: e.tensor_copy(
                    out=scb[:, v, kc, :], in_=sc1[:, kc, v:v + 1].to_broadcast([128, 128])),
                    reads=[B_cT], writes=[B_scb])
        wmod_v = wmod_d.rearrange("(kc p) n -> p kc n", p=128)
        nmr = 0
        for cc in range(12):
            s = cc % 2
            dma("pool", wm[s][:], wmod_v[:, :, cc * 512:(cc + 1) * 512], writes=[B_wm[s]])
            for v in range(NB + 1):
                bk = (cc * (NB + 1) + v) % 4
                def mm(e, v=v, s=s, bk=bk):
                    r = None
                    for kc in range(8):
                        r = e.matmul(pb[bk][:], lhsT=scb[:, v, kc, :], rhs=wm[s][:, kc, :], start=(kc == 0), stop=(kc == 7))
                    return r
                P.op("pe", mm, reads=[B_scb, B_wm[s]], writes=[B_pb[bk]])
                ms = nmr % 2
                nmr += 1
                P.op("dve", lambda e, bk=bk, ms=ms, cc=cc: e.tensor_tensor(
                    out=mrow[ms][:], in0=pb[bk][:], in1=bmod_bc[:, cc * 512:(cc + 1) * 512], op=ALU.add),
                    reads=[B_pb[bk], B_bmod], writes=[B_mrow[ms]])
                dma("sp", modrows_d[v:v + 1, cc * 512:(cc + 1) * 512], mrow[ms][0:1, :], reads=[B_mrow[ms]], writes=[B_modrows])

        P.barrier()

        def rstd_from_ss(ss, out, n, dep_bufs, wbuf):
            P.op("act", lambda e: e.activation(out=out, in_=ss, func=AF.Sqrt, bias=epsc[:, 0:1], scale=1.0 / n),
                 reads=list(dep_bufs) + [B_const], writes=[wbuf])
            P.op("dve", lambda e: e.reciprocal(out=out, in_=out), reads=[wbuf], writes=[wbuf])

        stop_at(1)
        for b in range(NB):
            if b > 0:
                P.barrier()
            P.phase += 1
            Bm = PH.sub()
            win = Bm.alloc([128, 8, 3584], BF16)
            B_win = Buf()
            win_v = win_d.rearrange("(kc p) n -> p kc n", p=128)
            for (c0, c1) in ((0, 1536), (1536, 3584)):
                dma("pool", win[:, :, c0:c1], win_v[:, :, c0:c1], writes=[B_win])
            A1 = Bm.alloc([128, D], F32)
            B1 = Bm.alloc([128, D], F32)
            A1c = Bm.alloc([128, D], F32)
            B1c = Bm.alloc([128, D], F32)
            B_ab = Buf()
            tmpg = Bm.alloc([128, D], F32)
            bc_load("sp", tmpg[:], n1g_d, writes=[B_ab])
            for (v, Ad, Bd) in ((b, A1, B1), (NB, A1c, B1c)):
                bc_load("sp", Ad[:], modrows_d[v, D:2 * D], reads=[B_modrows], writes=[B_ab])
                bc_load("sp", Bd[:], modrows_d[v, 0:D], reads=[B_modrows], writes=[B_ab])
                P.op("dve", lambda e, Ad=Ad: e.scalar_tensor_tensor(out=Ad[:], in0=Ad[:], scalar=1.0, in1=tmpg[:], op0=ALU.add, op1=ALU.mult),
                     reads=[B_ab], writes=[B_ab])
            xt = [Bm.alloc([128, D], F32) for _ in range(2)]
            B_xt = [Buf(), Buf()]
            junk = Bm.alloc([128, D], F32)
            B_junk = Buf()
            ssv = [Bm.alloc([128, 1], F32) for _ in range(2)]
            B_ss = [Buf(), Buf()]
            hf = Bm.alloc([128, D], F32)
            B_hf = Buf()
            hb = [Bm.alloc([128, D], BF16) for _ in range(2)]
            B_hb = [Buf(), Buf()]
            hT = [Bm.alloc([128, 8, 512], BF16) for _ in range(2)]
            B_hT = [Buf(), Buf()]
            rC = [Bm.alloc([128, 512], F32) for _ in range(2)]
            rS = [Bm.alloc([128, 512], F32) for _ in range(2)]
            B_rope = [Buf(), Buf()]
            xg = [Bm.alloc([128, 512], BF16) for _ in range(2)]
            sq = [Bm.alloc([128, 512], BF16) for _ in range(2)]
            B_xg = [Buf(), Buf()]
            B_sq = [Buf(), Buf()]
            t1 = [Bm.alloc([128, 512], F32) for _ in range(2)]
            t2 = [Bm.alloc([128, 512], F32) for _ in range(2)]
            rs = [Bm.alloc([128, 512], F32) for _ in range(2)]
            B_t1 = [Buf(), Buf()]
            B_t2 = [Buf(), Buf()]
            B_rs = [Buf(), Buf()]
            qo = [Bm.alloc([128, 512], BF16) for _ in range(2)]
            B_qo = [Buf() for _ in range(2)]
            go = [Bm.alloc([128, 512], BF16) for _ in range(2)]
            B_go = [Buf() for _ in range(2)]
            ctr = dict(tile=0, blk=0, qo=0, go=0, mm=0)

            def prep_tile(src_rows, Ad, Bd, slot, col0):
                i = ctr["tile"] % 2
                ctr["tile"] += 1
                dma("sp", xt[i][:], src_rows, writes=[B_xt[i]])
                P.op("act", lambda e: e.activation(out=junk[:], in_=xt[i][:], func=AF.Square, accum_out=ssv[i][:]),
                     reads=[B_xt[i]], writes=[B_junk, B_ss[i]])
                rstd_from_ss(ssv[i][:], ssv[i][:], float(D), [B_ss[i]], B_ss[i])
                P.op("dve", lambda e: e.scalar_tensor_tensor(out=hf[:], in0=xt[i][:], scalar=ssv[i][:, 0:1], in1=Ad[:], op0=ALU.mult, op1=ALU.mult),
                     reads=[B_xt[i], B_ss[i], B_ab], writes=[B_hf])
                P.op("pool", lambda e: e.tensor_tensor(out=hb[i][:], in0=hf[:], in1=Bd[:], op=ALU.add),
                     reads=[B_hf, B_ab], writes=[B_hb[i]])
                def tr(e):
                    r = None
                    for kc in range(8):
                        r = e.transpose(pbt[:, kc * 128:(kc + 1) * 128], hb[i][:, kc * 128:(kc + 1) * 128], ident_b)
                    return r
                P.op("pe", tr, reads=[B_hb[i], B_const], writes=[B_pbt])
                P.op("act", lambda e: e.activation(out=hT[slot][:, :, col0:col0 + 128], in_=pbt[:].rearrange("p (k t) -> p k t", k=8), func=AF.Copy),
                     reads=[B_pbt], writes=[B_hT[slot]])

            def proj_mm(slot, c0, ntok):
                bk = ctr["mm"] % 4
                ctr["mm"] += 1
                def mm(e):
                    r = None
                    for kc in range(8):
                        r = e.matmul(pb[bk][:, 0:ntok], lhsT=win[:, kc, c0:c0 + 128], rhs=hT[slot][:, kc, 0:ntok], start=(kc == 0), stop=(kc == 7))
                    return r
                P.op("pe", mm, reads=[B_win, B_hT[slot]], writes=[B_pb[bk]])
                return bk

            def qk_block(slot, c0, ntok, norm, rope, gvec, dest, dest_bufs, ri):
                bk = proj_mm(slot, c0, ntok)
                i = ctr["blk"] % 2
                ctr["blk"] += 1
                if not norm and not rope:
                    P.op("act", lambda e: e.activation(out=dest, in_=pb[bk][:, 0:ntok], func=AF.Copy), reads=[B_pb[bk]], writes=dest_bufs)
                    return
                if norm:
                    P.op("act", lambda e: e.activation(out=xg[i][:, 0:ntok], in_=pb[bk][:, 0:ntok], func=AF.Copy, scale=gvec[:, 0:1]),
                         reads=[B_pb[bk], B_small], writes=[B_xg[i]])
                    P.op("act", lambda e: e.activation(out=sq[i][:, 0:ntok], in_=pb[bk][:, 0:ntok], func=AF.Square),
                         reads=[B_pb[bk]], writes=[B_sq[i]])
                    P.op("pe", lambda e: e.matmul(pb[5][:, 0:ntok], lhsT=bones_b, rhs=sq[i][:, 0:ntok], start=True, stop=True),
                         reads=[B_sq[i], B_const], writes=[B_pb[5]])
                    P.op("act", lambda e: e.activation(out=rs[i][:, 0:ntok], in_=pb[5][:, 0:ntok], func=AF.Sqrt, bias=epsc[:, 1:2], scale=1.0),
                         reads=[B_pb[5], B_const], writes=[B_rs[i]])
                    P.op("dve", lambda e: e.reciprocal(out=rs[i][:, 0:ntok], in_=rs[i][:, 0:ntok]), reads=[B_rs[i]], writes=[B_rs[i]])
                else:
                    P.op("act", lambda e: e.activation(out=xg[i][:, 0:ntok], in_=pb[bk][:, 0:ntok], func=AF.Copy),
                         reads=[B_pb[bk]], writes=[B_xg[i]])
                if rope:
                    P.op("pe", lambda e: e.matmul(pb[4][:, 0:ntok], lhsT=swap_b, rhs=xg[i][:, 0:ntok], start=True, stop=True),
                         reads=[B_xg[i], B_const], writes=[B_pb[4]])
                    P.op("dve", lambda e: e.tensor_tensor(out=t1[i][:, 0:ntok], in0=xg[i][:, 0:ntok], in1=rC[ri][:, 0:ntok], op=ALU.mult),
                         reads=[B_xg[i], B_rope[ri]], writes=[B_t1[i]])
                    P.op("dve", lambda e: e.tensor_tensor(out=t2[i][:, 0:ntok], in0=pb[4][:, 0:ntok], in1=rS[ri][:, 0:ntok], op=ALU.mult),
                         reads=[B_pb[4], B_rope[ri]], writes=[B_t2[i]])
                    if norm:
                        P.op("pool", lambda e: e.tensor_tensor(out=t1[i][:, 0:ntok], in0=t1[i][:, 0:ntok], in1=t2[i][:, 0:ntok], op=ALU.add),
                             reads=[B_t1[i], B_t2[i]], writes=[B_t1[i]])
                        P.op("dve", lambda e: e.scalar_tensor_tensor(out=dest, in0=t1[i][:, 0:ntok], scalar=8.0, in1=rs[i][:, 0:ntok], op0=ALU.mult, op1=ALU.mult),
                             reads=[B_t1[i], B_rs[i]], writes=dest_bufs)
                    else:
                        P.op("pool", lambda e: e.tensor_tensor(out=dest, in0=t1[i][:, 0:ntok], in1=t2[i][:, 0:ntok], op=ALU.add),
                             reads=[B_t1[i], B_t2[i]], writes=dest_bufs)
                else:
                    P.op("dve", lambda e: e.scalar_tensor_tensor(out=dest, in0=xg[i][:, 0:ntok], scalar=8.0, in1=rs[i][:, 0:ntok], op0=ALU.mult, op1=ALU.mult),
                         reads=[B_xg[i], B_rs[i]], writes=dest_bufs)

            def v_tiles(slot, ntiles, kt0, gi):
                for tt in range(ntiles):
                    def mm(e, tt=tt):
                        r = None
                        for kc in range(8):
                            r = e.matmul(pb[6][:, 0:256], lhsT=hT[slot][:, kc, tt * 128:(tt + 1) * 128], rhs=win[:, kc, 1280:1536], start=(kc == 0), stop=(kc == 7))
                        return r
                    P.op("pe", mm, reads=[B_win, B_hT[slot]], writes=[B_pb[6]])
                    kt = kt0 + tt
                    pv = pb[6][:, 0:256].rearrange("p (br kv d) -> p br kv d", br=2, kv=2)
                    P.op("act", lambda e, kt=kt, pv=pv: e.activation(out=VA[:, kt, :, 0, 0:64], in_=pv[:, :, 0, :], func=AF.Copy),
                         reads=[B_pb[6]], writes=[B_VA[gi]])
                    P.op("act", lambda e, kt=kt, pv=pv: e.activation(out=VA[:, kt, :, 1, 64:128], in_=pv[:, :, 1, :], func=AF.Copy),
                         reads=[B_pb[6]], writes=[B_VA[gi]])

            slot = 0
            for tt in range(2):
                prep_tile(ctx_d[b * CTX + tt * 128: b * CTX + (tt + 1) * 128, :], A1c, B1c, slot, tt * 128)
            qk_block(slot, 0, 256, True, False, gk, KT[0][:, NQB * 128:NKT * 128], [B_KT[0][NG]], 0)
            qk_block(slot, 128, 256, False, False, None, KT[1][:, NQB * 128:NKT * 128], [B_KT[1][NG]], 0)
            v_tiles(slot, 2, NQB, NG)

            for g in range(NG):
                slot = (g + 1) % 2
                ri = g % 2
                tok0 = b * T + g * 512
                dma("sp", rC[ri][:], ropeC_d[:, g * 512:(g + 1) * 512], writes=[B_rope[ri]])
                dma("sp", rS[ri][:], ropeS_d[:, g * 512:(g + 1) * 512], writes=[B_rope[ri]])
                for tt in range(4):
                    prep_tile(x_d[tok0 + tt * 128: tok0 + (tt + 1) * 128, :], A1, B1, slot, tt * 128)
                qk_block(slot, 0, 512, True, True, gk, KT[0][:, g * 512:(g + 1) * 512], [B_KT[0][g]], ri)
                qk_block(slot, 128, 512, False, True, None, KT[1][:, g * 512:(g + 1) * 512], [B_KT[1][g]], ri)
                v_tiles(slot, 4, g * 4, g)
                for br in range(2):
                    for j in range(4):
                        qi = ctr["qo"] % 2
                        ctr["qo"] += 1
                        qk_block(slot, 256 + br * 512 + j * 128, 512, br == 0, True, gq if br == 0 else None, qo[qi][:], [B_qo[qi]], ri)
                        dma("sp", qt_d[br, :, j, tok0:tok0 + 512], qo[qi][:], reads=[B_qo[qi]], writes=[B_qt[b][g]])
                for gb in range(16):
                    bk = proj_mm(slot, 1536 + gb * 128, 512)
                    gi = ctr["go"] % 2
                    ctr["go"] += 1
                    P.op("act", lambda e, bk=bk, gi=gi: e.activation(out=go[gi][:], in_=pb[bk][:], func=AF.Sigmoid),
                         reads=[B_pb[bk]], writes=[B_go[gi]])
                    dma("sp", g_d[gb, :, tok0:tok0 + 512], go[gi][:], reads=[B_go[gi]], writes=[B_g[b][g]])

            stop_at(2)
            P.barrier()
            P.phase += 1
            Cm = PH.sub()
            wbr = [Cm.alloc([128, 4, D], BF16) for _ in range(2)]
            wo = Cm.alloc([128, 8, D], BF16)
            wr = Cm.alloc([128, 8, E], F32)
            brb = Cm.alloc([128, E], F32)
            B_wC = Buf()
            dma("pool", wbr[0][:], wbra_d.rearrange("j p n -> p j n"), writes=[B_wC])
            dma("pool", wbr[1][:], wbrb_d.rearrange("j p n -> p j n"), writes=[B_wC])
            dma("pool", wo[:], wo_d.rearrange("(kc p) n -> p kc n", p=128), writes=[B_wC])
            dma("sp", wr[:], wr_d.rearrange("(kc p) n -> p kc n", p=128), writes=[B_wC])
            bc_load("sp", brb[:], br_d, writes=[B_wC])
            g1bc = Cm.alloc([128, D], F32)
            A2 = Cm.alloc([128, D], F32)
            B2 = Cm.alloc([128, D], F32)
            B_c2 = Buf()
            tmpg2 = Cm.alloc([128, D], F32)
            bc_load("sp", tmpg2[:], n2g_d, writes=[B_c2])
            bc_load("sp", g1bc[:], modrows_d[b, 2 * D:3 * D], reads=[B_modrows], writes=[B_c2])
            bc_load("sp", B2[:], modrows_d[b, 3 * D:4 * D], reads=[B_modrows], writes=[B_c2])
            bc_load("sp", A2[:], modrows_d[b, 4 * D:5 * D], reads=[B_modrows], writes=[B_c2])
            P.op("dve", lambda e: e.scalar_tensor_tensor(out=A2[:], in0=A2[:], scalar=1.0, in1=tmpg2[:], op0=ALU.add, op1=ALU.mult),
                 reads=[B_c2], writes=[B_c2])
            QT = [[Cm.alloc([128, 4, 512], BF16) for _ in range(2)] for _ in range(2)]
            B_QT = [Buf(), Buf()]
            GT = Cm.alloc([128, 16, 512], BF16)
            B_GT = Buf()
            yT = [Cm.alloc([128, 4, 512], BF16) for _ in range(2)]
            B_yT = [Buf(), Buf()]
            rcp = [Cm.alloc([128, 512], F32) for _ in range(2)]
            B_rcp = [Buf(), Buf()]
            mT = Cm.alloc([128, 8, 512], BF16)
            B_mT = Buf()
            ma = Cm.alloc([128, 512], F32)
            mbb = Cm.alloc([128, 512], F32)
            B_ma, B_mbb = Buf(), Buf()
            xc = Cm.alloc([128, D], F32)
            x1 = Cm.alloc([128, D], F32)
            h2f = Cm.alloc([128, D], F32)
            h2b = Cm.alloc([128, D], BF16)
            h2T = Cm.alloc([128, 8, 128], F32)
            junk2 = Cm.alloc([128, D], F32)
            ss2 = Cm.alloc([128, 1], F32)
            B_xc, B_x1t, B_h2f, B_h2b, B_h2T, B_junk2, B_ss2 = Buf(), Buf(), Buf(), Buf(), Buf(), Buf(), Buf()
            lg = Cm.alloc([128, E], F32)
            mx8 = Cm.alloc([128, 8], F32)
            ix8 = Cm.alloc([128, 8], U32)
            e4 = Cm.alloc([128, 4], F32)
            s4 = Cm.alloc([128, 1], F32)
            nmx = Cm.alloc([128, 1], F32)
            Mf = Cm.alloc([128, E], F32)
            Mb = Cm.alloc([128, E], BF16)
            rkf = Cm.alloc([128, E], F32)
            oh = Cm.alloc([128, E], F32)
            B_lg, B_mx8, B_ix8, B_e4, B_M, B_rkf, B_oh = Buf(), Buf(), Buf(), Buf(), Buf(), Buf(), Buf()
            cc = dict(s=0, o=0, p=0)

            LA = 2
            PT2 = [Cm.alloc([128, 1024], BF16) for _ in range(3)]
            B_PT2 = [Buf() for _ in range(3)]

            def attn_items(qslot, br, nblk, nloc):
                if br == 0:
                    kts = [(kt, None) for kt in range(NKT)]
                else:
                    kts = []
                    if nblk > 0:
                        kts.append((nblk - 1, 0))
                    kts.append((nblk, None))
                    if nblk < NQB - 1:
                        kts.append((nblk + 1, 1))
                    kts += [(NQB, None), (NQB + 1, None)]
                oq = 3
                return [dict(qslot=qslot, br=br, nloc=nloc, kt=kt, mk=mk, first=(ci == 0), last=(ci == len(kts) - 1), oq=oq)
                        for ci, (kt, mk) in enumerate(kts)]

            def attn_s(it):
                br, kt, mk = it["br"], it["kt"], it["mk"]
                ss_ = cc["s"] % 3
                cc["s"] += 1
                it["ss"] = ss_
                gk_ = NG if kt >= NQB else kt // 4
                it["gk"] = gk_
                def smm(e):
                    r = None
                    for kvh in range(2):
                        r0 = kvh * 64
                        rhsq = QT[it["qslot"]][br][r0:r0 + 64, :, it["nloc"] * 128:(it["nloc"] + 1) * 128]
                        o2 = PQ[ss_][:, kvh * 512:(kvh + 1) * 512]
                        r = e.matmul(o2.rearrange("p (j q) -> p j q", j=4), lhsT=KT[br][r0:r0 + 64, kt * 128:(kt + 1) * 128], rhs=rhsq, start=True, stop=(mk is None))
                    if mk is not None:
                        for kvh in range(2):
                            r = e.matmul(PQ[ss_][:, kvh * 512:(kvh + 1) * 512], lhsT=ident_b, rhs=mb_b[:, mk, :], start=False, stop=True)
                    return r
                P.op("pe", smm, reads=[B_KT[br][gk_], B_QT[it["qslot"]], B_const], writes=[B_pb[2 * ss_], B_pb[2 * ss_ + 1]])

            def attn_pv(it):
                br, kt, oq, ss_ = it["br"], it["kt"], it["oq"], it["ss"]
                nloc = it["nloc"]
                pi = cc["p"] % 3
                cc["p"] += 1
                P.op("act", lambda e: e.activation(out=PT2[pi][:], in_=PQ[ss_][:], func=AF.Exp, scale=0.125),
                     reads=[B_pb[2 * ss_], B_pb[2 * ss_ + 1]], writes=[B_PT2[pi]])
                def pv(e):
                    r = None
                    for kvh in range(2):
                        r = e.matmul(PQ[oq][:, kvh * 512:(kvh + 1) * 512], lhsT=VA[:, kt, br, kvh, :], rhs=PT2[pi][:, kvh * 512:(kvh + 1) * 512],
                                     start=it["first"], stop=it["last"])
                    return r
                P.op("pe", pv, reads=[B_VA[it["gk"]], B_PT2[pi]], writes=[B_pb[2 * oq], B_pb[2 * oq + 1]])
                if not it["last"]:
                    return
                for kvh in range(2):
                    ob = 2 * oq + kvh
                    o0, d0 = (0, 64) if kvh == 0 else (64, 0)
                    ri_ = kvh
                    den = pb[ob][d0:d0 + 64, :]
                    if br == 1:
                        P.op("dve", lambda e, ri_=ri_, o0=o0, d0=d0, den=den: e.tensor_tensor(out=rcp[ri_][o0:o0 + 64, :].rearrange("p (j q) -> p j q", j=4),
                                                              in0=den.rearrange("p (j q) -> p j q", j=4),
                                                              in1=sinkE[d0:d0 + 64, :].unsqueeze(2).to_broadcast([64, 4, 128]), op=ALU.add),
                             reads=[B_pb[ob], B_small], writes=[B_rcp[ri_]])
                        P.op("dve", lambda e, ri_=ri_, o0=o0: e.reciprocal(out=rcp[ri_][o0:o0 + 64, :], in_=rcp[ri_][o0:o0 + 64, :]),
                             reads=[B_rcp[ri_]], writes=[B_rcp[ri_]])
                    else:
                        P.op("dve", lambda e, ri_=ri_, o0=o0, den=den: e.reciprocal(out=rcp[ri_][o0:o0 + 64, :], in_=den), reads=[B_pb[ob]], writes=[B_rcp[ri_]])
                    P.op("dve", lambda e, ri_=ri_, o0=o0, ob=ob: e.tensor_tensor(out=yT[br][o0:o0 + 64, :, nloc * 128:(nloc + 1) * 128],
                                                          in0=pb[ob][o0:o0 + 64, :].rearrange("p (j q) -> p j q", j=4),
                                                          in1=rcp[ri_][o0:o0 + 64, :].rearrange("p (j q) -> p j q", j=4), op=ALU.mult),
                         reads=[B_pb[ob], B_rcp[ri_]], writes=[B_yT[br]])

            def load_q(g):
                qslot = g % 2
                tok0 = b * T + g * 512
                for br in range(2):
                    dma("sp", QT[qslot][br][:], qt_d[br, :, :, tok0:tok0 + 512], reads=[B_qt[b][g]], writes=[B_QT[qslot]])

            load_q(0)
            for g in range(NG):
                qslot = g % 2
                tok0 = b * T + g * 512
                if g + 1 < NG:
                    load_q(g + 1)
                dma("sp", GT[:], g_d[:, :, tok0:tok0 + 512].rearrange("c p t -> p c t"), reads=[B_g[b][g]], writes=[B_GT])
                items = []
                for nloc in range(4):
                    for br in range(2):
                        items += attn_items(qslot, br, g * 4 + nloc, nloc)
                for ii in range(len(items) + LA):
                    if ii < len(items):
                        attn_s(items[ii])
                    if ii - LA >= 0:
                        attn_pv(items[ii - LA])
                for fb in range(8):
                    for br in range(2):
                        bk = 4 + br
                        def mm(e, br=br, fb=fb, bk=bk):
                            r = None
                            for j in range(4):
                                r = e.matmul(pb[bk][:], lhsT=wbr[br][:, j, fb * 128:(fb + 1) * 128], rhs=yT[br][:, j, :], start=(j == 0), stop=(j == 3))
                            return r
                        P.op("pe", mm, reads=[B_wC, B_yT[br]], writes=[B_pb[bk]])
                    P.op("dve", lambda e, fb=fb: e.tensor_tensor(out=ma[:], in0=pb[4][:], in1=GT[:, fb, :], op=ALU.mult),
                         reads=[B_pb[4], B_GT], writes=[B_ma])
                    P.op("dve", lambda e, fb=fb: e.tensor_tensor(out=mbb[:], in0=pb[5][:], in1=GT[:, 8 + fb, :], op=ALU.mult),
                         reads=[B_pb[5], B_GT], writes=[B_mbb])
                    P.op("pool", lambda e, fb=fb: e.tensor_tensor(out=mT[:, fb, :], in0=ma[:], in1=mbb[:], op=ALU.add),
                         reads=[B_ma, B_mbb], writes=[B_mT])
                for tt in range(4):
                    ti = (tok0 + tt * 128) // 128
                    rows = slice(tok0 + tt * 128, tok0 + (tt + 1) * 128)
                    dma("sp", xc[:], x_d[rows, :], writes=[B_xc])
                    for nh in range(2):
                        bk = 4 + nh
                        def mm(e, tt=tt, nh=nh, bk=bk):
                            r = None
                            for kc in range(8):
                                r = e.matmul(pb[bk][:], lhsT=mT[:, kc, tt * 128:(tt + 1) * 128], rhs=wo[:, kc, nh * 512:(nh + 1) * 512], start=(kc == 0), stop=(kc == 7))
                            return r
                        P.op("pe", mm, reads=[B_wC, B_mT], writes=[B_pb[bk]])
                        P.op("dve", lambda e, nh=nh, bk=bk: e.tensor_tensor(out=x1[:, nh * 512:(nh + 1) * 512], in0=pb[bk][:], in1=g1bc[:, nh * 512:(nh + 1) * 512], op=ALU.mult),
                             reads=[B_pb[bk], B_c2], writes=[B_x1t])
                    P.op("pool", lambda e: e.tensor_tensor(out=x1[:], in0=x1[:], in1=xc[:], op=ALU.add), reads=[B_x1t, B_xc], writes=[B_x1t])
                    dma("sp", x1_d[rows, :], x1[:], reads=[B_x1t], writes=[B_x1[ti]])
                    P.op("act", lambda e: e.activation(out=junk2[:], in_=x1[:], func=AF.Square, accum_out=ss2[:]),
                         reads=[B_x1t], writes=[B_junk2, B_ss2])
                    rstd_from_ss(ss2[:], ss2[:], float(D), [B_ss2], B_ss2)
                    P.op("dve", lambda e: e.scalar_tensor_tensor(out=h2f[:], in0=x1[:], scalar=ss2[:, 0:1], in1=A2[:], op0=ALU.mult, op1=ALU.mult),
                         reads=[B_x1t, B_ss2, B_c2], writes=[B_h2f])
                    P.op("pool", lambda e: e.tensor_tensor(out=h2f[:], in0=h2f[:], in1=B2[:], op=ALU.add), reads=[B_h2f, B_c2], writes=[B_h2f])
                    P.op("act", lambda e: e.activation(out=h2b[:], in_=h2f[:], func=AF.Copy), reads=[B_h2f], writes=[B_h2b])
                    dma("sp", h2_d[rows, :], h2b[:], reads=[B_h2b], writes=[B_h2[ti]])
                    for hh in range(2):
                        def tr(e, hh=hh):
                            r = None
                            for k4 in range(4):
                                kc = hh * 4 + k4
                                r = e.transpose(pb[6][:, k4 * 128:(k4 + 1) * 128], h2f[:, kc * 128:(kc + 1) * 128], ident_f)
                            return r
                        P.op("pe", tr, reads=[B_h2f, B_const], writes=[B_pb[6]])
                        P.op("act", lambda e, hh=hh: e.activation(out=h2T[:, hh * 4:(hh + 1) * 4, :], in_=pb[6][:].rearrange("p (k t) -> p k t", k=4), func=AF.Copy),
                             reads=[B_pb[6]], writes=[B_h2T])
                    def rmm(e):
                        r = None
                        for kc in range(8):
                            r = e.matmul(pb[6][:, 0:E], lhsT=h2T[:, kc, :], rhs=wr[:, kc, :], start=(kc == 0), stop=(kc == 7))
                        return r
                    P.op("pe", rmm, reads=[B_h2T, B_wC], writes=[B_pb[6]])
                    P.op("dve", lambda e: e.tensor_tensor(out=lg[:], in0=pb[6][:, 0:E], in1=brb[:], op=ALU.add), reads=[B_pb[6], B_wC], writes=[B_lg])
                    P.op("dve", lambda e: e.max(out=mx8[:], in_=lg[:]), reads=[B_lg], writes=[B_mx8])
                    P.op("dve", lambda e: e.max_index(out=ix8[:], in_max=mx8[:], in_values=lg[:]), reads=[B_lg, B_mx8], writes=[B_ix8])
                    c0 = ti * TOPK
                    P.op("dve", lambda e, c0=c0: e.tensor_copy(out=r_idx[:, c0:c0 + 4], in_=ix8[:, 0:4]), reads=[B_ix8], writes=[B_ridx])
                    P.op("dve", lambda e: e.tensor_scalar(out=nmx[:], in0=mx8[:, 0:1], scalar1=-1.0, scalar2=None, op0=ALU.mult), reads=[B_mx8], writes=[B_e4])
                    P.op("act", lambda e: e.activation(out=e4[:], in_=mx8[:, 0:4], func=AF.Exp, bias=nmx[:, 0:1], scale=1.0, accum_out=s4[:]),
                         reads=[B_mx8, B_e4], writes=[B_e4])
                    P.op("dve", lambda e: e.reciprocal(out=s4[:], in_=s4[:]), reads=[B_e4], writes=[B_e4])
                    P.op("dve", lambda e, c0=c0: e.tensor_scalar(out=r_w[:, c0:c0 + 4], in0=e4[:], scalar1=s4[:, 0:1], scalar2=None, op0=ALU.mult),
                         reads=[B_e4], writes=[B_rw])
                    P.op("dve", lambda e: e.tensor_scalar(out=Mf[:], in0=lg[:], scalar1=mx8[:, 3:4], scalar2=None, op0=ALU.is_ge), reads=[B_lg, B_mx8], writes=[B_M])
                    P.op("dve", lambda e: e.tensor_copy(out=Mb[:], in_=Mf[:]), reads=[B_M], writes=[B_M])
                    P.op("pe", lambda e: e.matmul(pb[6][:, 0:E], lhsT=ltri_b, rhs=Mb[:], start=True, stop=True), reads=[B_M, B_const], writes=[B_pb[6]])
                    P.op("dve", lambda e: e.tensor_tensor(out=rkf[:], in0=pb[6][:, 0:E], in1=r_run[:], op=ALU.add), reads=[B_pb[6], B_run], writes=[B_rkf])
                    P.op("pe", lambda e: e.matmul(pb[6][:, 0:E], lhsT=ones_t[:], rhs=Mb[:], start=True, stop=True), reads=[B_M, B_const], writes=[B_pb[6]])
                    P.op("dve", lambda e: e.tensor_tensor(out=r_run[:], in0=pb[6][:, 0:E], in1=r_run[:], op=ALU.add), reads=[B_pb[6], B_run], writes=[B_run])
                    for k in range(TOPK):
                        P.op("dve", lambda e, c=c0 + k: e.tensor_scalar(out=oh[:], in0=iota_f[:, 0:E], scalar1=r_idx[:, c:c + 1], scalar2=None, op0=ALU.is_equal),
                             reads=[B_ridx, B_const], writes=[B_oh])
                        P.op("dve", lambda e: e.tensor_tensor(out=oh[:], in0=oh[:], in1=rkf[:], op=ALU.mult), reads=[B_oh, B_rkf], writes=[B_oh])
                        P.op("dve", lambda e, c=c0 + k: e.reduce_sum(out=r_rank[:, c:c + 1], in_=oh[:], axis=AX.X), reads=[B_oh], writes=[B_rrank])

        stop_at(3)
        P.barrier()
        P.phase += 1
        Dm = root.sub()
        cntp = Dm.alloc([128, E], F32)
        mod_ = Dm.alloc([128, E], F32)
        incl = Dm.alloc([128, E], F32)
        base = Dm.alloc([128, E], F32)
        B_d = Buf()
        MT = NTOK // 512
        thr = Dm.alloc([128, MT], F32)
        cmpc = Dm.alloc([128, E, MT], F32)
        P.op("dve", lambda e: e.tensor_scalar(out=thr[:], in0=iota_f[:, 0:MT], scalar1=512.0, scalar2=None, op0=ALU.mult), reads=[B_const], writes=[B_d])
        P.op("dve", lambda e: e.tensor_tensor(out=cmpc[:], in0=r_run[:].unsqueeze(2).to_broadcast([128, E, MT]),
                                              in1=thr[:].unsqueeze(1).to_broadcast([128, E, MT]), op=ALU.is_gt), reads=[B_run, B_d], writes=[B_d])
        P.op("dve", lambda e: e.reduce_sum(out=cntp[:], in_=cmpc[:], axis=AX.X), reads=[B_d], writes=[B_d])
        P.op("dve", lambda e: e.tensor_scalar(out=cntp[:], in0=cntp[:], scalar1=512.0, scalar2=None, op0=ALU.mult), reads=[B_d], writes=[B_d])
        P.op("dve", lambda e: e.tensor_copy(out=incl[:, 0:1], in_=cntp[:, 0:1]), reads=[B_d], writes=[B_d])
        for ei in range(1, E):
            P.op("dve", lambda e, ei=ei: e.tensor_tensor(out=incl[:, ei:ei + 1], in0=incl[:, ei - 1:ei], in1=cntp[:, ei:ei + 1], op=ALU.add), reads=[B_d], writes=[B_d])
        P.op("dve", lambda e: e.tensor_tensor(out=base[:], in0=incl[:], in1=cntp[:], op=ALU.subtract), reads=[B_d], writes=[B_d])
        tef = Dm.alloc([128, NT], F32)
        cmp3 = Dm.alloc([128, NT, E], F32)
        jv = Dm.alloc([128, NT], F32)
        P.op("dve", lambda e: e.tensor_scalar(out=jv[:], in0=iota_f[:, 0:NT], scalar1=512.0, scalar2=None, op0=ALU.mult), reads=[B_const], writes=[B_d])
        P.op("dve", lambda e: e.tensor_tensor(out=cmp3[:], in0=incl[:].unsqueeze(1).to_broadcast([128, NT, E]),
                                              in1=jv[:].unsqueeze(2).to_broadcast([128, NT, E]), op=ALU.is_le), reads=[B_d], writes=[B_d])
        P.op("dve", lambda e: e.reduce_sum(out=tef[:], in_=cmp3[:], axis=AX.X), reads=[B_d], writes=[B_d])
        P.op("dve", lambda e: e.tensor_scalar(out=tef[:], in0=tef[:], scalar1=float(E - 1), scalar2=None, op0=ALU.min), reads=[B_d], writes=[B_d])
        P.op("dve", lambda e: e.tensor_copy(out=te_i[:], in_=tef[0:1, :]), reads=[B_d], writes=[B_te])
        tk = Dm.alloc([128, NT], F32)
        P.op("dve", lambda e: e.scalar_tensor_tensor(out=tk[:], in0=tef[:], scalar=128.0, in1=pcol[:, 0:1].to_broadcast([128, NT]), op0=ALU.mult, op1=ALU.add),
             reads=[B_d, B_const], writes=[B_d])
        P.op("dve", lambda e: e.tensor_copy(out=idx_b1[:], in_=tk[:]), reads=[B_d], writes=[B_te])
        P.op("dve", lambda e: e.tensor_copy(out=idx_b2[:], in_=tef[:]), reads=[B_d], writes=[B_te])
        CH = min(64, NTK)
        oh3 = Dm.alloc([128, CH, E], F32)
        posf = Dm.alloc([128, NTK], F32)
        for c0 in range(0, NTK, CH):
            P.op("dve", lambda e, c0=c0: e.tensor_tensor(out=oh3[:], in0=iota_f[:, 0:E].unsqueeze(1).to_broadcast([128, CH, E]),
                                                         in1=r_idx[:, c0:c0 + CH].unsqueeze(2).to_broadcast([128, CH, E]), op=ALU.is_equal),
                 reads=[B_ridx, B_const], writes=[B_oh])
            P.op("dve", lambda e: e.tensor_tensor(out=oh3[:], in0=oh3[:], in1=base[:].unsqueeze(1).to_broadcast([128, CH, E]), op=ALU.mult),
                 reads=[B_oh, B_d], writes=[B_oh])
            P.op("dve", lambda e, c0=c0: e.reduce_sum(out=posf[:, c0:c0 + CH], in_=oh3[:], axis=AX.X), reads=[B_oh], writes=[B_d])
        P.op("dve", lambda e: e.tensor_tensor(out=posf[:], in0=posf[:], in1=r_rank[:], op=ALU.add), reads=[B_d, B_rrank], writes=[B_d])
        P.op("dve", lambda e: e.tensor_copy(out=r_pos[:], in_=posf[:]), reads=[B_d], writes=[B_rpos])

        hrow = [Dm.alloc([128, D], BF16) for _ in range(3)]
        B_hrow = [Buf() for _ in range(3)]
        for ti in range(NTT):
            s = ti % 3
            dma("sp", hrow[s][:], h2_d[ti * 128:(ti + 1) * 128, :], reads=[B_h2[ti]], writes=[B_hrow[s]])
            for k in range(TOPK):
                c = ti * TOPK + k
                P.op("pool", lambda e, s=s, c=c: e.indirect_dma_start(
                    out=hs_d[:, :], out_offset=bass.IndirectOffsetOnAxis(ap=r_pos[:, c:c + 1], axis=0), in_=hrow[s][:], in_offset=None),
                    reads=[B_hrow[s], B_rpos], writes=[B_hs[c]], dma=True)

        stop_at(4)
        P.barrier()
        P.phase += 1
        Em = root.sub()
        w1 = [Em.alloc([128, 8, 2 * D], BF16) for _ in range(2)]
        w2 = Em.alloc([128, 8, D], BF16)
        NSTG = 5
        stg = [Em.alloc([128, 2048], F32) for _ in range(NSTG)]
        B_stg = [Buf() for _ in range(NSTG)]
        B_w1c = [[Buf() for _ in range(8)] for _ in range(2)]
        B_w2c = [Buf() for _ in range(4)]
        b1 = [Em.alloc([128, 16], F32) for _ in range(2)]
        b2 = [Em.alloc([128, D], F32) for _ in range(2)]
        B_b = [Buf(), Buf()]
        hs = [Em.alloc([128, 4, D], BF16) for _ in range(2)]
        B_hsb = [Buf(), Buf()]
        hsT = [Em.alloc([128, 8, 512], BF16) for _ in range(2)]
        B_hsT = [Buf(), Buf()]
        actT = Em.alloc([128, 8, 512], BF16)
        B_actT = Buf()
        glu = [Em.alloc([128, 512], F32) for _ in range(2)]
        sg = [Em.alloc([128, 512], F32) for _ in range(2)]
        lin = [Em.alloc([128, 512], F32) for _ in range(2)]
        B_glu, B_sg, B_lin = [Buf(), Buf()], [Buf(), Buf()], [Buf(), Buf()]
        yo = [Em.alloc([128, D], F32) for _ in range(2)]
        B_yo = [Buf(), Buf()]
        ec = dict(u=0, y=0, g=0)

        be1_rows = be1_d.rearrange("e p c -> (e p) c")

        sc_ = dict(n=0)

        def load_b(j):
            s = j % 2
            def ld(e_, s=s, j=j):
                r = []
                r.append(e_.indirect_dma_start(out=b1[s][:], out_offset=None, in_=be1_rows,
                                               in_offset=bass.IndirectOffsetOnAxis(ap=idx_b1[:, j:j + 1], axis=0)))
                r.append(e_.indirect_dma_start(out=b2[s][:], out_offset=None, in_=be2_d,
                                               in_offset=bass.IndirectOffsetOnAxis(ap=idx_b2[:, j:j + 1], axis=0)))
                return r
            P.op("pool", ld, reads=[B_te], writes=[B_b[s]], dma=True, ndma=2)

        def load_w1(j, qs=range(8)):
            for q in qs:
                r_ = sc_["n"] % NSTG
                sc_["n"] += 1
                P.op("pool", lambda e_, q=q, r_=r_, j=j: e_.indirect_dma_start(
                    out=stg[r_][:], out_offset=None, in_=we1_d[q], in_offset=bass.IndirectOffsetOnAxis(ap=idx_b1[:, j:j + 1], axis=0)),
                    reads=[B_te], writes=[B_stg[r_]], dma=True)
                P.op("act", lambda e, q=q, r_=r_, j=j: e.activation(out=w1[j % 2][:, q, :], in_=stg[r_][:], func=AF.Copy),
                     reads=[B_stg[r_]], writes=[B_w1c[j % 2][q]])

        def load_w2(j):
            for q in range(4):
                r_ = sc_["n"] % NSTG
                sc_["n"] += 1
                P.op("pool", lambda e_, q=q, r_=r_, j=j: e_.indirect_dma_start(
                    out=stg[r_][:], out_offset=None, in_=we2_d[q], in_offset=bass.IndirectOffsetOnAxis(ap=idx_b1[:, j:j + 1], axis=0)),
                    reads=[B_te], writes=[B_stg[r_]], dma=True)
                P.op("act", lambda e, q=q, r_=r_: e.activation(out=w2[:, 2 * q:2 * q + 2, :].rearrange("p k n -> p (k n)"), in_=stg[r_][:], func=AF.Copy),
                     reads=[B_stg[r_]], writes=[B_w2c[q]])

        def load_h(j):
            s = j % 2
            dma("sp", hs[s][:], hs_d[j * 512:(j + 1) * 512, :].rearrange("(s p) d -> p s d", p=128), reads=B_hs, writes=[B_hsb[s]])

        def moe_transposes(j):
            s = j % 2
            for st in range(4):
                def tr(e, st=st, s=s):
                    r = None
                    for kc in range(8):
                        r = e.transpose(pbt[:, kc * 128:(kc + 1) * 128], hs[s][:, st, kc * 128:(kc + 1) * 128], ident_b)
                    return r
                P.op("pe", tr, reads=[B_hsb[s], B_const], writes=[B_pbt])
                if st % 2 == 0:
                    P.op("act", lambda e, st=st, s=s: e.activation(out=hsT[s][:, :, st * 128:(st + 1) * 128], in_=pbt[:].rearrange("p (k t) -> p k t", k=8), func=AF.Copy),
                         reads=[B_pbt], writes=[B_hsT[s]])
                else:
                    P.op("dve", lambda e, st=st, s=s: e.tensor_copy(out=hsT[s][:, :, st * 128:(st + 1) * 128], in_=pbt[:].rearrange("p (k t) -> p k t", k=8)),
                         reads=[B_pbt], writes=[B_hsT[s]])

        load_b(0)
        load_w1(0)
        load_w2(0)
        load_h(0)
        moe_transposes(0)
        for j in range(NT):
            s = j % 2
            if j + 1 < NT:
                load_b(j + 1)
                load_h(j + 1)
            for c in range(8):
                bks = []
                for half in range(2):
                    bk = ec["u"] % 4
                    ec["u"] += 1
                    col = half * D + c * 128
                    def mm(e, s=s, col=col, bk=bk):
                        r = None
                        for kc in range(8):
                            r = e.matmul(pb[bk][:], lhsT=w1[s][:, kc, col:col + 128], rhs=hsT[s][:, kc, :], start=(kc == 0), stop=(kc == 7))
                        return r
                    P.op("pe", mm, reads=B_w1c[s] + [B_hsT[s]], writes=[B_pb[bk]])
                    bks.append(bk)
                gi = ec["g"] % 2
                ec["g"] += 1
                P.op("dve", lambda e, s=s, c=c, gi=gi, bk=bks[0]: e.tensor_scalar(out=glu[gi][:], in0=pb[bk][:], scalar1=b1[s][:, c:c + 1], scalar2=7.0, op0=ALU.add, op1=ALU.min),
                     reads=[B_pb[bks[0]], B_b[s]], writes=[B_glu[gi]])
                P.op("act", lambda e, gi=gi: e.activation(out=sg[gi][:], in_=glu[gi][:], func=AF.Sigmoid, scale=1.702), reads=[B_glu[gi]], writes=[B_sg[gi]])
                P.op("dve", lambda e, s=s, c=c, gi=gi, bk=bks[1]: e.tensor_scalar(out=lin[gi][:], in0=pb[bk][:], scalar1=b1[s][:, 8 + c:9 + c], scalar2=7.0, op0=ALU.add, op1=ALU.min),
                     reads=[B_pb[bks[1]], B_b[s]], writes=[B_lin[gi]])
                P.op("dve", lambda e, gi=gi: e.tensor_scalar(out=lin[gi][:], in0=lin[gi][:], scalar1=-7.0, scalar2=1.0, op0=ALU.max, op1=ALU.add),
                     reads=[B_lin[gi]], writes=[B_lin[gi]])
                P.op("dve", lambda e, gi=gi: e.tensor_tensor(out=glu[gi][:], in0=glu[gi][:], in1=sg[gi][:], op=ALU.mult),
                     reads=[B_glu[gi], B_sg[gi]], writes=[B_glu[gi]])
                P.op("dve", lambda e, gi=gi, c=c: e.tensor_tensor(out=actT[:, c, :], in0=glu[gi][:], in1=lin[gi][:], op=ALU.mult),
                     reads=[B_glu[gi], B_lin[gi]], writes=[B_actT])
                if j + 1 < NT:
                    load_w1(j + 1, [c])
            if j + 1 < NT:
                moe_transposes(j + 1)
            for st in range(4):
                yi = ec["y"] % 2
                ec["y"] += 1
                for nh in range(2):
                    bk = 4 + nh
                    def mm(e, s=s, st=st, nh=nh, bk=bk):
                        r = None
                        for c in range(8):
                            r = e.matmul(pb[bk][:], lhsT=actT[:, c, st * 128:(st + 1) * 128], rhs=w2[:, c, nh * 512:(nh + 1) * 512], start=(c == 0), stop=(c == 7))
                        return r
                    P.op("pe", mm, reads=B_w2c + [B_actT], writes=[B_pb[bk]])
                    P.op("dve", lambda e, s=s, nh=nh, bk=bk, yi=yi: e.tensor_tensor(out=yo[yi][:, nh * 512:(nh + 1) * 512], in0=pb[bk][:], in1=b2[s][:, nh * 512:(nh + 1) * 512], op=ALU.add),
                         reads=[B_pb[bk], B_b[s]], writes=[B_yo[yi]])
                r0 = j * 512 + st * 128
                dma("sp", ys_d[r0:r0 + 128, :], yo[yi][:], reads=[B_yo[yi]], writes=[B_ys[j]])
            if j + 1 < NT:
                load_w2(j + 1)

        stop_at(5)
        P.barrier()
        P.phase += 1
        Fm = root.sub()
        g2bc = [Fm.alloc([128, D], F32) for _ in range(NB)]
        fgbc = Fm.alloc([128, D], F32)
        B_f = Buf()
        for b in range(NB):
            bc_load("sp", g2bc[b][:], modrows_d[b, 5 * D:6 * D], reads=[B_modrows], writes=[B_f])
        bc_load("sp", fgbc[:], fg_d, writes=[B_f])
        yk = [[Fm.alloc([128, D], F32) for _ in range(TOPK)] for _ in range(2)]
        B_yk = [[Buf() for _ in range(TOPK)] for _ in range(2)]
        x1f = [Fm.alloc([128, D], F32) for _ in range(2)]
        B_x1f = [Buf(), Buf()]
        acc = Fm.alloc([128, D], F32)
        B_acc = Buf()
        jf = Fm.alloc([128, D], F32)
        B_jf = Buf()
        ssf = Fm.alloc([128, 1], F32)
        B_ssf = Buf()
        of = [Fm.alloc([128, D], F32) for _ in range(2)]
        B_of = [Buf(), Buf()]
        for ti in range(NTT):
            s = ti % 2
            b = (ti * 128) // T
            for k in range(TOPK):
                c = ti * TOPK + k
                P.op("pool", lambda e, s=s, k=k, c=c: e.indirect_dma_start(
                    out=yk[s][k][:], out_offset=None, in_=ys_d[:, :], in_offset=bass.IndirectOffsetOnAxis(ap=r_pos[:, c:c + 1], axis=0)),
                    reads=[B_rpos] + B_ys, writes=[B_yk[s][k]], dma=True)
            dma("sp", x1f[s][:], x1_d[ti * 128:(ti + 1) * 128, :], reads=[B_x1[ti]], writes=[B_x1f[s]])
            for k in range(TOPK):
                c = ti * TOPK + k
                if k == 0:
                    P.op("dve", lambda e, s=s, c=c: e.tensor_scalar(out=acc[:], in0=yk[s][0][:], scalar1=r_w[:, c:c + 1], scalar2=None, op0=ALU.mult),
                         reads=[B_yk[s][0], B_rw], writes=[B_acc])
                else:
                    P.op("dve", lambda e, s=s, k=k, c=c: e.scalar_tensor_tensor(out=acc[:], in0=yk[s][k][:], scalar=r_w[:, c:c + 1], in1=acc[:], op0=ALU.mult, op1=ALU.add),
                         reads=[B_yk[s][k], B_rw, B_acc], writes=[B_acc])
            P.op("dve", lambda e, b=b: e.tensor_tensor(out=acc[:], in0=acc[:], in1=g2bc[b][:], op=ALU.mult), reads=[B_acc, B_f], writes=[B_acc])
            P.op("dve", lambda e, s=s: e.tensor_tensor(out=acc[:], in0=acc[:], in1=x1f[s][:], op=ALU.add), reads=[B_acc, B_x1f[s]], writes=[B_acc])
            P.op("act", lambda e: e.activation(out=jf[:], in_=acc[:], func=AF.Square, accum_out=ssf[:]), reads=[B_acc], writes=[B_jf, B_ssf])
            rstd_from_ss(ssf[:], ssf[:], float(D), [B_ssf], B_ssf)
            P.op("dve", lambda e, s=s: e.scalar_tensor_tensor(out=of[s][:], in0=acc[:], scalar=ssf[:, 0:1], in1=fgbc[:], op0=ALU.mult, op1=ALU.mult),
                 reads=[B_acc, B_ssf, B_f], writes=[B_of[s]])
            P.op("sp", lambda e, ti=ti, s=s: e.dma_start(out=out_d[ti * 128:(ti + 1) * 128, :], in_=of[s][:]), reads=[B_of[s]], writes=[Buf()], dma=True, out=True)
    except _Stop:
        pass
    ops = P.ops
    for o in ops:
        for d in o["deps"]:
            ops[d]["sig"] = True
    for o in ops:
        if o["out"] or o["dma"]:
            o["sig"] = True
    nphase = P.phase + 1
    with contextlib.ExitStack() as es:
        engsem = {}
        for ph in range(nphase):
            for en in ENGS[:4]:
                engsem[(ph, en)] = es.enter_context(nc.semaphore("s_%s_%d" % (en, ph)))
        NDS = 8
        dmasem = {q: [es.enter_context(nc.semaphore("d_%s_%d" % (q, i))) for i in range(NDS)] for q in ("sp", "pool", "act")}
        seq = {}
        dcount = {q: [0] * NDS for q in dmasem}
        drr = {q: 0 for q in dmasem}
        for i, o in enumerate(ops):
            if not o["sig"]:
                continue
            if o["dma"]:
                q = o["eng"]
                k = drr[q] % NDS
                drr[q] += 1
                o["sem"] = dmasem[q][k]
                o["semk"] = (q, k)
            else:
                key = (o["phase"], o["eng"])
                seq[key] = seq.get(key, 0) + 1
                o["sem"] = engsem[key]
                o["val"] = seq[key]
        block = es.enter_context(nc.Block())

        def emit(en, e):
            waited = {}
            for i in P.byeng[en]:
                o = ops[i]
                need = {}
                for d in o["deps"]:
                    od = ops[d]
                    if en == "pe" and od["eng"] == "pe" and not od["dma"]:
                        continue
                    sem, val = od["sem"], od["val"]
                    key = id(sem)
                    if key not in need or need[key][1] < val:
                        need[key] = (sem, val)
                for key, (sem, val) in need.items():
                    if waited.get(key, 0) < val:
                        e.wait_ge(sem, val)
                        waited[key] = val
                if o["fn"] is None:
                    continue
                r = o["fn"](e)
                if o["sig"]:
                    if o["dma"]:
                        rl = r if isinstance(r, list) else [r]
                        q, k = o["semk"]
                        for ins in rl:
                            ins.then_inc(o["sem"], 16)
                    else:
                        r.then_inc(o["sem"], 1)
            if en == "sp":
                for k in range(NDS):
                    if dcount["sp"][k] > 0:
                        e.wait_ge(dmasem["sp"][k], dcount["sp"][k])

        for q in dmasem:
            for i in P.byeng[q]:
                o = ops[i]
                if o["dma"] and o["sig"]:
                    qq, k = o["semk"]
                    n = o["ndma"]
                    dcount[qq][k] += 16 * n
                    o["val"] = dcount[qq][k]

        @block.tensor
        def _(e):
            emit("pe", e)

        @block.scalar
        def _(e):
            emit("act", e)

        @block.vector
        def _(e):
            emit("dve", e)

        @block.gpsimd
        def _(e):
            emit("pool", e)

        @block.sync
        def _(e):
            emit("sp", e)
    return nc


def _rope_tables(T):
    half = 32
    inv = (10000.0 ** (-(np.arange(half // 2, dtype=np.float32) * 2.0 / half))).astype(np.float32)
    t = np.arange(T)
    row = (t // GRID_W).astype(np.float32)
    col = (t % GRID_W).astype(np.float32)
    C = np.zeros((128, T), np.float32)
    S = np.zeros((128, T), np.float32)
    for p in range(128):
        d = p % 64
        pos = row if d < 32 else col
        j = d % 16
        ang = (pos * inv[j]).astype(np.float32)
        C[p] = np.cos(ang)
        sgn = -1.0 if (d % 32) < 16 else 1.0
        S[p] = sgn * np.sin(ang)
    return C, S


def _consts():
    cm = np.zeros((128, 4, 128), np.float32)
    cm[:, 0, :] = np.eye(128)
    for m in range(128):
        d = m % 32
        k = m + 16 if d < 16 else m - 16
        cm[k, 1, m] = 1.0
    for k in range(128):
        for m in range(128):
            if k // 64 == m // 64:
                cm[k, 2, m] = 1.0
            if k < m:
                cm[k, 3, m] = 1.0
    mb = np.zeros((128, 2, 4, 128), np.float32)
    kj = np.arange(128)[:, None]
    qi = np.arange(128)[None, :]
    lo = np.where(kj >= qi, 0.0, MASKV).astype(np.float32)
    hi = np.where(kj <= qi, 0.0, MASKV).astype(np.float32)
    mb[:, 0, :, :] = lo[:, None, :]
    mb[:, 1, :, :] = hi[:, None, :]
    iot = np.tile(np.arange(128, dtype=np.float32)[None, :], (128, 1))
    return cm, mb.reshape(128, 2, 512), iot


def _prep_shared(cfg, inp):
    E = cfg["E"]
    f = lambda a: np.ascontiguousarray(a, dtype=np.float32)
    w_in = inp["w_in"][0]
    KVW = 128
    k_a, v_a, k_b, v_b = (w_in[:, i * KVW:(i + 1) * KVW] for i in range(4))
    q_a = w_in[:, 512:1024]
    q_b = w_in[:, 1024:1536]
    g_a = w_in[:, 1536:2560]
    g_b = w_in[:, 2560:3584]

    def pairs(q):
        cols = []
        for j in range(4):
            cols.append(q[:, j * 64:(j + 1) * 64])
            cols.append(q[:, (j + 4) * 64:(j + 5) * 64])
        return np.concatenate(cols, axis=1)

    w_in_p = np.concatenate([k_a, k_b, pairs(q_a), pairs(q_b), v_a, v_b, g_a, g_b], axis=1)

    def brp(w):
        w = w[0]
        return np.stack([np.concatenate([w[j * 64:(j + 1) * 64], w[(j + 4) * 64:(j + 5) * 64]], axis=0) for j in range(4)], axis=0)

    sink = inp["sink"][0]
    sinkT = np.zeros((128, 4), np.float32)
    sinkT[0:64, :] = sink[4:8][None, :]
    sinkT[64:128, :] = sink[0:4][None, :]
    we1 = inp["w_e1"][0]
    we1p = np.concatenate([we1[:, :, 0::2], we1[:, :, 1::2]], axis=2)
    be1 = inp["b_e1"][0]
    be1p = np.concatenate([be1[:, 0::2], be1[:, 1::2]], axis=1)
    be1T = be1p.reshape(E, 16, 128).transpose(0, 2, 1)
    cm, mb, iot = _consts()
    C, S = _rope_tables(cfg["T"])
    return {
        "w_mod": f(inp["w_mod"][0]), "b_mod": f(inp["b_mod"][0]),
        "norm1_g": f(inp["norm1_g"][0]), "norm2_g": f(inp["norm2_g"][0]), "final_g": f(inp["final_g"]),
        "w_in": f(w_in_p),
        "gq2": f(np.tile(inp["q_norm_g"][0], 2).reshape(128, 1)),
        "gk2": f(np.tile(inp["k_norm_g"][0], 2).reshape(128, 1)),
        "sinkT": f(sinkT),
        "w_br_a": f(brp(inp["w_br_a"])), "w_br_b": f(brp(inp["w_br_b"])),
        "w_o": f(inp["w_o"][0]), "w_router": f(inp["w_router"][0]), "b_router": f(inp["b_router"][0]),
        "b_e1T": f(be1T), "b_e2": f(inp["b_e2"][0]),
        **{"w_e1_%d" % q: f(we1p.reshape(E, 8, 128, 2 * D)[:, q, :, :].reshape(E * 128, 2048)) for q in range(8)},
        **{"w_e2_%d" % q: f(inp["w_e2"][0].reshape(E, 8, 128, D).transpose(0, 2, 1, 3)[:, :, 2 * q:2 * q + 2, :].reshape(E * 128, 2048)) for q in range(4)},
        "ropeC": f(C), "ropeS": f(S), "cmats": f(cm), "maskb": f(mb), "iotas": f(iot),
        "pcol": f(np.arange(128, dtype=np.float32)[:, None] + 128.0 * np.arange(8, dtype=np.float32)[None, :]),
    }


def run(cfg, inp, trace=False):
    NCORE, NB, T = cfg["NCORE"], cfg["NB"], cfg["T"]
    nc = build(cfg)
    shared = _prep_shared(cfg, inp)
    in_maps = []
    for c in range(NCORE):
        bs = slice(c * NB, (c + 1) * NB)
        m = dict(shared)
        m["x"] = np.ascontiguousarray(inp["x"][bs].reshape(NB * T, D), dtype=np.float32)
        m["ctx"] = np.ascontiguousarray(inp["ctx"][bs].reshape(NB * CTX, D), dtype=np.float32)
        cv = np.concatenate([inp["c"][bs], inp["c_ctx"][None, :]], axis=0)
        m["cT"] = np.ascontiguousarray(cv.reshape(NB + 1, 8, 128).transpose(2, 1, 0), dtype=np.float32)
        in_maps.append(m)
    res = run_bass_kernel_spmd(nc, in_maps, core_ids=list(range(NCORE)), trace=trace)
    out = np.concatenate([r["out"].reshape(NB, T, D) for r in res.results], axis=0)
    return out.astype(np.float32), res


def kernel(**inputs):
    inp = {k: np.asarray(v) for k, v in inputs.items()}
    out, _ = run(CFG, inp)
    return out
```

```python
import contextlib
import numpy as np
import ml_dtypes
import concourse.bass as bass
import concourse.mybir as mybir
from concourse.bass_utils import run_bass_kernel_spmd

F32 = mybir.dt.float32
BF16 = mybir.dt.bfloat16
I32 = mybir.dt.int32
U32 = mybir.dt.uint32
AF = mybir.ActivationFunctionType
ALU = mybir.AluOpType
AX = mybir.AxisListType

D = 1024
CTX = 256
GRID_W = 64
TOPK = 4
EPS = 1e-6
MASKV = -30000.0

CFG = dict(NCORE=8, NB=2, T=4096, E=32)

ENGS = ["pe", "act", "dve", "pool", "sp"]


class Buf:
    __slots__ = ("w", "r", "rd")

    def __init__(self):
        self.w = None
        self.r = {}
        self.rd = []


class Prog:
    def __init__(self):
        self.ops = []
        self.byeng = {e: [] for e in ENGS}
        self.phase = 0
        self.pending_dma = []

    def barrier(self):
        deps = set(self.pending_dma)
        for en in ENGS:
            for i in reversed(self.byeng[en]):
                if self.ops[i]["fn"] is not None:
                    deps.add(i)
                    break
        for en in ENGS:
            idx = len(self.ops)
            self.ops.append(dict(eng=en, fn=None, deps=set(deps), dma=False, sig=False, phase=self.phase, ndma=1, out=False))
            self.byeng[en].append(idx)
        self.pending_dma = []

    def op(self, eng, fn, reads=(), writes=(), dma=False, ndma=1, out=False):
        idx = len(self.ops)
        if dma:
            self.pending_dma.append(idx)
        deps = set()
        for b in reads:
            if b.w is not None:
                deps.add(b.w)
        for b in writes:
            if b.w is not None:
                deps.add(b.w)
            deps.update(b.r.values())
            deps.update(b.rd)
        for b in reads:
            if dma:
                b.rd.append(idx)
            else:
                b.r[eng] = idx
        for b in writes:
            b.w = idx
            b.r = {}
            b.rd = []
        deps.discard(idx)
        self.ops.append(dict(eng=eng, fn=fn, deps=deps, dma=dma, sig=False, phase=self.phase, ndma=ndma, out=out))
        self.byeng[eng].append(idx)
        return idx


def build(cfg):
    NB, T, E = cfg["NB"], cfg["T"], cfg["E"]
    NG = T // 512
    NQB = T // 128
    NKT = NQB + 2
    NTOK = NB * T
    NTT = NTOK // 128
    NT = (NTOK * TOPK) // 512 + E
    NSLOT = NT * 512

    nc = bass.Bass("TRN2", target_bir_lowering=False)
    P = Prog()

    def din(name, shape, dt=F32):
        return nc.dram_tensor(name, list(shape), dt, kind="ExternalInput").ap()

    def dscr(name, shape, dt):
        return nc.dram_tensor(name, list(shape), dt).ap()

    x_d = din("x", [NTOK, D])
    ctx_d = din("ctx", [NB * CTX, D])
    cT_d = din("cT", [128, 8, NB + 1])
    wmod_d = din("w_mod", [D, 6 * D])
    bmod_d = din("b_mod", [6 * D])
    n1g_d = din("norm1_g", [D])
    n2g_d = din("norm2_g", [D])
    fg_d = din("final_g", [D])
    win_d = din("w_in", [D, 3584])
    gq_d = din("gq2", [128, 1])
    gk_d = din("gk2", [128, 1])
    sink_d = din("sinkT", [128, 4])
    wbra_d = din("w_br_a", [4, 128, D])
    wbrb_d = din("w_br_b", [4, 128, D])
    wo_d = din("w_o", [D, D])
    wr_d = din("w_router", [D, E])
    br_d = din("b_router", [E])
    we1_d = [din("w_e1_%d" % q, [E * 128, 2048]) for q in range(8)]
    be1_d = din("b_e1T", [E, 128, 16])
    we2_d = [din("w_e2_%d" % q, [E * 128, 2048]) for q in range(4)]
    be2_d = din("b_e2", [E, D])
    ropeC_d = din("ropeC", [128, T])
    ropeS_d = din("ropeS", [128, T])
    cm_d = din("cmats", [128, 4, 128])
    mb_d = din("maskb", [128, 2, 512])
    iota_d = din("iotas", [128, 128])
    pcol_d = din("pcol", [128, 8])
    out_d = nc.dram_tensor("out", [NTOK, D], F32, kind="ExternalOutput").ap()

    modrows_d = dscr("modrows", [NB + 1, 6 * D], F32)
    qt_d = dscr("qt_scr", [2, 128, 4, NTOK], BF16)
    g_d = dscr("g_scr", [16, 128, NTOK], BF16)
    x1_d = dscr("x1_scr", [NTOK, D], F32)
    h2_d = dscr("h2_scr", [NTOK, D], BF16)
    hs_d = dscr("hs_scr", [NSLOT, D], BF16)
    ys_d = dscr("ys_scr", [NSLOT, D], F32)

    B_modrows = Buf()
    B_qt = [[Buf() for _ in range(NG)] for _ in range(NB)]
    B_g = [[Buf() for _ in range(NG)] for _ in range(NB)]
    B_x1 = [Buf() for _ in range(NTT)]
    B_h2 = [Buf() for _ in range(NTT)]
    B_hs = [Buf() for _ in range(NTT * TOPK)]
    B_ys = [Buf() for _ in range(NT)]

    SB_LO, SB_HI = 16896, 229376
    cnt = [0]

    class Arena:
        def __init__(self, lo, hi):
            self.lo, self.hi, self.cur = lo, hi, lo

        def alloc(self, shape, dt, nbuf=None):
            cnt[0] += 1
            esz = 4 if dt in (F32, I32, U32) else 2
            n = esz
            for s in shape[1:]:
                n *= s
            n = (n + 63) // 64 * 64
            assert self.cur + n <= self.hi, ("SBUF overflow", shape, self.cur, self.hi)
            t = nc.alloc_sbuf_tensor_at("sb%d" % cnt[0], list(shape), dt, offset=self.cur)
            self.cur += n
            return t

        def sub(self):
            return Arena(self.cur, self.hi)

    root = Arena(SB_LO, SB_HI)

    cm_f = root.alloc([128, 4, 128], F32)
    cm_b = root.alloc([128, 4, 128], BF16)
    mb_b = root.alloc([128, 2, 512], BF16)
    iota_f = root.alloc([128, 128], F32)
    B_const = Buf()
    ident_f, ident_b = cm_f[:, 0, :], cm_b[:, 0, :]
    swap_b, bones_b, ltri_b = cm_b[:, 1, :], cm_b[:, 2, :], cm_b[:, 3, :]
    ones_b = None

    ones_t = root.alloc([128, 128], BF16)
    NTK = NTT * TOPK
    r_idx = root.alloc([128, NTK], F32)
    r_rank = root.alloc([128, NTK], F32)
    r_w = root.alloc([128, NTK], F32)
    r_pos = root.alloc([128, NTK], I32)
    r_run = root.alloc([128, E], F32)
    te_i = root.alloc([1, NT], I32)
    idx_b1 = root.alloc([128, NT], I32)
    idx_b2 = root.alloc([128, NT], I32)
    pcol = root.alloc([128, 8], F32)
    epsc = root.alloc([128, 2], F32)
    B_ridx, B_rrank, B_rw, B_rpos, B_run, B_te = Buf(), Buf(), Buf(), Buf(), Buf(), Buf()

    class _V:
        def __init__(self, ap):
            self.ap = ap

        def __getitem__(self, k):
            return self.ap[k]

    PQ = [nc.alloc_psum_tensor("pq%d" % i, [128, 1024], F32) for i in range(4)]
    pb = [_V(PQ[i // 2][:, (i % 2) * 512:(i % 2 + 1) * 512]) for i in range(8)]
    pbt = _V(PQ[3][:, 512:1024].bitcast(BF16))
    B_pb = [Buf() for _ in range(8)]
    B_pbt = B_pb[7]

    def dma(q, out, in_, reads=(), writes=(), **kw):
        return P.op(q, lambda e: e.dma_start(out=out, in_=in_, **kw), reads=reads, writes=writes, dma=True)

    def bc_load(q, dst, src_row, writes, reads=()):
        return dma(q, dst, src_row.partition_broadcast(128), reads=reads, writes=writes)

    class _Stop(Exception):
        pass

    def stop_at(n):
        if cfg.get("STOP", 99) == n:
            raise _Stop()

    try:
        dma("sp", cm_f[:], cm_d[:, :, :], writes=[B_const])
        dma("sp", iota_f[:], iota_d[:, :], writes=[B_const])
        dma("sp", pcol[:], pcol_d[:, :], writes=[B_const])
        P.op("dve", lambda e: e.tensor_copy(out=cm_b[:], in_=cm_f[:]), reads=[B_const], writes=[B_const])
        P.op("dve", lambda e: e.memset(ones_t[:], 1.0), writes=[B_const])
        P.op("dve", lambda e: e.memset(epsc[:, 0:1], EPS), writes=[B_const])
        P.op("dve", lambda e: e.memset(epsc[:, 1:2], 64.0 * EPS), writes=[B_const])
        P.op("dve", lambda e: e.memset(r_run[:], 0.0), writes=[B_run])

        ATT = root.sub()
        KT = [ATT.alloc([128, NKT * 128], BF16) for _ in range(2)]
        VA = ATT.alloc([128, NKT, 2, 2, 128], BF16)
        B_KT = [[Buf() for _ in range(NG + 1)] for _ in range(2)]
        B_VA = [Buf() for _ in range(NG + 1)]
        gq = ATT.alloc([128, 1], F32)
        gk = ATT.alloc([128, 1], F32)
        sinkT = ATT.alloc([128, 4], F32)
        sinkE = ATT.alloc([128, 4], F32)
        B_small = Buf()
        dma("sp", gq[:], gq_d[:, :], writes=[B_small])
        dma("sp", gk[:], gk_d[:, :], writes=[B_small])
        dma("sp", sinkT[:], sink_d[:, :], writes=[B_small])
        P.op("act", lambda e: e.activation(out=sinkE[:], in_=sinkT[:], func=AF.Exp), reads=[B_small], writes=[B_small])
        P.op("pool", lambda e: e.memset(VA[:, :, :, 0, 64:128], 1.0), writes=B_VA)
        P.op("pool", lambda e: e.memset(VA[:, :, :, 1, 0:64], 1.0), writes=B_VA)

        PH = ATT.sub()
        A = PH.sub()
        mb_f = A.alloc([128, 2, 512], F32)
        B_mbf = Buf()
        dma("sp", mb_f[:], mb_d[:, :, :], writes=[B_mbf])
        P.op("dve", lambda e: e.tensor_copy(out=mb_b[:], in_=mb_f[:]), reads=[B_mbf], writes=[B_const])
        cT = A.alloc([128, 8, NB + 1], F32)
        scb = A.alloc([128, NB + 1, 8, 128], BF16)
        sc1 = A.alloc([128, 8, NB + 1], BF16)
        bmod_bc = A.alloc([128, 6 * D], F32)
        wm = [A.alloc([128, 8, 512], BF16) for _ in range(2)]
        mrow = [A.alloc([128, 512], F32) for _ in range(2)]
        B_cT, B_scb, B_bmod = Buf(), Buf(), Buf()
        B_wm = [Buf(), Buf()]
        B_mrow = [Buf(), Buf()]
        dma("sp", cT[:], cT_d[:, :, :], writes=[B_cT])
        bc_load("sp", bmod_bc[:], bmod_d, writes=[B_bmod])
        P.op("act", lambda e: e.activation(out=sc1[:], in_=cT[:], func=AF.Silu), reads=[B_cT], writes=[B_cT])
        for v in range(NB + 1):
            for kc in range(8):
                P.op("dve", lambda e, v=v, kc=kc: e.tensor_copy(
                    out=scb[:, v, kc, :], in_=sc1[:, kc, v:v + 1].to_broadcast([128, 128])),
                    reads=[B_cT], writes=[B_scb])
        wmod_v = wmod_d.rearrange("(kc p) n -> p kc n", p=128)
        nmr = 0
        for cc in range(12):
            s = cc % 2
            dma("pool", wm[s][:], wmod_v[:, :, cc * 512:(cc + 1) * 512], writes=[B_wm[s]])
            for v in range(NB + 1):
                bk = (cc * (NB + 1) + v) % 4
                def mm(e, v=v, s=s, bk=bk):
                    r = None
                    for kc in range(8):
                        r = e.matmul(pb[bk][:], lhsT=scb[:, v, kc, :], rhs=wm[s][:, kc, :], start=(kc == 0), stop=(kc == 7))
                    return r
                P.op("pe", mm, reads=[B_scb, B_wm[s]], writes=[B_pb[bk]])
                ms = nmr % 2
                nmr += 1
                P.op("dve", lambda e, bk=bk, ms=ms, cc=cc: e.tensor_tensor(
                    out=mrow[ms][:], in0=pb[bk][:], in1=bmod_bc[:, cc * 512:(cc + 1) * 512], op=ALU.add),
                    reads=[B_pb[bk], B_bmod], writes=[B_mrow[ms]])
                dma("sp", modrows_d[v:v + 1, cc * 512:(cc + 1) * 512], mrow[ms][0:1, :], reads=[B_mrow[ms]], writes=[B_modrows])

        P.barrier()

        def rstd_from_ss(ss, out, n, dep_bufs, wbuf):
            P.op("act", lambda e: e.activation(out=out, in_=ss, func=AF.Sqrt, bias=epsc[:, 0:1], scale=1.0 / n),
                 reads=list(dep_bufs) + [B_const], writes=[wbuf])
            P.op("dve", lambda e: e.reciprocal(out=out, in_=out), reads=[wbuf], writes=[wbuf])

        stop_at(1)
        for b in range(NB):
            if b > 0:
                P.barrier()
            P.phase += 1
            Bm = PH.sub()
            win = Bm.alloc([128, 8, 3584], BF16)
            B_win = Buf()
            win_v = win_d.rearrange("(kc p) n -> p kc n", p=128)
            for (c0, c1) in ((0, 1536), (1536, 3584)):
                dma("pool", win[:, :, c0:c1], win_v[:, :, c0:c1], writes=[B_win])
            A1 = Bm.alloc([128, D], F32)
            B1 = Bm.alloc([128, D], F32)
            A1c = Bm.alloc([128, D], F32)
            B1c = Bm.alloc([128, D], F32)
            B_ab = Buf()
            tmpg = Bm.alloc([128, D], F32)
            bc_load("sp", tmpg[:], n1g_d, writes=[B_ab])
            for (v, Ad, Bd) in ((b, A1, B1), (NB, A1c, B1c)):
                bc_load("sp", Ad[:], modrows_d[v, D:2 * D], reads=[B_modrows], writes=[B_ab])
                bc_load("sp", Bd[:], modrows_d[v, 0:D], reads=[B_modrows], writes=[B_ab])
                P.op("dve", lambda e, Ad=Ad: e.scalar_tensor_tensor(out=Ad[:], in0=Ad[:], scalar=1.0, in1=tmpg[:], op0=ALU.add, op1=ALU.mult),
                     reads=[B_ab], writes=[B_ab])
            xt = [Bm.alloc([128, D], F32) for _ in range(2)]
            B_xt = [Buf(), Buf()]
            junk = Bm.alloc([128, D], F32)
            B_junk = Buf()
            ssv = [Bm.alloc([128, 1], F32) for _ in range(2)]
            B_ss = [Buf(), Buf()]
            hf = Bm.alloc([128, D], F32)
            B_hf = Buf()
            hb = [Bm.alloc([128, D], BF16) for _ in range(2)]
            B_hb = [Buf(), Buf()]
            hT = [Bm.alloc([128, 8, 512], BF16) for _ in range(2)]
            B_hT = [Buf(), Buf()]
            rC = [Bm.alloc([128, 512], F32) for _ in range(2)]
            rS = [Bm.alloc([128, 512], F32) for _ in range(2)]
            B_rope = [Buf(), Buf()]
            xg = [Bm.alloc([128, 512], BF16) for _ in range(2)]
            sq = [Bm.alloc([128, 512], BF16) for _ in range(2)]
            B_xg = [Buf(), Buf()]
            B_sq = [Buf(), Buf()]
            t1 = [Bm.alloc([128, 512], F32) for _ in range(2)]
            t2 = [Bm.alloc([128, 512], F32) for _ in range(2)]
            rs = [Bm.alloc([128, 512], F32) for _ in range(2)]
            B_t1 = [Buf(), Buf()]
            B_t2 = [Buf(), Buf()]
            B_rs = [Buf(), Buf()]
            qo = [Bm.alloc([128, 512], BF16) for _ in range(2)]
            B_qo = [Buf() for _ in range(2)]
            go = [Bm.alloc([128, 512], BF16) for _ in range(2)]
            B_go = [Buf() for _ in range(2)]
            ctr = dict(tile=0, blk=0, qo=0, go=0, mm=0)

            def prep_tile(src_rows, Ad, Bd, slot, col0):
                i = ctr["tile"] % 2
                ctr["tile"] += 1
                dma("sp", xt[i][:], src_rows, writes=[B_xt[i]])
                P.op("act", lambda e: e.activation(out=junk[:], in_=xt[i][:], func=AF.Square, accum_out=ssv[i][:]),
                     reads=[B_xt[i]], writes=[B_junk, B_ss[i]])
                rstd_from_ss(ssv[i][:], ssv[i][:], float(D), [B_ss[i]], B_ss[i])
                P.op("dve", lambda e: e.scalar_tensor_tensor(out=hf[:], in0=xt[i][:], scalar=ssv[i][:, 0:1], in1=Ad[:], op0=ALU.mult, op1=ALU.mult),
                     reads=[B_xt[i], B_ss[i], B_ab], writes=[B_hf])
                P.op("pool", lambda e: e.tensor_tensor(out=hb[i][:], in0=hf[:], in1=Bd[:], op=ALU.add),
                     reads=[B_hf, B_ab], writes=[B_hb[i]])
                def tr(e):
                    r = None
                    for kc in range(8):
                        r = e.transpose(pbt[:, kc * 128:(kc + 1) * 128], hb[i][:, kc * 128:(kc + 1) * 128], ident_b)
                    return r
                P.op("pe", tr, reads=[B_hb[i], B_const], writes=[B_pbt])
                P.op("act", lambda e: e.activation(out=hT[slot][:, :, col0:col0 + 128], in_=pbt[:].rearrange("p (k t) -> p k t", k=8), func=AF.Copy),
                     reads=[B_pbt], writes=[B_hT[slot]])

            def proj_mm(slot, c0, ntok):
                bk = ctr["mm"] % 4
                ctr["mm"] += 1
                def mm(e):
                    r = None
                    for kc in range(8):
                        r = e.matmul(pb[bk][:, 0:ntok], lhsT=win[:, kc, c0:c0 + 128], rhs=hT[slot][:, kc, 0:ntok], start=(kc == 0), stop=(kc == 7))
                    return r
                P.op("pe", mm, reads=[B_win, B_hT[slot]], writes=[B_pb[bk]])
                return bk

            def qk_block(slot, c0, ntok, norm, rope, gvec, dest, dest_bufs, ri):
                bk = proj_mm(slot, c0, ntok)
                i = ctr["blk"] % 2
                ctr["blk"] += 1
                if not norm and not rope:
                    P.op("act", lambda e: e.activation(out=dest, in_=pb[bk][:, 0:ntok], func=AF.Copy), reads=[B_pb[bk]], writes=dest_bufs)
                    return
                if norm:
                    P.op("act", lambda e: e.activation(out=xg[i][:, 0:ntok], in_=pb[bk][:, 0:ntok], func=AF.Copy, scale=gvec[:, 0:1]),
                         reads=[B_pb[bk], B_small], writes=[B_xg[i]])
                    P.op("act", lambda e: e.activation(out=sq[i][:, 0:ntok], in_=pb[bk][:, 0:ntok], func=AF.Square),
                         reads=[B_pb[bk]], writes=[B_sq[i]])
                    P.op("pe", lambda e: e.matmul(pb[5][:, 0:ntok], lhsT=bones_b, rhs=sq[i][:, 0:ntok], start=True, stop=True),
                         reads=[B_sq[i], B_const], writes=[B_pb[5]])
                    P.op("act", lambda e: e.activation(out=rs[i][:, 0:ntok], in_=pb[5][:, 0:ntok], func=AF.Sqrt, bias=epsc[:, 1:2], scale=1.0),
                         reads=[B_pb[5], B_const], writes=[B_rs[i]])
                    P.op("dve", lambda e: e.reciprocal(out=rs[i][:, 0:ntok], in_=rs[i][:, 0:ntok]), reads=[B_rs[i]], writes=[B_rs[i]])
                else:
                    P.op("act", lambda e: e.activation(out=xg[i][:, 0:ntok], in_=pb[bk][:, 0:ntok], func=AF.Copy),
                         reads=[B_pb[bk]], writes=[B_xg[i]])
                if rope:
                    P.op("pe", lambda e: e.matmul(pb[4][:, 0:ntok], lhsT=swap_b, rhs=xg[i][:, 0:ntok], start=True, stop=True),
                         reads=[B_xg[i], B_const], writes=[B_pb[4]])
                    P.op("dve", lambda e: e.tensor_tensor(out=t1[i][:, 0:ntok], in0=xg[i][:, 0:ntok], in1=rC[ri][:, 0:ntok], op=ALU.mult),
                         reads=[B_xg[i], B_rope[ri]], writes=[B_t1[i]])
                    P.op("dve", lambda e: e.tensor_tensor(out=t2[i][:, 0:ntok], in0=pb[4][:, 0:ntok], in1=rS[ri][:, 0:ntok], op=ALU.mult),
                         reads=[B_pb[4], B_rope[ri]], writes=[B_t2[i]])
                    if norm:
                        P.op("pool", lambda e: e.tensor_tensor(out=t1[i][:, 0:ntok], in0=t1[i][:, 0:ntok], in1=t2[i][:, 0:ntok], op=ALU.add),
                             reads=[B_t1[i], B_t2[i]], writes=[B_t1[i]])
                        P.op("dve", lambda e: e.scalar_tensor_tensor(out=dest, in0=t1[i][:, 0:ntok], scalar=8.0, in1=rs[i][:, 0:ntok], op0=ALU.mult, op1=ALU.mult),
                             reads=[B_t1[i], B_rs[i]], writes=dest_bufs)
                    else:
                        P.op("pool", lambda e: e.tensor_tensor(out=dest, in0=t1[i][:, 0:ntok], in1=t2[i][:, 0:ntok], op=ALU.add),
                             reads=[B_t1[i], B_t2[i]], writes=dest_bufs)
                else:
                    P.op("dve", lambda e: e.scalar_tensor_tensor(out=dest, in0=xg[i][:, 0:ntok], scalar=8.0, in1=rs[i][:, 0:ntok], op0=ALU.mult, op1=ALU.mult),
                         reads=[B_xg[i], B_rs[i]], writes=dest_bufs)

            def v_tiles(slot, ntiles, kt0, gi):
                for tt in range(ntiles):
                    def mm(e, tt=tt):
                        r = None
                        for kc in range(8):
                            r = e.matmul(pb[6][:, 0:256], lhsT=hT[slot][:, kc, tt * 128:(tt + 1) * 128], rhs=win[:, kc, 1280:1536], start=(kc == 0), stop=(kc == 7))
                        return r
                    P.op("pe", mm, reads=[B_win, B_hT[slot]], writes=[B_pb[6]])
                    kt = kt0 + tt
                    pv = pb[6][:, 0:256].rearrange("p (br kv d) -> p br kv d", br=2, kv=2)
                    P.op("act", lambda e, kt=kt, pv=pv: e.activation(out=VA[:, kt, :, 0, 0:64], in_=pv[:, :, 0, :], func=AF.Copy),
                         reads=[B_pb[6]], writes=[B_VA[gi]])
                    P.op("act", lambda e, kt=kt, pv=pv: e.activation(out=VA[:, kt, :, 1, 64:128], in_=pv[:, :, 1, :], func=AF.Copy),
                         reads=[B_pb[6]], writes=[B_VA[gi]])

            slot = 0
            for tt in range(2):
                prep_tile(ctx_d[b * CTX + tt * 128: b * CTX + (tt + 1) * 128, :], A1c, B1c, slot, tt * 128)
            qk_block(slot, 0, 256, True, False, gk, KT[0][:, NQB * 128:NKT * 128], [B_KT[0][NG]], 0)
            qk_block(slot, 128, 256, False, False, None, KT[1][:, NQB * 128:NKT * 128], [B_KT[1][NG]], 0)
            v_tiles(slot, 2, NQB, NG)

            for g in range(NG):
                slot = (g + 1) % 2
                ri = g % 2
                tok0 = b * T + g * 512
                dma("sp", rC[ri][:], ropeC_d[:, g * 512:(g + 1) * 512], writes=[B_rope[ri]])
                dma("sp", rS[ri][:], ropeS_d[:, g * 512:(g + 1) * 512], writes=[B_rope[ri]])
                for tt in range(4):
                    prep_tile(x_d[tok0 + tt * 128: tok0 + (tt + 1) * 128, :], A1, B1, slot, tt * 128)
                qk_block(slot, 0, 512, True, True, gk, KT[0][:, g * 512:(g + 1) * 512], [B_KT[0][g]], ri)
                qk_block(slot, 128, 512, False, True, None, KT[1][:, g * 512:(g + 1) * 512], [B_KT[1][g]], ri)
                v_tiles(slot, 4, g * 4, g)
                for br in range(2):
                    for j in range(4):
                        qi = ctr["qo"] % 2
                        ctr["qo"] += 1
                        qk_block(slot, 256 + br * 512 + j * 128, 512, br == 0, True, gq if br == 0 else None, qo[qi][:], [B_qo[qi]], ri)
                        dma("sp", qt_d[br, :, j, tok0:tok0 + 512], qo[qi][:], reads=[B_qo[qi]], writes=[B_qt[b][g]])
                for gb in range(16):
                    bk = proj_mm(slot, 1536 + gb * 128, 512)
                    gi = ctr["go"] % 2
                    ctr["go"] += 1
                    P.op("act", lambda e, bk=bk, gi=gi: e.activation(out=go[gi][:], in_=pb[bk][:], func=AF.Sigmoid),
                         reads=[B_pb[bk]], writes=[B_go[gi]])
                    dma("act", g_d[gb, :, tok0:tok0 + 512], go[gi][:], reads=[B_go[gi]], writes=[B_g[b][g]])

            stop_at(2)
            P.barrier()
            P.phase += 1
            Cm = PH.sub()
            wbr = [Cm.alloc([128, 4, D], BF16) for _ in range(2)]
            wo = Cm.alloc([128, 8, D], BF16)
            wr = Cm.alloc([128, 8, E], F32)
            brb = Cm.alloc([128, E], F32)
            B_wC = Buf()
            dma("pool", wbr[0][:], wbra_d.rearrange("j p n -> p j n"), writes=[B_wC])
            dma("pool", wbr[1][:], wbrb_d.rearrange("j p n -> p j n"), writes=[B_wC])
            dma("pool", wo[:], wo_d.rearrange("(kc p) n -> p kc n", p=128), writes=[B_wC])
            dma("sp", wr[:], wr_d.rearrange("(kc p) n -> p kc n", p=128), writes=[B_wC])
            bc_load("sp", brb[:], br_d, writes=[B_wC])
            g1bc = Cm.alloc([128, D], F32)
            A2 = Cm.alloc([128, D], F32)
            B2 = Cm.alloc([128, D], F32)
            B_c2 = Buf()
            tmpg2 = Cm.alloc([128, D], F32)
            bc_load("sp", tmpg2[:], n2g_d, writes=[B_c2])
            bc_load("sp", g1bc[:], modrows_d[b, 2 * D:3 * D], reads=[B_modrows], writes=[B_c2])
            bc_load("sp", B2[:], modrows_d[b, 3 * D:4 * D], reads=[B_modrows], writes=[B_c2])
            bc_load("sp", A2[:], modrows_d[b, 4 * D:5 * D], reads=[B_modrows], writes=[B_c2])
            P.op("dve", lambda e: e.scalar_tensor_tensor(out=A2[:], in0=A2[:], scalar=1.0, in1=tmpg2[:], op0=ALU.add, op1=ALU.mult),
                 reads=[B_c2], writes=[B_c2])
            QT = [[Cm.alloc([128, 4, 512], BF16) for _ in range(2)] for _ in range(2)]
            B_QT = [Buf(), Buf()]
            GT = Cm.alloc([128, 16, 512], BF16)
            B_GT = Buf()
            yT = [Cm.alloc([128, 4, 512], BF16) for _ in range(2)]
            B_yT = [Buf(), Buf()]
            rcp = [Cm.alloc([128, 512], F32) for _ in range(2)]
            B_rcp = [Buf(), Buf()]
            mT = Cm.alloc([128, 8, 512], BF16)
            B_mT = Buf()
            ma = Cm.alloc([128, 512], F32)
            mbb = Cm.alloc([128, 512], F32)
            B_ma, B_mbb = Buf(), Buf()
            xc = Cm.alloc([128, D], F32)
            x1 = Cm.alloc([128, D], F32)
            h2f = Cm.alloc([128, D], F32)
            h2b = Cm.alloc([128, D], BF16)
            h2T = Cm.alloc([128, 8, 128], F32)
            junk2 = Cm.alloc([128, D], F32)
            ss2 = Cm.alloc([128, 1], F32)
            B_xc, B_x1t, B_h2f, B_h2b, B_h2T, B_junk2, B_ss2 = Buf(), Buf(), Buf(), Buf(), Buf(), Buf(), Buf()
            lg = Cm.alloc([128, E], F32)
            mx8 = Cm.alloc([128, 8], F32)
            ix8 = Cm.alloc([128, 8], U32)
            e4 = Cm.alloc([128, 4], F32)
            s4 = Cm.alloc([128, 1], F32)
            nmx = Cm.alloc([128, 1], F32)
            Mf = Cm.alloc([128, E], F32)
            Mb = Cm.alloc([128, E], BF16)
            rkf = Cm.alloc([128, E], F32)
            oh = Cm.alloc([128, E], F32)
            B_lg, B_mx8, B_ix8, B_e4, B_M, B_rkf, B_oh = Buf(), Buf(), Buf(), Buf(), Buf(), Buf(), Buf()
            cc = dict(s=0, o=0, p=0)

            LA = 1
            PT2 = [Cm.alloc([128, 1024], BF16) for _ in range(3)]
            B_PT2 = [Buf() for _ in range(3)]

            def attn_items(qslot, br, nblk, nloc):
                if br == 0:
                    kts = [(kt, None) for kt in range(NKT)]
                else:
                    kts = []
                    if nblk > 0:
                        kts.append((nblk - 1, 0))
                    kts.append((nblk, None))
                    if nblk < NQB - 1:
                        kts.append((nblk + 1, 1))
                    kts += [(NQB, None), (NQB + 1, None)]
                oq = 2 + cc["o"] % 2
                cc["o"] += 1
                return [dict(qslot=qslot, br=br, nloc=nloc, kt=kt, mk=mk, first=(ci == 0), last=(ci == len(kts) - 1), oq=oq)
                        for ci, (kt, mk) in enumerate(kts)]

            def attn_s(it):
                br, kt, mk = it["br"], it["kt"], it["mk"]
                ss_ = cc["s"] % 2
                cc["s"] += 1
                it["ss"] = ss_
                gk_ = NG if kt >= NQB else kt // 4
                it["gk"] = gk_
                def smm(e):
                    r = None
                    for kvh in range(2):
                        r0 = kvh * 64
                        rhsq = QT[it["qslot"]][br][r0:r0 + 64, :, it["nloc"] * 128:(it["nloc"] + 1) * 128]
                        o2 = PQ[ss_][:, kvh * 512:(kvh + 1) * 512]
                        r = e.matmul(o2.rearrange("p (j q) -> p j q", j=4), lhsT=KT[br][r0:r0 + 64, kt * 128:(kt + 1) * 128], rhs=rhsq, start=True, stop=(mk is None))
                    if mk is not None:
                        for kvh in range(2):
                            r = e.matmul(PQ[ss_][:, kvh * 512:(kvh + 1) * 512], lhsT=ident_b, rhs=mb_b[:, mk, :], start=False, stop=True)
                    return r
                P.op("pe", smm, reads=[B_KT[br][gk_], B_QT[it["qslot"]], B_const], writes=[B_pb[2 * ss_], B_pb[2 * ss_ + 1]])

            def attn_pv(it):
                br, kt, oq, ss_ = it["br"], it["kt"], it["oq"], it["ss"]
                nloc = it["nloc"]
                pi = cc["p"] % 3
                cc["p"] += 1
                P.op("act", lambda e: e.activation(out=PT2[pi][:], in_=PQ[ss_][:], func=AF.Exp, scale=0.125),
                     reads=[B_pb[2 * ss_], B_pb[2 * ss_ + 1]], writes=[B_PT2[pi]])
                def pv(e):
                    r = None
                    for kvh in range(2):
                        r = e.matmul(PQ[oq][:, kvh * 512:(kvh + 1) * 512], lhsT=VA[:, kt, br, kvh, :], rhs=PT2[pi][:, kvh * 512:(kvh + 1) * 512],
                                     start=it["first"], stop=it["last"])
                    return r
                P.op("pe", pv, reads=[B_VA[it["gk"]], B_PT2[pi]], writes=[B_pb[2 * oq], B_pb[2 * oq + 1]])
                if not it["last"]:
                    return
                for kvh in range(2):
                    ob = 2 * oq + kvh
                    o0, d0 = (0, 64) if kvh == 0 else (64, 0)
                    ri_ = kvh
                    den = pb[ob][d0:d0 + 64, :]
                    if br == 1:
                        P.op("dve", lambda e, ri_=ri_, o0=o0, d0=d0, den=den: e.tensor_tensor(out=rcp[ri_][o0:o0 + 64, :].rearrange("p (j q) -> p j q", j=4),
                                                              in0=den.rearrange("p (j q) -> p j q", j=4),
                                                              in1=sinkE[d0:d0 + 64, :].unsqueeze(2).to_broadcast([64, 4, 128]), op=ALU.add),
                             reads=[B_pb[ob], B_small], writes=[B_rcp[ri_]])
                        P.op("dve", lambda e, ri_=ri_, o0=o0: e.reciprocal(out=rcp[ri_][o0:o0 + 64, :], in_=rcp[ri_][o0:o0 + 64, :]),
                             reads=[B_rcp[ri_]], writes=[B_rcp[ri_]])
                    else:
                        P.op("dve", lambda e, ri_=ri_, o0=o0, den=den: e.reciprocal(out=rcp[ri_][o0:o0 + 64, :], in_=den), reads=[B_pb[ob]], writes=[B_rcp[ri_]])
                    P.op("dve", lambda e, ri_=ri_, o0=o0, ob=ob: e.tensor_tensor(out=yT[br][o0:o0 + 64, :, nloc * 128:(nloc + 1) * 128],
                                                          in0=pb[ob][o0:o0 + 64, :].rearrange("p (j q) -> p j q", j=4),
                                                          in1=rcp[ri_][o0:o0 + 64, :].rearrange("p (j q) -> p j q", j=4), op=ALU.mult),
                         reads=[B_pb[ob], B_rcp[ri_]], writes=[B_yT[br]])

            def load_q(g):
                qslot = g % 2
                tok0 = b * T + g * 512
                for br in range(2):
                    dma("sp", QT[qslot][br][:], qt_d[br, :, :, tok0:tok0 + 512], reads=[B_qt[b][g]], writes=[B_QT[qslot]])

            load_q(0)
            for g in range(NG):
                qslot = g % 2
                tok0 = b * T + g * 512
                if g + 1 < NG:
                    load_q(g + 1)
                dma("sp", GT[:], g_d[:, :, tok0:tok0 + 512].rearrange("c p t -> p c t"), reads=[B_g[b][g]], writes=[B_GT])
                items = []
                for nloc in range(4):
                    for br in range(2):
                        items += attn_items(qslot, br, g * 4 + nloc, nloc)
                for ii in range(len(items) + LA):
                    if ii < len(items):
                        attn_s(items[ii])
                    if ii - LA >= 0:
                        attn_pv(items[ii - LA])
                for fb in range(8):
                    for br in range(2):
                        bk = 4 + br
                        def mm(e, br=br, fb=fb, bk=bk):
                            r = None
                            for j in range(4):
                                r = e.matmul(pb[bk][:], lhsT=wbr[br][:, j, fb * 128:(fb + 1) * 128], rhs=yT[br][:, j, :], start=(j == 0), stop=(j == 3))
                            return r
                        P.op("pe", mm, reads=[B_wC, B_yT[br]], writes=[B_pb[bk]])
                    P.op("dve", lambda e, fb=fb: e.tensor_tensor(out=ma[:], in0=pb[4][:], in1=GT[:, fb, :], op=ALU.mult),
                         reads=[B_pb[4], B_GT], writes=[B_ma])
                    P.op("dve", lambda e, fb=fb: e.tensor_tensor(out=mbb[:], in0=pb[5][:], in1=GT[:, 8 + fb, :], op=ALU.mult),
                         reads=[B_pb[5], B_GT], writes=[B_mbb])
                    P.op("pool", lambda e, fb=fb: e.tensor_tensor(out=mT[:, fb, :], in0=ma[:], in1=mbb[:], op=ALU.add),
                         reads=[B_ma, B_mbb], writes=[B_mT])
                for tt in range(4):
                    ti = (tok0 + tt * 128) // 128
                    rows = slice(tok0 + tt * 128, tok0 + (tt + 1) * 128)
                    dma("sp", xc[:], x_d[rows, :], writes=[B_xc])
                    for nh in range(2):
                        bk = 4 + nh
                        def mm(e, tt=tt, nh=nh, bk=bk):
                            r = None
                            for kc in range(8):
                                r = e.matmul(pb[bk][:], lhsT=mT[:, kc, tt * 128:(tt + 1) * 128], rhs=wo[:, kc, nh * 512:(nh + 1) * 512], start=(kc == 0), stop=(kc == 7))
                            return r
                        P.op("pe", mm, reads=[B_wC, B_mT], writes=[B_pb[bk]])
                        P.op("dve", lambda e, nh=nh, bk=bk: e.tensor_tensor(out=x1[:, nh * 512:(nh + 1) * 512], in0=pb[bk][:], in1=g1bc[:, nh * 512:(nh + 1) * 512], op=ALU.mult),
                             reads=[B_pb[bk], B_c2], writes=[B_x1t])
                    P.op("pool", lambda e: e.tensor_tensor(out=x1[:], in0=x1[:], in1=xc[:], op=ALU.add), reads=[B_x1t, B_xc], writes=[B_x1t])
                    dma("sp", x1_d[rows, :], x1[:], reads=[B_x1t], writes=[B_x1[ti]])
                    P.op("act", lambda e: e.activation(out=junk2[:], in_=x1[:], func=AF.Square, accum_out=ss2[:]),
                         reads=[B_x1t], writes=[B_junk2, B_ss2])
                    rstd_from_ss(ss2[:], ss2[:], float(D), [B_ss2], B_ss2)
                    P.op("dve", lambda e: e.scalar_tensor_tensor(out=h2f[:], in0=x1[:], scalar=ss2[:, 0:1], in1=A2[:], op0=ALU.mult, op1=ALU.mult),
                         reads=[B_x1t, B_ss2, B_c2], writes=[B_h2f])
                    P.op("pool", lambda e: e.tensor_tensor(out=h2f[:], in0=h2f[:], in1=B2[:], op=ALU.add), reads=[B_h2f, B_c2], writes=[B_h2f])
                    P.op("act", lambda e: e.activation(out=h2b[:], in_=h2f[:], func=AF.Copy), reads=[B_h2f], writes=[B_h2b])
                    dma("act", h2_d[rows, :], h2b[:], reads=[B_h2b], writes=[B_h2[ti]])
                    for hh in range(2):
                        def tr(e, hh=hh):
                            r = None
                            for k4 in range(4):
                                kc = hh * 4 + k4
                                r = e.transpose(pb[6][:, k4 * 128:(k4 + 1) * 128], h2f[:, kc * 128:(kc + 1) * 128], ident_f)
                            return r
                        P.op("pe", tr, reads=[B_h2f, B_const], writes=[B_pb[6]])
                        P.op("act", lambda e, hh=hh: e.activation(out=h2T[:, hh * 4:(hh + 1) * 4, :], in_=pb[6][:].rearrange("p (k t) -> p k t", k=4), func=AF.Copy),
                             reads=[B_pb[6]], writes=[B_h2T])
                    def rmm(e):
                        r = None
                        for kc in range(8):
                            r = e.matmul(pb[6][:, 0:E], lhsT=h2T[:, kc, :], rhs=wr[:, kc, :], start=(kc == 0), stop=(kc == 7))
                        return r
                    P.op("pe", rmm, reads=[B_h2T, B_wC], writes=[B_pb[6]])
                    P.op("dve", lambda e: e.tensor_tensor(out=lg[:], in0=pb[6][:, 0:E], in1=brb[:], op=ALU.add), reads=[B_pb[6], B_wC], writes=[B_lg])
                    P.op("dve", lambda e: e.max(out=mx8[:], in_=lg[:]), reads=[B_lg], writes=[B_mx8])
                    P.op("dve", lambda e: e.max_index(out=ix8[:], in_max=mx8[:], in_values=lg[:]), reads=[B_lg, B_mx8], writes=[B_ix8])
                    c0 = ti * TOPK
                    P.op("dve", lambda e, c0=c0: e.tensor_copy(out=r_idx[:, c0:c0 + 4], in_=ix8[:, 0:4]), reads=[B_ix8], writes=[B_ridx])
                    P.op("dve", lambda e: e.tensor_scalar(out=nmx[:], in0=mx8[:, 0:1], scalar1=-1.0, scalar2=None, op0=ALU.mult), reads=[B_mx8], writes=[B_e4])
                    P.op("act", lambda e: e.activation(out=e4[:], in_=mx8[:, 0:4], func=AF.Exp, bias=nmx[:, 0:1], scale=1.0, accum_out=s4[:]),
                         reads=[B_mx8, B_e4], writes=[B_e4])
                    P.op("dve", lambda e: e.reciprocal(out=s4[:], in_=s4[:]), reads=[B_e4], writes=[B_e4])
                    P.op("dve", lambda e, c0=c0: e.tensor_scalar(out=r_w[:, c0:c0 + 4], in0=e4[:], scalar1=s4[:, 0:1], scalar2=None, op0=ALU.mult),
                         reads=[B_e4], writes=[B_rw])
                    P.op("dve", lambda e: e.tensor_scalar(out=Mf[:], in0=lg[:], scalar1=mx8[:, 3:4], scalar2=None, op0=ALU.is_ge), reads=[B_lg, B_mx8], writes=[B_M])
                    P.op("dve", lambda e: e.tensor_copy(out=Mb[:], in_=Mf[:]), reads=[B_M], writes=[B_M])
                    P.op("pe", lambda e: e.matmul(pb[6][:, 0:E], lhsT=ltri_b, rhs=Mb[:], start=True, stop=True), reads=[B_M, B_const], writes=[B_pb[6]])
                    P.op("dve", lambda e: e.tensor_tensor(out=rkf[:], in0=pb[6][:, 0:E], in1=r_run[:], op=ALU.add), reads=[B_pb[6], B_run], writes=[B_rkf])
                    P.op("pe", lambda e: e.matmul(pb[6][:, 0:E], lhsT=ones_t[:], rhs=Mb[:], start=True, stop=True), reads=[B_M, B_const], writes=[B_pb[6]])
                    P.op("dve", lambda e: e.tensor_tensor(out=r_run[:], in0=pb[6][:, 0:E], in1=r_run[:], op=ALU.add), reads=[B_pb[6], B_run], writes=[B_run])
                    for k in range(TOPK):
                        P.op("dve", lambda e, c=c0 + k: e.tensor_scalar(out=oh[:], in0=iota_f[:, 0:E], scalar1=r_idx[:, c:c + 1], scalar2=None, op0=ALU.is_equal),
                             reads=[B_ridx, B_const], writes=[B_oh])
                        P.op("dve", lambda e: e.tensor_tensor(out=oh[:], in0=oh[:], in1=rkf[:], op=ALU.mult), reads=[B_oh, B_rkf], writes=[B_oh])
                        P.op("dve", lambda e, c=c0 + k: e.reduce_sum(out=r_rank[:, c:c + 1], in_=oh[:], axis=AX.X), reads=[B_oh], writes=[B_rrank])

        stop_at(3)
        P.barrier()
        P.phase += 1
        Dm = root.sub()
        cntp = Dm.alloc([128, E], F32)
        mod_ = Dm.alloc([128, E], F32)
        incl = Dm.alloc([128, E], F32)
        base = Dm.alloc([128, E], F32)
        B_d = Buf()
        MT = NTOK // 512
        thr = Dm.alloc([128, MT], F32)
        cmpc = Dm.alloc([128, E, MT], F32)
        P.op("dve", lambda e: e.tensor_scalar(out=thr[:], in0=iota_f[:, 0:MT], scalar1=512.0, scalar2=None, op0=ALU.mult), reads=[B_const], writes=[B_d])
        P.op("dve", lambda e: e.tensor_tensor(out=cmpc[:], in0=r_run[:].unsqueeze(2).to_broadcast([128, E, MT]),
                                              in1=thr[:].unsqueeze(1).to_broadcast([128, E, MT]), op=ALU.is_gt), reads=[B_run, B_d], writes=[B_d])
        P.op("dve", lambda e: e.reduce_sum(out=cntp[:], in_=cmpc[:], axis=AX.X), reads=[B_d], writes=[B_d])
        P.op("dve", lambda e: e.tensor_scalar(out=cntp[:], in0=cntp[:], scalar1=512.0, scalar2=None, op0=ALU.mult), reads=[B_d], writes=[B_d])
        P.op("dve", lambda e: e.tensor_copy(out=incl[:, 0:1], in_=cntp[:, 0:1]), reads=[B_d], writes=[B_d])
        for ei in range(1, E):
            P.op("dve", lambda e, ei=ei: e.tensor_tensor(out=incl[:, ei:ei + 1], in0=incl[:, ei - 1:ei], in1=cntp[:, ei:ei + 1], op=ALU.add), reads=[B_d], writes=[B_d])
        P.op("dve", lambda e: e.tensor_tensor(out=base[:], in0=incl[:], in1=cntp[:], op=ALU.subtract), reads=[B_d], writes=[B_d])
        tef = Dm.alloc([128, NT], F32)
        cmp3 = Dm.alloc([128, NT, E], F32)
        jv = Dm.alloc([128, NT], F32)
        P.op("dve", lambda e: e.tensor_scalar(out=jv[:], in0=iota_f[:, 0:NT], scalar1=512.0, scalar2=None, op0=ALU.mult), reads=[B_const], writes=[B_d])
        P.op("dve", lambda e: e.tensor_tensor(out=cmp3[:], in0=incl[:].unsqueeze(1).to_broadcast([128, NT, E]),
                                              in1=jv[:].unsqueeze(2).to_broadcast([128, NT, E]), op=ALU.is_le), reads=[B_d], writes=[B_d])
        P.op("dve", lambda e: e.reduce_sum(out=tef[:], in_=cmp3[:], axis=AX.X), reads=[B_d], writes=[B_d])
        P.op("dve", lambda e: e.tensor_scalar(out=tef[:], in0=tef[:], scalar1=float(E - 1), scalar2=None, op0=ALU.min), reads=[B_d], writes=[B_d])
        P.op("dve", lambda e: e.tensor_copy(out=te_i[:], in_=tef[0:1, :]), reads=[B_d], writes=[B_te])
        tk = Dm.alloc([128, NT], F32)
        P.op("dve", lambda e: e.scalar_tensor_tensor(out=tk[:], in0=tef[:], scalar=128.0, in1=pcol[:, 0:1].to_broadcast([128, NT]), op0=ALU.mult, op1=ALU.add),
             reads=[B_d, B_const], writes=[B_d])
        P.op("dve", lambda e: e.tensor_copy(out=idx_b1[:], in_=tk[:]), reads=[B_d], writes=[B_te])
        P.op("dve", lambda e: e.tensor_copy(out=idx_b2[:], in_=tef[:]), reads=[B_d], writes=[B_te])
        CH = min(64, NTK)
        oh3 = Dm.alloc([128, CH, E], F32)
        posf = Dm.alloc([128, NTK], F32)
        for c0 in range(0, NTK, CH):
            P.op("dve", lambda e, c0=c0: e.tensor_tensor(out=oh3[:], in0=iota_f[:, 0:E].unsqueeze(1).to_broadcast([128, CH, E]),
                                                         in1=r_idx[:, c0:c0 + CH].unsqueeze(2).to_broadcast([128, CH, E]), op=ALU.is_equal),
                 reads=[B_ridx, B_const], writes=[B_oh])
            P.op("dve", lambda e: e.tensor_tensor(out=oh3[:], in0=oh3[:], in1=base[:].unsqueeze(1).to_broadcast([128, CH, E]), op=ALU.mult),
                 reads=[B_oh, B_d], writes=[B_oh])
            P.op("dve", lambda e, c0=c0: e.reduce_sum(out=posf[:, c0:c0 + CH], in_=oh3[:], axis=AX.X), reads=[B_oh], writes=[B_d])
        P.op("dve", lambda e: e.tensor_tensor(out=posf[:], in0=posf[:], in1=r_rank[:], op=ALU.add), reads=[B_d, B_rrank], writes=[B_d])
        P.op("dve", lambda e: e.tensor_copy(out=r_pos[:], in_=posf[:]), reads=[B_d], writes=[B_rpos])

        hrow = [Dm.alloc([128, D], BF16) for _ in range(3)]
        B_hrow = [Buf() for _ in range(3)]
        for ti in range(NTT):
            s = ti % 3
            dma("sp", hrow[s][:], h2_d[ti * 128:(ti + 1) * 128, :], reads=[B_h2[ti]], writes=[B_hrow[s]])
            for k in range(TOPK):
                c = ti * TOPK + k
                P.op("pool", lambda e, s=s, c=c: e.indirect_dma_start(
                    out=hs_d[:, :], out_offset=bass.IndirectOffsetOnAxis(ap=r_pos[:, c:c + 1], axis=0), in_=hrow[s][:], in_offset=None),
                    reads=[B_hrow[s], B_rpos], writes=[B_hs[c]], dma=True)

        stop_at(4)
        P.barrier()
        P.phase += 1
        Em = root.sub()
        w1 = [Em.alloc([128, 8, 2 * D], BF16) for _ in range(2)]
        w2 = Em.alloc([128, 8, D], BF16)
        NSTG = 5
        stg = [Em.alloc([128, 2048], F32) for _ in range(NSTG)]
        B_stg = [Buf() for _ in range(NSTG)]
        B_w1c = [[Buf() for _ in range(8)] for _ in range(2)]
        B_w2c = [Buf() for _ in range(4)]
        b1 = [Em.alloc([128, 16], F32) for _ in range(2)]
        b2 = [Em.alloc([128, D], F32) for _ in range(2)]
        B_b = [Buf(), Buf()]
        hs = [Em.alloc([128, 4, D], BF16) for _ in range(2)]
        B_hsb = [Buf(), Buf()]
        hsT = [Em.alloc([128, 8, 512], BF16) for _ in range(2)]
        B_hsT = [Buf(), Buf()]
        actT = Em.alloc([128, 8, 512], BF16)
        B_actT = Buf()
        glu = [Em.alloc([128, 512], F32) for _ in range(2)]
        sg = [Em.alloc([128, 512], F32) for _ in range(2)]
        lin = [Em.alloc([128, 512], F32) for _ in range(2)]
        B_glu, B_sg, B_lin = [Buf(), Buf()], [Buf(), Buf()], [Buf(), Buf()]
        yo = [Em.alloc([128, D], F32) for _ in range(2)]
        B_yo = [Buf(), Buf()]
        ec = dict(u=0, y=0, g=0)

        be1_rows = be1_d.rearrange("e p c -> (e p) c")

        sc_ = dict(n=0)

        def load_b(j):
            s = j % 2
            def ld(e_, s=s, j=j):
                r = []
                r.append(e_.indirect_dma_start(out=b1[s][:], out_offset=None, in_=be1_rows,
                                               in_offset=bass.IndirectOffsetOnAxis(ap=idx_b1[:, j:j + 1], axis=0)))
                r.append(e_.indirect_dma_start(out=b2[s][:], out_offset=None, in_=be2_d,
                                               in_offset=bass.IndirectOffsetOnAxis(ap=idx_b2[:, j:j + 1], axis=0)))
                return r
            P.op("pool", ld, reads=[B_te], writes=[B_b[s]], dma=True, ndma=2)

        def load_w1(j, qs=range(8)):
            for q in qs:
                r_ = sc_["n"] % NSTG
                sc_["n"] += 1
                P.op("pool", lambda e_, q=q, r_=r_, j=j: e_.indirect_dma_start(
                    out=stg[r_][:], out_offset=None, in_=we1_d[q], in_offset=bass.IndirectOffsetOnAxis(ap=idx_b1[:, j:j + 1], axis=0)),
                    reads=[B_te], writes=[B_stg[r_]], dma=True)
                P.op("act", lambda e, q=q, r_=r_, j=j: e.activation(out=w1[j % 2][:, q, :], in_=stg[r_][:], func=AF.Copy),
                     reads=[B_stg[r_]], writes=[B_w1c[j % 2][q]])

        def load_w2(j):
            for q in range(4):
                r_ = sc_["n"] % NSTG
                sc_["n"] += 1
                P.op("pool", lambda e_, q=q, r_=r_, j=j: e_.indirect_dma_start(
                    out=stg[r_][:], out_offset=None, in_=we2_d[q], in_offset=bass.IndirectOffsetOnAxis(ap=idx_b1[:, j:j + 1], axis=0)),
                    reads=[B_te], writes=[B_stg[r_]], dma=True)
                P.op("act", lambda e, q=q, r_=r_: e.activation(out=w2[:, 2 * q:2 * q + 2, :].rearrange("p k n -> p (k n)"), in_=stg[r_][:], func=AF.Copy),
                     reads=[B_stg[r_]], writes=[B_w2c[q]])

        def load_h(j):
            s = j % 2
            dma("sp", hs[s][:], hs_d[j * 512:(j + 1) * 512, :].rearrange("(s p) d -> p s d", p=128), reads=B_hs, writes=[B_hsb[s]])

        def moe_transposes(j):
            s = j % 2
            for st in range(4):
                def tr(e, st=st, s=s):
                    r = None
                    for kc in range(8):
                        r = e.transpose(pbt[:, kc * 128:(kc + 1) * 128], hs[s][:, st, kc * 128:(kc + 1) * 128], ident_b)
                    return r
                P.op("pe", tr, reads=[B_hsb[s], B_const], writes=[B_pbt])
                if st % 2 == 0:
                    P.op("act", lambda e, st=st, s=s: e.activation(out=hsT[s][:, :, st * 128:(st + 1) * 128], in_=pbt[:].rearrange("p (k t) -> p k t", k=8), func=AF.Copy),
                         reads=[B_pbt], writes=[B_hsT[s]])
                else:
                    P.op("dve", lambda e, st=st, s=s: e.tensor_copy(out=hsT[s][:, :, st * 128:(st + 1) * 128], in_=pbt[:].rearrange("p (k t) -> p k t", k=8)),
                         reads=[B_pbt], writes=[B_hsT[s]])

        load_b(0)
        load_w1(0)
        load_w2(0)
        load_h(0)
        moe_transposes(0)
        for j in range(NT):
            s = j % 2
            if j + 1 < NT:
                load_b(j + 1)
                load_h(j + 1)
            for c in range(8):
                bks = []
                for half in range(2):
                    bk = ec["u"] % 4
                    ec["u"] += 1
                    col = half * D + c * 128
                    def mm(e, s=s, col=col, bk=bk):
                        r = None
                        for kc in range(8):
                            r = e.matmul(pb[bk][:], lhsT=w1[s][:, kc, col:col + 128], rhs=hsT[s][:, kc, :], start=(kc == 0), stop=(kc == 7))
                        return r
                    P.op("pe", mm, reads=B_w1c[s] + [B_hsT[s]], writes=[B_pb[bk]])
                    bks.append(bk)
                gi = ec["g"] % 2
                ec["g"] += 1
                P.op("dve", lambda e, s=s, c=c, gi=gi, bk=bks[0]: e.tensor_scalar(out=glu[gi][:], in0=pb[bk][:], scalar1=b1[s][:, c:c + 1], scalar2=7.0, op0=ALU.add, op1=ALU.min),
                     reads=[B_pb[bks[0]], B_b[s]], writes=[B_glu[gi]])
                P.op("act", lambda e, gi=gi: e.activation(out=sg[gi][:], in_=glu[gi][:], func=AF.Sigmoid, scale=1.702), reads=[B_glu[gi]], writes=[B_sg[gi]])
                P.op("dve", lambda e, s=s, c=c, gi=gi, bk=bks[1]: e.tensor_scalar(out=lin[gi][:], in0=pb[bk][:], scalar1=b1[s][:, 8 + c:9 + c], scalar2=7.0, op0=ALU.add, op1=ALU.min),
                     reads=[B_pb[bks[1]], B_b[s]], writes=[B_lin[gi]])
                P.op("dve", lambda e, gi=gi: e.tensor_scalar(out=lin[gi][:], in0=lin[gi][:], scalar1=-7.0, scalar2=1.0, op0=ALU.max, op1=ALU.add),
                     reads=[B_lin[gi]], writes=[B_lin[gi]])
                P.op("dve", lambda e, gi=gi: e.tensor_tensor(out=glu[gi][:], in0=glu[gi][:], in1=sg[gi][:], op=ALU.mult),
                     reads=[B_glu[gi], B_sg[gi]], writes=[B_glu[gi]])
                P.op("dve", lambda e, gi=gi, c=c: e.tensor_tensor(out=actT[:, c, :], in0=glu[gi][:], in1=lin[gi][:], op=ALU.mult),
                     reads=[B_glu[gi], B_lin[gi]], writes=[B_actT])
                if j + 1 < NT:
                    load_w1(j + 1, [c])
            if j + 1 < NT:
                moe_transposes(j + 1)
            for st in range(4):
                yi = ec["y"] % 2
                ec["y"] += 1
                for nh in range(2):
                    bk = 4 + nh
                    def mm(e, s=s, st=st, nh=nh, bk=bk):
                        r = None
                        for c in range(8):
                            r = e.matmul(pb[bk][:], lhsT=actT[:, c, st * 128:(st + 1) * 128], rhs=w2[:, c, nh * 512:(nh + 1) * 512], start=(c == 0), stop=(c == 7))
                        return r
                    P.op("pe", mm, reads=B_w2c + [B_actT], writes=[B_pb[bk]])
                    P.op("dve", lambda e, s=s, nh=nh, bk=bk, yi=yi: e.tensor_tensor(out=yo[yi][:, nh * 512:(nh + 1) * 512], in0=pb[bk][:], in1=b2[s][:, nh * 512:(nh + 1) * 512], op=ALU.add),
                         reads=[B_pb[bk], B_b[s]], writes=[B_yo[yi]])
                r0 = j * 512 + st * 128
                dma("sp", ys_d[r0:r0 + 128, :], yo[yi][:], reads=[B_yo[yi]], writes=[B_ys[j]])
            if j + 1 < NT:
                load_w2(j + 1)

        stop_at(5)
        P.barrier()
        P.phase += 1
        Fm = root.sub()
        g2bc = [Fm.alloc([128, D], F32) for _ in range(NB)]
        fgbc = Fm.alloc([128, D], F32)
        B_f = Buf()
        for b in range(NB):
            bc_load("sp", g2bc[b][:], modrows_d[b, 5 * D:6 * D], reads=[B_modrows], writes=[B_f])
        bc_load("sp", fgbc[:], fg_d, writes=[B_f])
        yk = [[Fm.alloc([128, D], F32) for _ in range(TOPK)] for _ in range(2)]
        B_yk = [[Buf() for _ in range(TOPK)] for _ in range(2)]
        x1f = [Fm.alloc([128, D], F32) for _ in range(2)]
        B_x1f = [Buf(), Buf()]
        acc = Fm.alloc([128, D], F32)
        B_acc = Buf()
        jf = Fm.alloc([128, D], F32)
        B_jf = Buf()
        ssf = Fm.alloc([128, 1], F32)
        B_ssf = Buf()
        of = [Fm.alloc([128, D], F32) for _ in range(2)]
        B_of = [Buf(), Buf()]
        for ti in range(NTT):
            s = ti % 2
            b = (ti * 128) // T
            for k in range(TOPK):
                c = ti * TOPK + k
                P.op("pool", lambda e, s=s, k=k, c=c: e.indirect_dma_start(
                    out=yk[s][k][:], out_offset=None, in_=ys_d[:, :], in_offset=bass.IndirectOffsetOnAxis(ap=r_pos[:, c:c + 1], axis=0)),
                    reads=[B_rpos] + B_ys, writes=[B_yk[s][k]], dma=True)
            dma("sp", x1f[s][:], x1_d[ti * 128:(ti + 1) * 128, :], reads=[B_x1[ti]], writes=[B_x1f[s]])
            for k in range(TOPK):
                c = ti * TOPK + k
                if k == 0:
                    P.op("dve", lambda e, s=s, c=c: e.tensor_scalar(out=acc[:], in0=yk[s][0][:], scalar1=r_w[:, c:c + 1], scalar2=None, op0=ALU.mult),
                         reads=[B_yk[s][0], B_rw], writes=[B_acc])
                else:
                    P.op("dve", lambda e, s=s, k=k, c=c: e.scalar_tensor_tensor(out=acc[:], in0=yk[s][k][:], scalar=r_w[:, c:c + 1], in1=acc[:], op0=ALU.mult, op1=ALU.add),
                         reads=[B_yk[s][k], B_rw, B_acc], writes=[B_acc])
            P.op("dve", lambda e, b=b: e.tensor_tensor(out=acc[:], in0=acc[:], in1=g2bc[b][:], op=ALU.mult), reads=[B_acc, B_f], writes=[B_acc])
            P.op("dve", lambda e, s=s: e.tensor_tensor(out=acc[:], in0=acc[:], in1=x1f[s][:], op=ALU.add), reads=[B_acc, B_x1f[s]], writes=[B_acc])
            P.op("act", lambda e: e.activation(out=jf[:], in_=acc[:], func=AF.Square, accum_out=ssf[:]), reads=[B_acc], writes=[B_jf, B_ssf])
            rstd_from_ss(ssf[:], ssf[:], float(D), [B_ssf], B_ssf)
            P.op("dve", lambda e, s=s: e.scalar_tensor_tensor(out=of[s][:], in0=acc[:], scalar=ssf[:, 0:1], in1=fgbc[:], op0=ALU.mult, op1=ALU.mult),
                 reads=[B_acc, B_ssf, B_f], writes=[B_of[s]])
            P.op("sp", lambda e, ti=ti, s=s: e.dma_start(out=out_d[ti * 128:(ti + 1) * 128, :], in_=of[s][:]), reads=[B_of[s]], writes=[Buf()], dma=True, out=True)
    except _Stop:
        pass
    ops = P.ops
    for o in ops:
        for d in o["deps"]:
            ops[d]["sig"] = True
    for o in ops:
        if o["out"] or o["dma"]:
            o["sig"] = True
    nphase = P.phase + 1
    with contextlib.ExitStack() as es:
        engsem = {}
        for ph in range(nphase):
            for en in ENGS[:4]:
                engsem[(ph, en)] = es.enter_context(nc.semaphore("s_%s_%d" % (en, ph)))
        NDS = 8
        dmasem = {q: [es.enter_context(nc.semaphore("d_%s_%d" % (q, i))) for i in range(NDS)] for q in ("sp", "pool", "act")}
        seq = {}
        dcount = {q: [0] * NDS for q in dmasem}
        drr = {q: 0 for q in dmasem}
        for i, o in enumerate(ops):
            if not o["sig"]:
                continue
            if o["dma"]:
                q = o["eng"]
                k = drr[q] % NDS
                drr[q] += 1
                o["sem"] = dmasem[q][k]
                o["semk"] = (q, k)
            else:
                key = (o["phase"], o["eng"])
                seq[key] = seq.get(key, 0) + 1
                o["sem"] = engsem[key]
                o["val"] = seq[key]
        block = es.enter_context(nc.Block())

        def emit(en, e):
            waited = {}
            for i in P.byeng[en]:
                o = ops[i]
                need = {}
                for d in o["deps"]:
                    od = ops[d]
                    if en == "pe" and od["eng"] == "pe" and not od["dma"]:
                        continue
                    sem, val = od["sem"], od["val"]
                    key = id(sem)
                    if key not in need or need[key][1] < val:
                        need[key] = (sem, val)
                for key, (sem, val) in need.items():
                    if waited.get(key, 0) < val:
                        e.wait_ge(sem, val)
                        waited[key] = val
                if o["fn"] is None:
                    continue
                r = o["fn"](e)
                if o["sig"]:
                    if o["dma"]:
                        rl = r if isinstance(r, list) else [r]
                        q, k = o["semk"]
                        for ins in rl:
                            ins.then_inc(o["sem"], 16)
                    else:
                        r.then_inc(o["sem"], 1)
            if en == "sp":
                for k in range(NDS):
                    if dcount["sp"][k] > 0:
                        e.wait_ge(dmasem["sp"][k], dcount["sp"][k])

        for q in dmasem:
            for i in P.byeng[q]:
                o = ops[i]
                if o["dma"] and o["sig"]:
                    qq, k = o["semk"]
                    n = o["ndma"]
                    dcount[qq][k] += 16 * n
                    o["val"] = dcount[qq][k]

        @block.tensor
        def _(e):
            emit("pe", e)

        @block.scalar
        def _(e):
            emit("act", e)

        @block.vector
        def _(e):
            emit("dve", e)

        @block.gpsimd
        def _(e):
            emit("pool", e)

        @block.sync
        def _(e):
            emit("sp", e)
    return nc


def _rope_tables(T):
    half = 32
    inv = (10000.0 ** (-(np.arange(half // 2, dtype=np.float32) * 2.0 / half))).astype(np.float32)
    t = np.arange(T)
    row = (t // GRID_W).astype(np.float32)
    col = (t % GRID_W).astype(np.float32)
    C = np.zeros((128, T), np.float32)
    S = np.zeros((128, T), np.float32)
    for p in range(128):
        d = p % 64
        pos = row if d < 32 else col
        j = d % 16
        ang = (pos * inv[j]).astype(np.float32)
        C[p] = np.cos(ang)
        sgn = -1.0 if (d % 32) < 16 else 1.0
        S[p] = sgn * np.sin(ang)
    return C, S


def _consts():
    cm = np.zeros((128, 4, 128), np.float32)
    cm[:, 0, :] = np.eye(128)
    for m in range(128):
        d = m % 32
        k = m + 16 if d < 16 else m - 16
        cm[k, 1, m] = 1.0
    for k in range(128):
        for m in range(128):
            if k // 64 == m // 64:
                cm[k, 2, m] = 1.0
            if k < m:
                cm[k, 3, m] = 1.0
    mb = np.zeros((128, 2, 4, 128), np.float32)
    kj = np.arange(128)[:, None]
    qi = np.arange(128)[None, :]
    lo = np.where(kj >= qi, 0.0, MASKV).astype(np.float32)
    hi = np.where(kj <= qi, 0.0, MASKV).astype(np.float32)
    mb[:, 0, :, :] = lo[:, None, :]
    mb[:, 1, :, :] = hi[:, None, :]
    iot = np.tile(np.arange(128, dtype=np.float32)[None, :], (128, 1))
    return cm, mb.reshape(128, 2, 512), iot


def _prep_shared(cfg, inp):
    E = cfg["E"]
    f = lambda a: np.ascontiguousarray(a, dtype=np.float32)
    w_in = inp["w_in"][0]
    KVW = 128
    k_a, v_a, k_b, v_b = (w_in[:, i * KVW:(i + 1) * KVW] for i in range(4))
    q_a = w_in[:, 512:1024]
    q_b = w_in[:, 1024:1536]
    g_a = w_in[:, 1536:2560]
    g_b = w_in[:, 2560:3584]

    def pairs(q):
        cols = []
        for j in range(4):
            cols.append(q[:, j * 64:(j + 1) * 64])
            cols.append(q[:, (j + 4) * 64:(j + 5) * 64])
        return np.concatenate(cols, axis=1)

    w_in_p = np.concatenate([k_a, k_b, pairs(q_a), pairs(q_b), v_a, v_b, g_a, g_b], axis=1)

    def brp(w):
        w = w[0]
        return np.stack([np.concatenate([w[j * 64:(j + 1) * 64], w[(j + 4) * 64:(j + 5) * 64]], axis=0) for j in range(4)], axis=0)

    sink = inp["sink"][0]
    sinkT = np.zeros((128, 4), np.float32)
    sinkT[0:64, :] = sink[4:8][None, :]
    sinkT[64:128, :] = sink[0:4][None, :]
    we1 = inp["w_e1"][0]
    we1p = np.concatenate([we1[:, :, 0::2], we1[:, :, 1::2]], axis=2)
    be1 = inp["b_e1"][0]
    be1p = np.concatenate([be1[:, 0::2], be1[:, 1::2]], axis=1)
    be1T = be1p.reshape(E, 16, 128).transpose(0, 2, 1)
    cm, mb, iot = _consts()
    C, S = _rope_tables(cfg["T"])
    return {
        "w_mod": f(inp["w_mod"][0]), "b_mod": f(inp["b_mod"][0]),
        "norm1_g": f(inp["norm1_g"][0]), "norm2_g": f(inp["norm2_g"][0]), "final_g": f(inp["final_g"]),
        "w_in": f(w_in_p),
        "gq2": f(np.tile(inp["q_norm_g"][0], 2).reshape(128, 1)),
        "gk2": f(np.tile(inp["k_norm_g"][0], 2).reshape(128, 1)),
        "sinkT": f(sinkT),
        "w_br_a": f(brp(inp["w_br_a"])), "w_br_b": f(brp(inp["w_br_b"])),
        "w_o": f(inp["w_o"][0]), "w_router": f(inp["w_router"][0]), "b_router": f(inp["b_router"][0]),
        "b_e1T": f(be1T), "b_e2": f(inp["b_e2"][0]),
        **{"w_e1_%d" % q: f(we1p.reshape(E, 8, 128, 2 * D)[:, q, :, :].reshape(E * 128, 2048)) for q in range(8)},
        **{"w_e2_%d" % q: f(inp["w_e2"][0].reshape(E, 8, 128, D).transpose(0, 2, 1, 3)[:, :, 2 * q:2 * q + 2, :].reshape(E * 128, 2048)) for q in range(4)},
        "ropeC": f(C), "ropeS": f(S), "cmats": f(cm), "maskb": f(mb), "iotas": f(iot),
        "pcol": f(np.arange(128, dtype=np.float32)[:, None] + 128.0 * np.arange(8, dtype=np.float32)[None, :]),
    }


def run(cfg, inp, trace=False):
    NCORE, NB, T = cfg["NCORE"], cfg["NB"], cfg["T"]
    nc = build(cfg)
    shared = _prep_shared(cfg, inp)
    in_maps = []
    for c in range(NCORE):
        bs = slice(c * NB, (c + 1) * NB)
        m = dict(shared)
        m["x"] = np.ascontiguousarray(inp["x"][bs].reshape(NB * T, D), dtype=np.float32)
        m["ctx"] = np.ascontiguousarray(inp["ctx"][bs].reshape(NB * CTX, D), dtype=np.float32)
        cv = np.concatenate([inp["c"][bs], inp["c_ctx"][None, :]], axis=0)
        m["cT"] = np.ascontiguousarray(cv.reshape(NB + 1, 8, 128).transpose(2, 1, 0), dtype=np.float32)
        in_maps.append(m)
    res = run_bass_kernel_spmd(nc, in_maps, core_ids=list(range(NCORE)), trace=trace)
    out = np.concatenate([r["out"].reshape(NB, T, D) for r in res.results], axis=0)
    return out.astype(np.float32), res


def kernel(**inputs):
    inp = {k: np.asarray(v) for k, v in inputs.items()}
    out, _ = run(CFG, inp)
    return out
```
